# Optimizing a Trainium2 kernel written in Bass

```python
import math
import jax, jax.numpy as jnp
from jax import lax
import numpy as np

D_MODEL = 1024
BATCH = 8
SEQ = 4096
DEPTH = 1
DEC_BATCH = 4
DEC_SEQ = 8192
PAST_LEN = 128

GRID_W = 64
N_HEADS = 8
N_KV_HEADS = 2
HEAD_DIM = 128
ATTN_WIDTH = N_HEADS * HEAD_DIM
KV_WIDTH = N_KV_HEADS * HEAD_DIM
ROPE_THETA = 10000.0
Q_BLOCK = 128
RNN_WIDTH = D_MODEL
RNN_BLOCKS = 8
RNN_BLOCK_W = RNN_WIDTH // RNN_BLOCKS
CONV_W = 4
CONV_PAD = (2, 1)
LRU_C = 8.0
N_BRANCH = 2
N_EXPERTS = 32
TOP_K = 4
D_FF = D_MODEL
SWIGLU_LIMIT = 7.0
SWIGLU_ALPHA = 1.702
MOE_BLOCK = 512
DN_ALPHA = (2 * DEPTH) ** 0.25
DN_BETA = (8 * DEPTH) ** -0.25
LN_EPS = 1e-5
RMS_EPS = 1e-6
N_MOD = 6
IN_SPLITS = (ATTN_WIDTH, ATTN_WIDTH + KV_WIDTH, ATTN_WIDTH + 2 * KV_WIDTH,
             ATTN_WIDTH + 2 * KV_WIDTH + RNN_WIDTH, ATTN_WIDTH + 2 * KV_WIDTH + 2 * RNN_WIDTH)
IN_WIDTH = ATTN_WIDTH + 2 * KV_WIDTH + 2 * RNN_WIDTH + N_BRANCH * D_MODEL

kernel_name = "hybrid_gqa_rglru_moe_encoder"


def layer_norm(x, gain=None, bias=None):
    xf = x.astype(jnp.float32)
    mu = xf.mean(-1, keepdims=True)
    var = jnp.square(xf - mu).mean(-1, keepdims=True)
    y = (xf - mu) * lax.rsqrt(var + LN_EPS)
    if gain is not None:
        y = y * gain.astype(jnp.float32) + bias.astype(jnp.float32)
    return y.astype(x.dtype)


def rms_norm(x, gain):
    xf = x.astype(jnp.float32)
    y = xf * lax.rsqrt(jnp.mean(xf * xf, axis=-1, keepdims=True) + RMS_EPS) * gain.astype(jnp.float32)
    return y.astype(x.dtype)


def axial_rope_tables(seq_len):
    n_rows = seq_len // GRID_W
    rows = jnp.repeat(jnp.arange(n_rows), GRID_W).astype(jnp.float32)
    cols = jnp.tile(jnp.arange(GRID_W), n_rows).astype(jnp.float32)
    axis_dim = HEAD_DIM // 2
    inv = ROPE_THETA ** (-jnp.arange(0, axis_dim, 2, dtype=jnp.float32) / axis_dim)
    ang_r = rows[:, None] * inv
    ang_c = cols[:, None] * inv
    return (jnp.cos(ang_r), jnp.sin(ang_r), jnp.cos(ang_c), jnp.sin(ang_c))


def rotate(x, cos, sin):
    m = x.shape[-1] // 2
    x1, x2 = x[..., :m], x[..., m:]
    c, s = cos[:, None, :], sin[:, None, :]
    return jnp.concatenate([x1 * c - x2 * s, x2 * c + x1 * s], axis=-1)


def apply_axial_rope(x, tables):
    cos_r, sin_r, cos_c, sin_c = tables
    xf = x.astype(jnp.float32)
    half = HEAD_DIM // 2
    y = jnp.concatenate([rotate(xf[..., :half], cos_r, sin_r), rotate(xf[..., half:], cos_c, sin_c)], axis=-1)
    return y.astype(x.dtype)


def block_attention(q, k, v):
    b, s = q.shape[:2]
    groups = N_HEADS // N_KV_HEADS
    nb = s // Q_BLOCK
    qb = q.reshape(b, nb, Q_BLOCK, N_KV_HEADS, groups, HEAD_DIM).transpose(1, 0, 2, 3, 4, 5)
    scale = HEAD_DIM ** -0.5

    def one_block(qblk):
        sc = jnp.einsum('bqkgd,bskd->bkgqs', qblk, k, preferred_element_type=jnp.float32) * scale
        p = jax.nn.softmax(sc, axis=-1)
        return jnp.einsum('bkgqs,bskd->bqkgd', p.astype(v.dtype), v)

    o = lax.map(one_block, qb)
    return o.transpose(1, 0, 2, 3, 4, 5).reshape(b, s, ATTN_WIDTH)


def block_diag(x, w, bias):
    xb = x.reshape(x.shape[:-1] + (RNN_BLOCKS, RNN_BLOCK_W))
    return jnp.einsum('bsni,nij->bsnj', xb, w).reshape(x.shape) + bias


def linear_scan(a, u, reverse):
    def combine(l, r):
        a_l, u_l = l
        a_r, u_r = r
        return a_r * a_l, a_r * u_l + u_r
    _, h = lax.associative_scan(combine, (a, u), reverse=reverse, axis=1)
    return h


def rg_lru(xc, w_a, b_a, w_x, b_x, lam, reverse):
    r = jax.nn.sigmoid(block_diag(xc, w_a, b_a).astype(jnp.float32))
    i = jax.nn.sigmoid(block_diag(xc, w_x, b_x).astype(jnp.float32))
    log_a = -LRU_C * r * jax.nn.softplus(-lam.astype(jnp.float32))
    a = jnp.exp(log_a)
    u = jnp.sqrt(-jnp.expm1(2.0 * log_a)) * (i * xc.astype(jnp.float32))
    return linear_scan(a, u, reverse)


def recurrent_branch(xr, gate, conv_w, conv_b, lru_wa, lru_ba, lru_wx, lru_bx, lru_lam):
    xc = lax.conv_general_dilated(xr, conv_w[:, None, :], window_strides=(1,), padding=[CONV_PAD],
                                  dimension_numbers=('NWC', 'WIO', 'NWC'),
                                  feature_group_count=RNN_WIDTH) + conv_b
    h = (rg_lru(xc, lru_wa[0], lru_ba[0], lru_wx[0], lru_bx[0], lru_lam[0], False)
         + rg_lru(xc, lru_wa[1], lru_ba[1], lru_wx[1], lru_bx[1], lru_lam[1], True))
    return h.astype(xr.dtype) * jax.nn.gelu(gate)


def mixer(h, rope, w_in, q_gain, k_gain, conv_w, conv_b, lru_wa, lru_ba, lru_wx, lru_bx, lru_lam,
          w_pa, w_pr, w_out):
    b, s, _ = h.shape
    z = h @ w_in
    q, k, v, xr, gr, gl = jnp.split(z, IN_SPLITS, axis=-1)
    q = apply_axial_rope(rms_norm(q.reshape(b, s, N_HEADS, HEAD_DIM), q_gain), rope)
    k = apply_axial_rope(rms_norm(k.reshape(b, s, N_KV_HEADS, HEAD_DIM), k_gain), rope)
    v = v.reshape(b, s, N_KV_HEADS, HEAD_DIM)
    o_att = block_attention(q, k, v) @ w_pa
    o_rec = recurrent_branch(xr, gr, conv_w, conv_b, lru_wa, lru_ba, lru_wx, lru_bx, lru_lam) @ w_pr
    g = jax.nn.sigmoid(gl.astype(jnp.float32)).reshape(b, s, N_BRANCH, D_MODEL)
    merged = (g[:, :, 0] * o_att + g[:, :, 1] * o_rec).astype(h.dtype)
    return merged @ w_out


def moe(h, w_router, b_router, w1, b1, w2, b2):
    b, s, d = h.shape
    xf = h.reshape(-1, d)
    n = xf.shape[0]
    logits = (xf @ w_router + b_router).astype(jnp.float32)
    top_logits, top_idx = lax.top_k(logits, TOP_K)
    top_w = jax.nn.softmax(top_logits, axis=-1)
    n_assign = n * TOP_K
    e_flat = top_idx.reshape(-1)
    order = jnp.argsort(e_flat, stable=True)
    e_sorted = e_flat[order]
    tok_sorted = (order // TOP_K).astype(jnp.int32)
    w_sorted = top_w.reshape(-1)[order]
    counts = jnp.bincount(e_flat, length=N_EXPERTS)
    padded = (counts + MOE_BLOCK - 1) // MOE_BLOCK * MOE_BLOCK
    pad_end = jnp.cumsum(padded)
    pad_start = pad_end - padded
    start = jnp.cumsum(counts) - counts
    dest = pad_start[e_sorted] + jnp.arange(n_assign) - start[e_sorted]
    n_blocks = -(-n_assign // MOE_BLOCK) + N_EXPERTS
    cap = n_blocks * MOE_BLOCK
    slot_tok = jnp.zeros((cap,), jnp.int32).at[dest].set(tok_sorted)
    slot_w = jnp.zeros((cap,), jnp.float32).at[dest].set(w_sorted)
    block_expert = jnp.minimum(
        jnp.searchsorted(pad_end, jnp.arange(n_blocks) * MOE_BLOCK, side='right'), N_EXPERTS - 1)
    xs = xf[slot_tok].reshape(n_blocks, MOE_BLOCK, d)

    def expert_block(args):
        xb, e = args
        gu = xb @ w1[e] + b1[e]
        glu = jnp.minimum(gu[..., :D_FF], SWIGLU_LIMIT)
        lin = jnp.clip(gu[..., D_FF:], -SWIGLU_LIMIT, SWIGLU_LIMIT)
        act = (lin + 1.0) * glu * jax.nn.sigmoid(SWIGLU_ALPHA * glu)
        return act @ w2[e] + b2[e]

    ys = lax.map(expert_block, (xs, block_expert))
    y = jax.ops.segment_sum(ys.reshape(cap, d) * slot_w[:, None].astype(ys.dtype), slot_tok,
                            num_segments=n)
    return y.reshape(b, s, d)


def encoder_layer(x, c, rope, w_ada, b_ada, w_in, q_gain, k_gain, conv_w, conv_b, lru_wa, lru_ba,
                  lru_wx, lru_bx, lru_lam, w_pa, w_pr, w_out, ln1_g, ln1_b, w_router, b_router,
                  w1, b1, w2, b2, ln2_g, ln2_b):
    b = x.shape[0]
    mod = (jax.nn.silu(c) @ w_ada + b_ada).reshape(b, N_MOD, 1, D_MODEL)
    sh1, sc1, g1, sh2, sc2, g2 = (mod[:, j] for j in range(N_MOD))
    h = layer_norm(x) * (1.0 + sc1) + sh1
    mix = mixer(h, rope, w_in, q_gain, k_gain, conv_w, conv_b, lru_wa, lru_ba, lru_wx, lru_bx,
                lru_lam, w_pa, w_pr, w_out)
    x = layer_norm(DN_ALPHA * x + (1.0 + g1) * mix, ln1_g, ln1_b)
    h = layer_norm(x) * (1.0 + sc2) + sh2
    ff = moe(h, w_router, b_router, w1, b1, w2, b2)
    x = layer_norm(DN_ALPHA * x + (1.0 + g2) * ff, ln2_g, ln2_b)
    return x


def run_trunk(x, c, params):
    rope = axial_rope_tables(x.shape[1])
    for l in range(DEPTH):
        x = encoder_layer(x, c, rope, *[p[l] for p in params])
    return x


def setup_inputs(seed: int = 0) -> dict:
    key = jax.random.key(seed)
    ks = jax.random.split(key, 40)
    f32 = jnp.float32

    def nrm(k, shape, scale):
        return jax.random.normal(k, shape, f32) * scale

    u = jax.random.uniform(ks[14], (DEPTH, 2, RNN_WIDTH), f32, minval=0.9, maxval=0.999)
    s_lam = u ** (1.0 / LRU_C)
    lru_lam = jnp.log(s_lam) - jnp.log1p(-s_lam)
    return {
        "x_prompt": nrm(ks[0], (BATCH, SEQ, D_MODEL), 1.0),
        "x_sample": nrm(ks[1], (DEC_BATCH, DEC_SEQ, D_MODEL), 1.0),
        "c_prompt": nrm(ks[2], (BATCH, D_MODEL), 1.0),
        "c_sample": nrm(ks[3], (DEC_BATCH, D_MODEL), 1.0),
        "w_ada": nrm(ks[4], (DEPTH, D_MODEL, N_MOD * D_MODEL), 0.2 * D_MODEL ** -0.5),
        "b_ada": nrm(ks[5], (DEPTH, N_MOD * D_MODEL), 0.01),
        "w_in": nrm(ks[6], (DEPTH, D_MODEL, IN_WIDTH), D_MODEL ** -0.5),
        "q_gain": 1.0 + nrm(ks[7], (DEPTH, HEAD_DIM), 0.02),
        "k_gain": 1.0 + nrm(ks[8], (DEPTH, HEAD_DIM), 0.02),
        "conv_w": nrm(ks[9], (DEPTH, CONV_W, RNN_WIDTH), CONV_W ** -0.5),
        "conv_b": nrm(ks[10], (DEPTH, RNN_WIDTH), 0.01),
        "lru_wa": nrm(ks[11], (DEPTH, 2, RNN_BLOCKS, RNN_BLOCK_W, RNN_BLOCK_W), RNN_BLOCK_W ** -0.5),
        "lru_ba": nrm(ks[12], (DEPTH, 2, RNN_WIDTH), 0.01),
        "lru_wx": nrm(ks[13], (DEPTH, 2, RNN_BLOCKS, RNN_BLOCK_W, RNN_BLOCK_W), RNN_BLOCK_W ** -0.5),
        "lru_bx": nrm(ks[15], (DEPTH, 2, RNN_WIDTH), 0.01),
        "lru_lam": lru_lam,
        "w_pa": nrm(ks[16], (DEPTH, ATTN_WIDTH, D_MODEL), DN_BETA * ATTN_WIDTH ** -0.5),
        "w_pr": nrm(ks[17], (DEPTH, RNN_WIDTH, D_MODEL), DN_BETA * RNN_WIDTH ** -0.5),
        "w_out": nrm(ks[18], (DEPTH, D_MODEL, D_MODEL), DN_BETA * D_MODEL ** -0.5),
        "ln1_g": 1.0 + nrm(ks[19], (DEPTH, D_MODEL), 0.02),
        "ln1_b": nrm(ks[20], (DEPTH, D_MODEL), 0.01),
        "w_router": nrm(ks[21], (DEPTH, D_MODEL, N_EXPERTS), D_MODEL ** -0.5),
        "b_router": nrm(ks[22], (DEPTH, N_EXPERTS), 0.01),
        "w1": nrm(ks[23], (DEPTH, N_EXPERTS, D_MODEL, 2 * D_FF), D_MODEL ** -0.5),
        "b1": nrm(ks[24], (DEPTH, N_EXPERTS, 2 * D_FF), 0.01),
        "w2": nrm(ks[25], (DEPTH, N_EXPERTS, D_FF, D_MODEL), DN_BETA * D_FF ** -0.5),
        "b2": nrm(ks[26], (DEPTH, N_EXPERTS, D_MODEL), 0.01),
        "ln2_g": 1.0 + nrm(ks[27], (DEPTH, D_MODEL), 0.02),
        "ln2_b": nrm(ks[28], (DEPTH, D_MODEL), 0.01),
    }


def reference(x_prompt, x_sample, c_prompt, c_sample, w_ada, b_ada, w_in, q_gain, k_gain, conv_w,
              conv_b, lru_wa, lru_ba, lru_wx, lru_bx, lru_lam, w_pa, w_pr, w_out, ln1_g, ln1_b,
              w_router, b_router, w1, b1, w2, b2, ln2_g, ln2_b):
    params = (w_ada, b_ada, w_in, q_gain, k_gain, conv_w, conv_b, lru_wa, lru_ba, lru_wx, lru_bx,
              lru_lam, w_pa, w_pr, w_out, ln1_g, ln1_b, w_router, b_router, w1, b1, w2, b2,
              ln2_g, ln2_b)
    y_prompt = run_trunk(x_prompt, c_prompt, params)
    y_sample = run_trunk(x_sample, c_sample, params)
    return (y_prompt, y_sample)
```

```python
import numpy as np
import ml_dtypes
from contextlib import ExitStack
import concourse.bass as bass
import concourse.mybir as mybir
from concourse.bass_utils import run_bass_kernel_spmd

F32 = mybir.dt.float32
BF16 = mybir.dt.bfloat16
I32 = mybir.dt.int32
U32 = mybir.dt.uint32
AF = mybir.ActivationFunctionType
ALU = mybir.AluOpType
AX = mybir.AxisListType

D = 1024
NH = 8
NKV = 2
HD = 128
GRID_W = 64
ROPE_THETA = 10000.0
LRU_C = 8.0
TOPK = 4
DFF = 1024
LIMIT = 7.0
ALPHA = 1.702
LN_EPS = 1e-5
RMS_EPS = 1e-6
MB = 512


class Prog:
    COMPUTE = ("pe", "act", "dve", "pool")
    STREAMS = ("pe", "act", "dve", "pool", "sp")

    def __init__(self, nc, stack, ring_sizes=None):
        self.nc = nc
        ring_sizes = ring_sizes or {"sp": 20, "pool": 20, "act": 6}
        self.owners = list(self.COMPUTE)
        self.rings = {}
        for q, n in ring_sizes.items():
            self.rings[q] = []
            for i in range(n):
                self.rings[q].append(len(self.owners))
                self.owners.append(f"d_{q}{i}")
        self.nown = len(self.owners)
        self.sems = [stack.enter_context(nc.semaphore(f"s_{nm}")) for nm in self.owners]
        self.count = [0] * self.nown
        self.clock = {s: [0] * self.nown for s in self.STREAMS}
        self.vcs = [[None] for _ in range(self.nown)]
        self.rr = {q: 0 for q in self.rings}
        self.buf = {}
        self.sigbase = [0] * 4
        self.phase_start = [1] * 4
        self.reset_phase()

    def reset_phase(self):
        self.stream = {s: [] for s in self.STREAMS}
        self.signal = [set() for _ in range(4)]
        for o in range(4):
            self.phase_start[o] = self.count[o] + 1

    def _deps(self, reads, writes, accum_owner=None):
        deps = []
        for k in reads:
            st = self.buf.get(k)
            if st is not None and st[0] is not None:
                deps.append(st[0])
        for k in writes:
            st = self.buf.get(k)
            if st is not None:
                if st[0] is not None and not (accum_owner is not None and st[0][0] == accum_owner and not st[1]):
                    deps.append(st[0])
                for o, n in st[1].items():
                    deps.append((o, n))
        return deps

    def _sync(self, stream, deps):
        clk = self.clock[stream]
        for (o, n) in deps:
            if clk[o] < n:
                self.stream[stream].append(("wait", o, n))
                if o < 4:
                    assert n >= self.phase_start[o], "cross-phase dependency without barrier"
                    self.signal[o].add(n)
                vc = self.vcs[o][n]
                for i in range(self.nown):
                    if vc[i] > clk[i]:
                        clk[i] = vc[i]

    def _record(self, ev, reads, writes):
        for k in reads:
            st = self.buf.get(k)
            if st is None:
                st = self.buf[k] = [None, {}]
            st[1][ev[0]] = ev[1]
        for k in writes:
            self.buf[k] = [ev, {}]

    def op(self, stream, fn, reads=(), writes=(), accum=False):
        o = self.COMPUTE.index(stream)
        self._sync(stream, self._deps(reads, writes, o if accum else None))
        self.count[o] += 1
        n = self.count[o]
        vc = list(self.clock[stream])
        vc[o] = n
        self.vcs[o].append(vc)
        self.stream[stream].append(("op", fn, o, n))
        self._record((o, n), reads, writes)
        return (o, n)

    def dma(self, queue, fn, reads=(), writes=()):
        ring = self.rings[queue]
        o = ring[self.rr[queue] % len(ring)]
        self.rr[queue] += 1
        deps = self._deps(reads, writes)
        prev = self.count[o]
        if prev > 0:
            deps.append((o, prev))
        self._sync(queue, deps)
        self.count[o] = prev + 1
        n = prev + 1
        vc = list(self.clock[queue])
        vc[o] = n
        self.vcs[o].append(vc)
        self.stream[queue].append(("dma", fn, o, n))
        self._record((o, n), reads, writes)
        return (o, n)

    def barrier(self):
        for s in self.STREAMS:
            deps = [(o, self.count[o]) for o in range(self.nown) if self.count[o] > 0]
            self._sync(s, deps)

    def emit(self):
        nc = self.nc
        sig_sorted = [sorted(s) for s in self.signal]
        rank = [{n: self.sigbase[o] + i + 1 for i, n in enumerate(sig_sorted[o])} for o in range(4)]

        def val(o, n):
            if o < 4:
                return rank[o][n]
            return 16 * n

        def run(eng, ents):
            for ent in ents:
                if ent[0] == "wait":
                    eng.wait_ge(self.sems[ent[1]], val(ent[1], ent[2]))
                elif ent[0] == "op":
                    ins = ent[1](eng)
                    if ent[3] in self.signal[ent[2]]:
                        ins.then_inc(self.sems[ent[2]], 1)
                else:
                    ins = ent[1](eng)
                    ins.then_inc(self.sems[ent[2]], 16)

        with nc.Block() as block:
            @block.tensor
            def _(e):
                run(e, self.stream["pe"])

            @block.scalar
            def _(e):
                run(e, self.stream["act"])

            @block.vector
            def _(e):
                run(e, self.stream["dve"])

            @block.gpsimd
            def _(e):
                run(e, self.stream["pool"])

            @block.sync
            def _(e):
                run(e, self.stream["sp"])

        for o in range(4):
            self.sigbase[o] += len(sig_sorted[o])
        self.reset_phase()

    def end_phase(self):
        self.barrier()
        self.emit()


class Cfg:
    def __init__(self, SA=4096, H=4096, NE=32, dbg=()):
        self.SA = SA
        self.H = H
        self.NE = NE
        self.NT = SA + H
        self.NBLK = self.NT * TOPK // MB + NE
        self.CAP = self.NBLK * MB
        self.dbg = tuple(dbg)


W_IN_COLS = 5632


def build_program(cfg):
    nc = bass.Bass("TRN2", target_bir_lowering=False)
    SA, H, NE, NT = cfg.SA, cfg.H, cfg.NE, cfg.NT
    dbg = cfg.dbg

    class _LazyIn:
        def __init__(self, name, shape, dt):
            self.name, self.shape, self.dt, self._ap = name, list(shape), dt, None

        def ap(self):
            if self._ap is None:
                self._ap = nc.dram_tensor(self.name, self.shape, self.dt, kind="ExternalInput").ap()
            return self._ap

        def __getitem__(self, idx):
            return self.ap()[idx]

        def rearrange(self, *a, **k):
            return self.ap().rearrange(*a, **k)

    def din(name, shape, dt=F32):
        return _LazyIn(name, shape, dt)

    def dout(name, shape, dt=F32):
        return nc.dram_tensor(name, list(shape), dt, kind="ExternalOutput").ap()

    def dscr(name, shape, dt=F32):
        kind = "ExternalOutput" if name in dbg else "Internal"
        return nc.dram_tensor(name, list(shape), dt, kind=kind).ap()

    x_seg = {"A": din("xa", [SA, D]), "B": din("xb", [H, D]), "C": din("xc", [H, D])}
    cT = din("cT", [128, 16])
    rope_cos = {"A": din("cosA", [128, SA]), "B": din("cosB", [128, H]), "C": din("cosC", [128, H])}
    rope_sin = {"A": din("sinA", [128, SA]), "B": din("sinB", [128, H]), "C": din("sinC", [128, H])}
    flags = din("flags", [128, 2])
    ident_in = din("ident", [128, 128])
    perm_in = din("perm", [128, 128])
    w_ada = din("w_ada", [D, 6 * D])
    b_adaT = din("b_adaT", [128, 48])
    b_ada_row = din("b_ada_row", [1, 6 * D])
    w_in = din("w_in", [D, W_IN_COLS])
    qk_gain = din("qk_gain", [128, 2])
    conv_wT = din("conv_wT", [128, 8, 4])
    conv_bT = din("conv_bT", [128, 8])
    lru_wa = din("lru_wa", [2, 8, 128, 128])
    lru_wx = din("lru_wx", [2, 8, 128, 128])
    lru_baT = din("lru_baT", [128, 2, 8])
    lru_bxT = din("lru_bxT", [128, 2, 8])
    lru_lamT = din("lru_lamT", [128, 2, 8])
    w_pa = din("w_pa", [D, D])
    w_pr = din("w_pr", [D, D])
    w_out = din("w_out", [D, D])
    ln1_g = din("ln1_g", [1, D])
    ln1_b = din("ln1_b", [1, D])
    w_router = din("w_router", [D, NE])
    b_router = din("b_router", [1, NE])
    w1 = din("w1", [NE * D, 2 * DFF])
    b1T = din("b1T", [NE * 128, 16])
    w2 = din("w2", [NE * DFF, D])
    b2 = din("b2", [NE, D])
    ln2_g = din("ln2_g", [1, D])
    ln2_b = din("ln2_b", [1, D])

    y_seg = {"A": dout("ya", [SA, D]), "B": dout("yb", [H, D])}

    seglen = {"A": SA, "B": H, "C": H}
    QT = {s: dscr(f"QT{s}", [NH, 128, seglen[s]], BF16) for s in "AB"}
    KT = {"A": dscr("KTA", [NKV, 128, SA], BF16), "S": dscr("KTS", [NKV, 128, 2 * H], BF16)}
    VV = {"A": dscr("VA", [SA, 256], BF16), "S": dscr("VS", [2 * H, 256], BF16)}
    XR = {s: dscr(f"XR{s}", [8, 128, seglen[s]], F32) for s in "ABC"}
    GG = {s: dscr(f"GG{s}", [8, 128, seglen[s]], F32) for s in "AB"}
    GL = {s: dscr(f"GL{s}", [16, 128, seglen[s]], F32) for s in "AB"}
    ATT = {s: dscr(f"ATT{s}", [NH, 128, seglen[s]], BF16) for s in "AB"}
    REC = {s: dscr(f"REC{s}", [8, 128, seglen[s]], BF16) for s in "AB"}

    with ExitStack() as gstack:
        P = Prog(nc, gstack)
        sb = lambda name, shape, dt=F32: gstack.enter_context(nc.sbuf_tensor("g_" + name, list(shape), dt))

        ident = sb("ident", [128, 128])
        ident_bf = sb("ident_bf", [128, 128], BF16)
        perm_bf = sb("perm_bf", [128, 128], BF16)
        ones_bf = sb("ones_bf", [128, 128], BF16)
        ones_f = sb("ones_f", [128, 128])
        flags_sb = sb("flags_sb", [128, 2])
        sh1T = sb("sh1T", [128, 2, 8])
        sc1T = sb("sc1T", [128, 2, 8])
        BC = dscr("BC", [128, 2, 4, D])
        gain_sb = sb("gain_sb", [128, 2])
        negM = sb("negM", [128, 1])

        with ExitStack() as st:
            psb = lambda name, shape, dt=F32: st.enter_context(nc.sbuf_tensor("p0_" + name, list(shape), dt))
            pps = lambda name, shape, dt=F32: st.enter_context(nc.psum_tensor("p0_" + name, list(shape), dt))
            bc = psb("bc", [128, 2, 4, D])
            c_sb = psb("c_sb", [128, 16])
            sg_sb = psb("sg_sb", [128, 16])
            sc_sb = psb("sc_sb", [128, 16])
            screp = psb("screp", [128, 2, 8, 128])
            perm_f = psb("perm_f", [128, 128])
            badaT_sb = psb("badaT_sb", [128, 48])
            bada_row_sb = psb("bada_row_sb", [1, 6 * D])
            wad = [psb(f"wad{i}", [128, 8, D]) for i in range(2)]
            gq = psb("gq", [1, 2, 128])
            gmax = psb("gmax", [1, 2])
            gprod = psb("gprod", [1, 1])
            ps_m = pps("ps_m", [128, 32])
            ps_b = [pps(f"ps_b{i}", [128, 512]) for i in range(2)]
            ps_g = pps("ps_g", [128, 1])

            P.dma("sp", lambda e: e.dma_start(out=ident[:], in_=ident_in.ap()), writes=["ident"])
            P.dma("sp", lambda e: e.dma_start(out=perm_f[:], in_=perm_in.ap()), writes=["perm_f"])
            P.dma("sp", lambda e: e.dma_start(out=c_sb[:], in_=cT.ap()), writes=["c_sb"])
            P.dma("sp", lambda e: e.dma_start(out=flags_sb[:], in_=flags.ap()), writes=["flags"])
            P.dma("sp", lambda e: e.dma_start(out=badaT_sb[:], in_=b_adaT.ap()), writes=["badaT"])
            P.dma("sp", lambda e: e.dma_start(out=bada_row_sb[:], in_=b_ada_row.ap()), writes=["bada_row"])
            P.dma("sp", lambda e: e.dma_start(out=gain_sb[:], in_=qk_gain.ap()), writes=["gain"])
            P.dma("sp", lambda e: e.dma_start(out=gq[:], in_=qk_gain.rearrange("p (o g) -> o g p", o=1),
                                              allow_slow_non_contiguous=True), writes=["gq"])
            P.op("dve", lambda e: e.memset(ones_f[:], 1.0), writes=["ones_f"])
            P.op("dve", lambda e: e.memset(ones_bf[:], 1.0), writes=["ones_bf"])
            P.op("dve", lambda e: e.tensor_copy(out=ident_bf[:], in_=ident[:]), reads=["ident"], writes=["ident_bf"])
            P.op("dve", lambda e: e.tensor_copy(out=perm_bf[:], in_=perm_f[:]), reads=["perm_f"], writes=["perm_bf"])
            P.op("dve", lambda e: e.tensor_reduce(out=gmax[:], in_=gq[:], axis=AX.X, op=ALU.max,
                                                  apply_absolute_value=True), reads=["gq"], writes=["gmax"])
            P.op("dve", lambda e: e.scalar_tensor_tensor(out=gprod[:], in0=gmax[:, 0:1], scalar=-float(np.sqrt(128.0)),
                                                         in1=gmax[:, 1:2], op0=ALU.mult, op1=ALU.mult),
                 reads=["gmax"], writes=["gprod"])
            P.op("pe", lambda e: e.matmul(ps_g[:], lhsT=ones_f[0:1, :], rhs=gprod[:], start=True, stop=True),
                 reads=["ones_f", "gprod"], writes=["ps_g"])
            P.op("dve", lambda e: e.tensor_copy(out=negM[:], in_=ps_g[:]), reads=["ps_g"], writes=["negM"])
            P.op("act", lambda e: e.activation(out=sg_sb[:], in_=c_sb[:], func=AF.Sigmoid), reads=["c_sb"], writes=["sg"])
            P.op("dve", lambda e: e.tensor_tensor(out=sc_sb[:], in0=c_sb[:], in1=sg_sb[:], op=ALU.mult),
                 reads=["c_sb", "sg"], writes=["sc"])
            for s in range(2):
                for i in range(8):
                    P.op("dve", lambda e, s=s, i=i: e.tensor_scalar(out=screp[:, s, i, :], in0=ones_f[:],
                                                                   scalar1=sc_sb[:, i * 2 + s:i * 2 + s + 1], scalar2=None,
                                                                   op0=ALU.mult),
                         reads=["ones_f", "sc"], writes=[("screp", s, i)])
            for j in range(6):
                wt = wad[j % 2]
                wk = ("wad", j % 2)
                for half in range(2):
                    P.dma("sp", lambda e, wt=wt, j=j, half=half: e.dma_start(
                        out=wt[:, half * 4:(half + 1) * 4, :],
                        in_=w_ada[half * 512:(half + 1) * 512, j * D:(j + 1) * D].rearrange("(i p) n -> p i n", p=128)),
                        writes=[(wk, half)])
                if j < 2:
                    for k in range(8):
                        for i in range(8):
                            P.op("pe", lambda e, wt=wt, j=j, k=k, i=i: e.matmul(
                                ps_m[:, (j * 8 + k) * 2:(j * 8 + k) * 2 + 2], lhsT=wt[:, i, k * 128:(k + 1) * 128],
                                rhs=sc_sb[:, i * 2:i * 2 + 2], start=(i == 0), stop=(i == 7)),
                                reads=[(wk, i // 4), "sc"], writes=["ps_m"], accum=True)
                    if j == 1:
                        for s in range(2):
                            P.op("dve", lambda e, s=s: e.tensor_tensor(
                                out=sh1T[:, s, :], in0=ps_m[:, s:16:2], in1=badaT_sb[:, 0:8], op=ALU.add),
                                reads=["ps_m", "badaT"], writes=[("sh1T", s)])
                            P.op("dve", lambda e, s=s: e.scalar_tensor_tensor(
                                out=sc1T[:, s, :], in0=ps_m[:, 16 + s:32:2], scalar=1.0, in1=badaT_sb[:, 8:16],
                                op0=ALU.add, op1=ALU.add),
                                reads=["ps_m", "badaT"], writes=[("sc1T", s)])
                else:
                    for s in range(2):
                        for n in range(2):
                            pb = ps_b[(s * 2 + n) % 2]
                            pk = ("ps_b", (s * 2 + n) % 2)
                            for i in range(8):
                                P.op("pe", lambda e, pb=pb, wt=wt, s=s, n=n, i=i: e.matmul(
                                    pb[:], lhsT=screp[:, s, i, :], rhs=wt[:, i, n * 512:(n + 1) * 512],
                                    start=(i == 0), stop=False),
                                    reads=[(wk, i // 4), ("screp", s, i)], writes=[pk], accum=True)
                            P.op("pe", lambda e, pb=pb, j=j, n=n: e.matmul(
                                pb[:], lhsT=ones_f[0:1, :], rhs=bada_row_sb[0:1, j * D + n * 512:j * D + (n + 1) * 512],
                                start=False, stop=True),
                                reads=["ones_f", "bada_row"], writes=[pk], accum=True)
                            addc = 0.0 if j == 3 else 1.0
                            P.op("act" if n == 0 else "dve",
                                 (lambda e, pb=pb, s=s, j=j, n=n, addc=addc: e.activation(
                                     out=bc[:, s, j - 2, n * 512:(n + 1) * 512], in_=pb[:], func=AF.Identity, bias=addc))
                                 if n == 0 else
                                 (lambda e, pb=pb, s=s, j=j, n=n, addc=addc: e.tensor_scalar(
                                     out=bc[:, s, j - 2, n * 512:(n + 1) * 512], in0=pb[:], scalar1=addc, scalar2=None,
                                     op0=ALU.add)),
                                 reads=[pk], writes=[("bc", s, j - 2, n)])
            P.dma("sp", lambda e: e.dma_start(out=BC, in_=bc[:]),
                  reads=[("bc", s_, j_, n_) for s_ in range(2) for j_ in range(4) for n_ in range(2)], writes=["BC"])
            P.end_phase()

        if "p0" in dbg:
            o_sh1 = dout("o_sh1T", [128, 16])
            o_sc1 = dout("o_sc1T", [128, 16])
            o_negM = dout("o_negM", [128, 1])
            P.dma("sp", lambda e: e.dma_start(out=o_sh1, in_=sh1T[:].rearrange("p a b -> p (a b)")))
            P.dma("sp", lambda e: e.dma_start(out=o_sc1, in_=sc1T[:].rearrange("p a b -> p (a b)")))
            P.dma("sp", lambda e: e.dma_start(out=o_negM, in_=negM[:]))
            P.end_phase()

        if "stop0" not in dbg:
            G = locals()
            phase1(nc, P, cfg, G)
            if "stop1" not in dbg:
                if "skip2" not in dbg:
                    phase2(nc, P, cfg, G)
                if "stop2" not in dbg:
                    if "skip3" not in dbg:
                        phase3(nc, P, cfg, G)
                    if "stop3" not in dbg:
                        NTL = NT // 128
                        X1 = dscr("X1", [NT, D])
                        H2 = dscr("H2", [NT, D], BF16)
                        XS = dscr("XS", [cfg.CAP, D], BF16)
                        YS = dscr("YS", [cfg.CAP, D])
                        maskS = sb("maskS", [128, NTL, NE])
                        wS = sb("wS", [128, NTL, NE])
                        d4i = sb("d4i", [128, NTL, 4], I32)
                        w4 = sb("w4", [128, NTL, 4])
                        idxw = sb("idxw", [128, cfg.NBLK, 8], I32)
                        idxb1 = sb("idxb1", [128, cfg.NBLK], I32)
                        idxb2 = sb("idxb2", [128, cfg.NBLK], I32)
                        G = locals()
                        phase4(nc, P, cfg, G)
                        if "stop4" not in dbg:
                            phase5(nc, P, cfg, G)
                            phase6(nc, P, cfg, G)
                            phase7(nc, P, cfg, G)

    return nc


def phase1(nc, P, cfg, G):
    SA, H = cfg.SA, cfg.H
    x_seg, rope_cos, rope_sin = G["x_seg"], G["rope_cos"], G["rope_sin"]
    QT, KT, VV, XR, GG, GL = G["QT"], G["KT"], G["VV"], G["XR"], G["GG"], G["GL"]
    ident, perm_bf, ones_bf = G["ident"], G["perm_bf"], G["ones_bf"]
    sh1T, sc1T, gain_sb, w_in = G["sh1T"], G["sc1T"], G["gain_sb"], G["w_in"]
    with ExitStack() as st:
        psb = lambda name, shape, dt=F32: st.enter_context(nc.sbuf_tensor("p1_" + name, list(shape), dt))
        pps = lambda name, shape, dt=F32: st.enter_context(nc.psum_tensor("p1_" + name, list(shape), dt))
        w_sb = psb("w_in_sb", [128, 8, W_IN_COLS], BF16)
        xbuf = [psb(f"xbuf{i}", [128, 4, D]) for i in range(2)]
        st6 = psb("st6", [128, 4, 2, 6])
        mv = psb("mv", [128, 4, 2])
        sd = psb("sd", [128, 4])
        rstd = psb("rstd", [128, 4])
        nmr = psb("nmr", [128, 4])
        mhalf = psb("mhalf", [128, 512])
        P.op("dve", lambda e: e.memset(mhalf[:], -0.5), writes=["mhalf"])
        hT = [psb(f"hT{i}", [128, 8, 512], BF16) for i in range(2)]
        cosb = [psb(f"cosb{i}", [128, 512]) for i in range(2)]
        sinb = [psb(f"sinb{i}", [128, 512]) for i in range(2)]
        NST = 4
        zc = [psb(f"zc{i}", [128, 512], BF16) for i in range(NST)]
        zsq = [psb(f"zsq{i}", [128, 512], BF16) for i in range(NST)]
        rt = [psb(f"rt{i}", [128, 512]) for i in range(NST)]
        t1 = [psb(f"t1{i}", [128, 512]) for i in range(NST)]
        t2 = [psb(f"t2{i}", [128, 512]) for i in range(NST)]
        qo = [psb(f"qo{i}", [128, 512], BF16) for i in range(NST)]
        fo = [psb(f"fo{i}", [128, 512]) for i in range(3)]
        g1 = [psb(f"g1{i}", [128, 512]) for i in range(2)]
        g2 = [psb(f"g2{i}", [128, 512]) for i in range(2)]
        vo = [psb(f"vo{i}", [128, 4, 256], BF16) for i in range(2)]
        ps_h = [pps(f"ps_h{i}", [128, 512]) for i in range(2)]
        ps_z = [pps(f"ps_z{i}", [128, 512]) for i in range(3)]
        ps_a = [pps(f"ps_a{i}", [128, 512]) for i in range(2)]

        for k in range(8):
            for c0 in range(0, W_IN_COLS, 1408):
                P.dma("pool", lambda e, k=k, c0=c0: e.dma_start(out=w_sb[:, k, c0:c0 + 1408],
                                                               in_=w_in[k * 128:(k + 1) * 128, c0:c0 + 1408]),
                      writes=[("w_sb", k, c0)])
        wkeys = lambda k, c: [("w_sb", k, (c // 1408) * 1408)] + (
            [("w_sb", k, ((c + 127) // 1408) * 1408)] if (c + 127) // 1408 != c // 1408 else [])

        cnt = {"z": 0, "st": 0, "f": 0, "g": 0, "blk": 0}
        segs = [("A", SA, 0, "A", 0), ("B", H, 1, "S", 0), ("C", H, 1, "S", H)]
        blocks = []
        for (seg, ntok, sidx, kvname, kvoff) in segs:
            for b in range(ntok // 512):
                blocks.append((seg, ntok, sidx, kvname, kvoff, b))

        def load_block(bi):
            seg, ntok, sidx, kvname, kvoff, b = blocks[bi]
            t0 = b * 512
            xb_, cb_, sb_ = xbuf[bi % 2], cosb[bi % 2], sinb[bi % 2]
            kx, kn = ("xbuf", bi % 2), ("xn", bi % 2)
            P.dma("sp", lambda e: e.dma_start(
                out=xb_[:], in_=x_seg[seg][t0:t0 + 512, :].rearrange("(t p) d -> p t d", p=128)), writes=[kx] + [(kn, t) for t in range(4)])
            P.dma("sp", lambda e: e.dma_start(out=cb_[:], in_=rope_cos[seg][:, t0:t0 + 512]), writes=[("cos", bi % 2)])
            P.dma("sp", lambda e: e.dma_start(out=sb_[:], in_=rope_sin[seg][:, t0:t0 + 512]), writes=[("sin", bi % 2)])

        def front_gen(bi):
            seg, ntok, sidx, kvname, kvoff, b = blocks[bi]
            xb_, hT_ = xbuf[bi % 2], hT[bi % 2]
            xn_ = xb_
            kx, kn, kh = ("xbuf", bi % 2), ("xn", bi % 2), ("hT", bi % 2)
            for t in range(4):
                for c in range(2):
                    P.op("dve", lambda e, xb_=xb_, t=t, c=c: e.bn_stats(out=st6[:, t, c, :], in_=xb_[:, t, c * 512:(c + 1) * 512]),
                         reads=[kx], writes=[("st6", t, c)])
                P.op("dve", lambda e, t=t: e.bn_aggr(out=mv[:, t, :], in_=st6[:, t, :, :].rearrange("p a b -> p (a b)")),
                     reads=[("st6", t, 0), ("st6", t, 1)], writes=[("mv", t)])
            yield
            P.op("act", lambda e: e.activation(out=sd[:], in_=mv[:, :, 1], func=AF.Sqrt, bias=LN_EPS),
                 reads=[("mv", t) for t in range(4)], writes=["sd"])
            P.op("dve", lambda e: e.reciprocal(out=rstd[:], in_=sd[:]), reads=["sd"], writes=["rstd"])
            P.op("dve", lambda e: e.scalar_tensor_tensor(out=nmr[:], in0=mv[:, :, 0], scalar=-1.0, in1=rstd[:],
                                                         op0=ALU.mult, op1=ALU.mult),
                 reads=[("mv", t) for t in range(4)] + ["rstd"], writes=["nmr"])
            yield
            for t in range(4):
                P.op("act", lambda e, xb_=xb_, xn_=xn_, t=t: e.activation(
                    out=xn_[:, t, :], in_=xb_[:, t, :], func=AF.Identity, scale=rstd[:, t:t + 1], bias=nmr[:, t:t + 1]),
                    reads=[kx, "nmr", "rstd"], writes=[kx, (kn, t)])
            for k in range(8):
                yield
                ph = ps_h[k % 2]
                for t in range(4):
                    P.op("pe", lambda e, ph=ph, xn_=xn_, t=t, k=k: e.transpose(
                        out=ph[:, t * 128:(t + 1) * 128], in_=xn_[:, t, k * 128:(k + 1) * 128], identity=ident[:]),
                        reads=[(kn, t), "ident"], writes=[("ps_h", k % 2)], accum=True)
                if k % 2 == 0:
                    P.op("act", lambda e, ph=ph, hT_=hT_, k=k, sidx=sidx: e.activation(
                        out=hT_[:, k, :], in_=ph[:], func=AF.Identity, scale=sc1T[:, sidx, k:k + 1],
                        bias=sh1T[:, sidx, k:k + 1]), reads=[("ps_h", k % 2), ("sc1T", sidx), ("sh1T", sidx)],
                        writes=[(kh, k)])
                else:
                    P.op("dve", lambda e, ph=ph, hT_=hT_, k=k, sidx=sidx: e.tensor_scalar(
                        out=hT_[:, k, :], in0=ph[:], scalar1=sc1T[:, sidx, k:k + 1], scalar2=sh1T[:, sidx, k:k + 1],
                        op0=ALU.mult, op1=ALU.add), reads=[("ps_h", k % 2), ("sc1T", sidx), ("sh1T", sidx)],
                        writes=[(kh, k)])


        def advance(g_):
            try:
                next(g_)
                return g_
            except StopIteration:
                return None

        load_block(0)
        for _ in front_gen(0):
            pass
        for bi_ in range(len(blocks)):
            if True:
                seg, ntok, sidx, kvname, kvoff, b = blocks[bi_]
                full = seg != "C"
                bi = bi_
                nf = None
                if bi + 1 < len(blocks):
                    load_block(bi + 1)
                    nf = front_gen(bi + 1)
                t0 = b * 512
                xb_, hT_ = xbuf[bi % 2], hT[bi % 2]
                xn_ = xb_
                cb_, sb_ = cosb[bi % 2], sinb[bi % 2]
                kx, kn, kh = ("xbuf", bi % 2), ("xn", bi % 2), ("hT", bi % 2)
                if "hT0" in cfg.dbg and bi == 0:
                    o_hT = nc.dram_tensor("o_hT", [128, 8, 512], BF16, kind="ExternalOutput").ap()
                    o_xn = nc.dram_tensor("o_xn", [128, 4, D], F32, kind="ExternalOutput").ap()
                    P.dma("sp", lambda e, hT_=hT_: e.dma_start(out=o_hT, in_=hT_[:]), reads=[(kh, k) for k in range(8)])
                    P.dma("sp", lambda e, xn_=xn_: e.dma_start(out=o_xn, in_=xn_[:]), reads=[(kn, t) for t in range(4)])

                def zmm(c):
                    zi = cnt["z"] % 3
                    cnt["z"] += 1
                    pz = ps_z[zi]
                    for k in range(8):
                        P.op("pe", lambda e, pz=pz, k=k, c=c, hT_=hT_: e.matmul(
                            pz[:], lhsT=w_sb[:, k, c * 128:(c + 1) * 128], rhs=hT_[:, k, :], start=(k == 0), stop=(k == 7)),
                            reads=wkeys(k, c * 128) + [(kh, k)], writes=[("ps_z", zi)], accum=True)
                    return pz, ("ps_z", zi)

                chunks = list(range(0, 10)) + list(range(12, 44)) if full else [8, 9] + list(range(12, 20))
                for c in chunks:
                    pz, pzk = zmm(c)
                    if c >= 12 and nf is not None:
                        nf = advance(nf)
                    if c < 10:
                        si = cnt["st"] % NST
                        cnt["st"] += 1
                        ai = si % 2
                        gi = 0 if c < 8 else 1
                        zc_, zsq_, rt_, t1_, t2_, qo_ = zc[si], zsq[si], rt[si], t1[si], t2[si], qo[si]
                        P.op("act", lambda e, zc_=zc_, pz=pz, gi=gi: e.activation(out=zc_[:], in_=pz[:], func=AF.Identity,
                                                                            scale=gain_sb[:, gi:gi + 1]),
                             reads=[pzk, "gain"], writes=[("zc", si)])
                        P.op("act", lambda e, zsq_=zsq_, pz=pz: e.activation(out=zsq_[:], in_=pz[:], func=AF.Square),
                             reads=[pzk], writes=[("zsq", si)])
                        pa_ss, pa_rot = ps_a[0], ps_a[1]
                        P.op("pe", lambda e, zsq_=zsq_, pa_ss=pa_ss: e.matmul(pa_ss[:], lhsT=ones_bf[:], rhs=zsq_[:], start=True, stop=True),
                             reads=["ones_bf", ("zsq", si)], writes=[("ps_a", 0)])
                        P.op("pe", lambda e, zc_=zc_, pa_rot=pa_rot: e.matmul(pa_rot[:], lhsT=perm_bf[:], rhs=zc_[:], start=True, stop=True),
                             reads=["perm_bf", ("zc", si)], writes=[("ps_a", 1)])
                        P.op("act", lambda e, rt_=rt_, pa_ss=pa_ss: e.activation(out=rt_[:], in_=pa_ss[:], func=AF.Identity,
                                                                           scale=1.0 / 128.0, bias=RMS_EPS),
                             reads=[("ps_a", 0)], writes=[("rt", si)])
                        P.op("pool", lambda e, t1_=t1_, zc_=zc_, cb_=cb_: e.tensor_tensor(out=t1_[:], in0=zc_[:], in1=cb_[:], op=ALU.mult),
                             reads=[("zc", si), ("cos", bi % 2)], writes=[("t1", si)])
                        P.op("pool", lambda e, rt_=rt_: e.tensor_tensor(out=rt_[:], in0=rt_[:], in1=mhalf[:], op=ALU.pow),
                             reads=[("rt", si), "mhalf"], writes=[("rt", si)])
                        P.op("dve", lambda e, t2_=t2_, pa_rot=pa_rot, sb_=sb_: e.tensor_tensor(out=t2_[:], in0=pa_rot[:], in1=sb_[:], op=ALU.mult),
                             reads=[("ps_a", 1), ("sin", bi % 2)], writes=[("t2", si)])
                        P.op("pool", lambda e, t1_=t1_, t2_=t2_: e.tensor_tensor(out=t1_[:], in0=t1_[:], in1=t2_[:], op=ALU.add),
                             reads=[("t1", si), ("t2", si)], writes=[("t1", si)])
                        P.op("dve", lambda e, qo_=qo_, t1_=t1_, rt_=rt_: e.tensor_tensor(out=qo_[:], in0=t1_[:], in1=rt_[:], op=ALU.mult),
                             reads=[("t1", si), ("rt", si)], writes=[("qo", si)])
                        if c < 8:
                            dst = QT[seg][c, :, t0:t0 + 512]
                        else:
                            dst = KT[kvname][c - 8, :, kvoff + t0:kvoff + t0 + 512]
                        P.dma("sp", lambda e, dst=dst, qo_=qo_: e.dma_start(out=dst, in_=qo_[:]),
                              reads=[("qo", si)], writes=[("QK", seg, c, b)])
                    elif c < 20:
                        fi = cnt["f"] % 3
                        cnt["f"] += 1
                        fo_ = fo[fi]
                        P.op("act" if c % 2 == 0 else "dve",
                             (lambda e, fo_=fo_, pz=pz: e.activation(out=fo_[:], in_=pz[:], func=AF.Identity)) if c % 2 == 0 else
                             (lambda e, fo_=fo_, pz=pz: e.tensor_copy(out=fo_[:], in_=pz[:])),
                             reads=[pzk], writes=[("fo", fi)])
                        P.dma("sp", lambda e, fo_=fo_, seg=seg, c=c, t0=t0: e.dma_start(out=XR[seg][c - 12, :, t0:t0 + 512], in_=fo_[:]),
                              reads=[("fo", fi)], writes=[("XR", seg, c, b)])
                    elif c < 28:
                        gi_ = cnt["g"] % 2
                        cnt["g"] += 1
                        fi = cnt["f"] % 3
                        cnt["f"] += 1
                        g1_, g2_, fo_ = g1[gi_], g2[gi_], fo[fi]
                        P.op("act", lambda e, g1_=g1_, pz=pz: e.activation(out=g1_[:], in_=pz[:], func=AF.Square),
                             reads=[pzk], writes=[("g1", gi_)])
                        P.op("dve", lambda e, g1_=g1_: e.tensor_scalar(out=g1_[:], in0=g1_[:], scalar1=0.044715, scalar2=1.0,
                                                                        op0=ALU.mult, op1=ALU.add),
                             reads=[("g1", gi_)], writes=[("g1", gi_)])
                        P.op("dve", lambda e, g1_=g1_, g2_=g2_, pz=pz: e.tensor_tensor(out=g2_[:], in0=pz[:], in1=g1_[:], op=ALU.mult),
                             reads=[pzk, ("g1", gi_)], writes=[("g2", gi_)])
                        P.op("act", lambda e, g2_=g2_: e.activation(out=g2_[:], in_=g2_[:], func=AF.Sigmoid, scale=1.5957691216057308),
                             reads=[("g2", gi_)], writes=[("g2", gi_)])
                        P.op("dve", lambda e, fo_=fo_, g2_=g2_, pz=pz: e.tensor_tensor(out=fo_[:], in0=pz[:], in1=g2_[:], op=ALU.mult),
                             reads=[pzk, ("g2", gi_)], writes=[("fo", fi)])
                        P.dma("sp", lambda e, fo_=fo_, seg=seg, c=c, t0=t0: e.dma_start(out=GG[seg][c - 20, :, t0:t0 + 512], in_=fo_[:]),
                              reads=[("fo", fi)], writes=[("GG", seg, c, b)])
                    else:
                        fi = cnt["f"] % 3
                        cnt["f"] += 1
                        fo_ = fo[fi]
                        P.op("act", lambda e, fo_=fo_, pz=pz: e.activation(out=fo_[:], in_=pz[:], func=AF.Sigmoid),
                             reads=[pzk], writes=[("fo", fi)])
                        P.dma("sp", lambda e, fo_=fo_, seg=seg, c=c, t0=t0: e.dma_start(out=GL[seg][c - 28, :, t0:t0 + 512], in_=fo_[:]),
                              reads=[("fo", fi)], writes=[("GL", seg, c, b)])
                while nf is not None:
                    nf = advance(nf)
                vo_ = vo[bi % 2]
                for half in range(2):
                    zi = cnt["z"] % 3
                    cnt["z"] += 1
                    pz = ps_z[zi]
                    for tt in range(2):
                        t = half * 2 + tt
                        for k in range(8):
                            P.op("pe", lambda e, pz=pz, tt=tt, t=t, k=k, hT_=hT_: e.matmul(
                                pz[:, tt * 256:(tt + 1) * 256], lhsT=hT_[:, k, t * 128:(t + 1) * 128], rhs=w_sb[:, k, 1280:1536],
                                start=(k == 0), stop=(k == 7)),
                                reads=[(kh, k), ("w_sb", k, 0), ("w_sb", k, 1408)], writes=[("ps_z", zi)], accum=True)
                    P.op("dve", lambda e, vo_=vo_, pz=pz, half=half: e.tensor_copy(
                        out=vo_[:, half * 2:half * 2 + 2, :].rearrange("p a b -> p (a b)"), in_=pz[:]),
                        reads=[("ps_z", zi)], writes=[("vo", bi % 2, half)])
                P.dma("sp", lambda e, vo_=vo_, kvname=kvname, r0=kvoff + t0: e.dma_start(
                    out=VV[kvname][r0:r0 + 512, :].rearrange("(t p) d -> p t d", p=128), in_=vo_[:]),
                    reads=[("vo", bi % 2, 0), ("vo", bi % 2, 1)], writes=[("VV", seg, b)])
        P.end_phase()


def phase2(nc, P, cfg, G):
    SA, H = cfg.SA, cfg.H
    QT, KT, VV, ATT = G["QT"], G["KT"], G["VV"], G["ATT"]
    ones_bf, negM = G["ones_bf"], G["negM"]
    NKMAX = max(SA, 2 * H)
    scale = float(HD) ** -0.5
    with ExitStack() as st:
        psb = lambda name, shape, dt=F32: st.enter_context(nc.sbuf_tensor("p2_" + name, list(shape), dt))
        pps = lambda name, shape, dt=F32: st.enter_context(nc.psum_tensor("p2_" + name, list(shape), dt))
        kT = [psb(f"kT{i}", [128, NKMAX], BF16) for i in range(2)]
        vS = [psb(f"vS{i}", [128, NKMAX // 128, 128], BF16) for i in range(2)]
        qS = [psb(f"qS{i}", [128, 512], BF16) for i in range(3)]
        pS = [psb(f"pS{i}", [128, 1024], BF16) for i in range(3)]
        pA = [psb(f"pA{i}", [128, 512], BF16) for i in range(3)]
        rd = [psb(f"rd{i}", [128, 512]) for i in range(2)]
        ao = [psb(f"ao{i}", [128, 512], BF16) for i in range(2)]
        ps_s = [pps(f"ps_s{i}", [128, 1024]) for i in range(2)]
        ps_o = [pps(f"ps_o{i}", [128, 512]) for i in range(2)]
        ps_d = [pps(f"ps_d{i}", [128, 512]) for i in range(2)]

        groups = []
        qblocks = []
        for (seg, nq, kvname, nk) in (("A", SA, "A", SA), ("B", H, "S", 2 * H)):
            for g in range(NKV):
                gidx = len(groups)
                groups.append((kvname, g, nk))
                for hq in range(4):
                    for qb in range(nq // 512):
                        qblocks.append((seg, g * 4 + hq, qb, gidx))

        def load_group(gidx):
            kvname, g, nk = groups[gidx]
            nkc = nk // 128
            kT_, vS_ = kT[gidx % 2], vS[gidx % 2]
            kk, kvk = ("kT", gidx % 2), ("vS", gidx % 2)
            P.dma("sp", lambda e: e.dma_start(out=kT_[:, 0:nk], in_=KT[kvname][g, :, :]), reads=[("KT", kvname)], writes=[kk])
            for v0 in range(0, nkc, 16):
                v1 = min(nkc, v0 + 16)
                P.dma("sp", lambda e, v0=v0, v1=v1: e.dma_start(
                    out=vS_[:, v0:v1, :], in_=VV[kvname][v0 * 128:v1 * 128, g * 128:(g + 1) * 128].rearrange("(t p) d -> p t d", p=128)),
                    reads=[("VV", kvname)], writes=[kvk])

        def load_q(qi):
            seg, h, qb, gidx = qblocks[qi]
            q_ = qS[qi % 3]
            P.dma("sp", lambda e: e.dma_start(out=q_[:], in_=QT[seg][h, :, qb * 512:(qb + 1) * 512]),
                  reads=[("QT", seg)], writes=[("qS", qi % 3)])

        loaded_groups = set()
        load_group(0)
        loaded_groups.add(0)
        load_q(0)
        si = 0
        pi = 0
        def do_block(qi):
            nonlocal si, pi
            seg, h, qb, gidx = qblocks[qi]
            if qi + 1 < len(qblocks):
                ng = qblocks[qi + 1][3]
                if ng not in loaded_groups:
                    load_group(ng)
                    loaded_groups.add(ng)
                load_q(qi + 1)
            kvname, g, nk = groups[gidx]
            nkp = nk // 256
            kT_, vS_ = kT[gidx % 2], vS[gidx % 2]
            kk, kvk = ("kT", gidx % 2), ("vS", gidx % 2)
            q_ = qS[qi % 3]
            qk = ("qS", qi % 3)
            po, pd = ps_o[qi % 2], ps_d[qi % 2]
            pok, pdk = ("ps_o", qi % 2), ("ps_d", qi % 2)
            rd_, ao_ = rd[qi % 2], ao[qi % 2]
            rdk, aok = ("rd", qi % 2), ("ao", qi % 2)

            def smm(kp):
                nonlocal si
                ps = ps_s[si % 2]
                key = ("ps_s", si % 2)
                si += 1
                for hh in range(2):
                    kc = kp * 2 + hh
                    P.op("pe", lambda e, ps=ps, kc=kc, hh=hh: e.matmul(
                        ps[:, hh * 512:(hh + 1) * 512], lhsT=kT_[:, kc * 128:(kc + 1) * 128], rhs=q_[:], start=True, stop=True),
                        reads=[kk, qk], writes=[(key, hh)])
                return ps, key

            nxt = smm(0)
            for kp in range(nkp):
                cur = nxt
                if kp + 1 < nkp:
                    nxt = smm(kp + 1)
                p_, pa_ = pS[pi % 3], pA[pi % 3]
                pk, pak = ("pS", pi % 3), ("pA", pi % 3)
                pi += 1
                P.op("act", lambda e, p_=p_, cur=cur: e.activation(out=p_[:], in_=cur[0][:], func=AF.Exp, scale=scale, bias=negM[:, 0:1]),
                     reads=[(cur[1], 0), (cur[1], 1), "negM"], writes=[pk])
                for hh in range(2):
                    kc = kp * 2 + hh
                    P.op("pe", lambda e, kc=kc, hh=hh, p_=p_: e.matmul(
                        po[:], lhsT=vS_[:, kc, :], rhs=p_[:, hh * 512:(hh + 1) * 512], start=(kc == 0), stop=(kc == 2 * nkp - 1)),
                        reads=[kvk, pk], writes=[pok], accum=True)
                P.op("dve", lambda e, p_=p_, pa_=pa_: e.tensor_tensor(out=pa_[:], in0=p_[:, 0:512], in1=p_[:, 512:1024], op=ALU.add),
                     reads=[pk], writes=[pak])
                P.op("pe", lambda e, kp=kp, pa_=pa_: e.matmul(pd[:], lhsT=ones_bf[:], rhs=pa_[:], start=(kp == 0), stop=(kp == nkp - 1)),
                     reads=["ones_bf", pak], writes=[pdk], accum=True)
            P.op("dve", lambda e: e.reciprocal(out=rd_[:], in_=pd[:]), reads=[pdk], writes=[rdk])
            P.op("dve", lambda e: e.tensor_tensor(out=ao_[:], in0=po[:], in1=rd_[:], op=ALU.mult), reads=[pok, rdk], writes=[aok])
            P.dma("sp", lambda e: e.dma_start(out=ATT[seg][h, :, qb * 512:(qb + 1) * 512], in_=ao_[:]),
                  reads=[aok], writes=[("ATT", seg, h, qb)])

        for qi in range(len(qblocks)):
            do_block(qi)
        P.end_phase()


def phase3(nc, P, cfg, G):
    SA, H = cfg.SA, cfg.H
    XR, GG, REC = G["XR"], G["GG"], G["REC"]
    ident, flags_sb = G["ident"], G["flags_sb"]
    conv_wT, conv_bT = G["conv_wT"], G["conv_bT"]
    lru_wa, lru_wx = G["lru_wa"], G["lru_wx"]
    SMAX = max(SA, H)
    with ExitStack() as st:
        psb = lambda name, shape, dt=F32: st.enter_context(nc.sbuf_tensor("p3_" + name, list(shape), dt))
        pps = lambda name, shape, dt=F32: st.enter_context(nc.psum_tensor("p3_" + name, list(shape), dt))
        SL = min(1024, SA, H)
        cw = psb("cw", [128, 8, 4])
        cb = psb("cb", [128, 8])
        ba = psb("ba", [128, 16])
        bx = psb("bx", [128, 16])
        lam = psb("lam", [128, 16])
        cf = psb("cf", [128, 16])
        diag = psb("diag", [128, 4, 128])
        wab = [psb(f"wab{i}", [128, 2, 128], BF16) for i in range(2)]
        wxb = [psb(f"wxb{i}", [128, 2, 128], BF16) for i in range(2)]
        xpad = [psb(f"xpad{i}", [128, SMAX + 3]) for i in range(2)]
        hal = psb("hal", [128, 3])
        xc2 = [psb(f"xc{i}", [128, SMAX]) for i in range(2)]
        xcb2 = [psb(f"xcb{i}", [128, SMAX], BF16) for i in range(2)]
        hf = psb("hf", [128, SMAX])
        hbt = psb("hbt", [128, SMAX])
        gg_one = psb("gg0", [128, SMAX])
        gg2 = [gg_one, gg_one]
        r2 = [psb(f"r_{i}", [128, SL]) for i in range(2)]
        i2 = [psb(f"i_{i}", [128, SL]) for i in range(4)]
        a2 = [psb(f"a_{i}", [128, SL]) for i in range(4)]
        t2 = [psb(f"t_{i}", [128, SL]) for i in range(4)]
        hs2 = [psb(f"hs{i}", [128, SL]) for i in range(2)]
        cf2 = psb("cf2", [128, 16])
        carry2 = psb("carry2", [128, 2])
        ro = [psb(f"ro{i}", [128, SL], BF16) for i in range(2)]
        stC = psb("stC", [128, 2])
        init2 = psb("init2", [128, 2])
        zero1 = psb("zero1", [128, 1])
        ps_r2 = [pps(f"ps_r{i}", [128, SL]) for i in range(2)]
        ps_i2 = [pps(f"ps_i{i}", [128, SL]) for i in range(2)]
        ps_r = ps_r2[0]

        P.dma("sp", lambda e: e.dma_start(out=cw[:], in_=conv_wT.ap()), writes=["cw"])
        P.dma("sp", lambda e: e.dma_start(out=cb[:], in_=conv_bT.ap()), writes=["cb"])
        P.dma("sp", lambda e: e.dma_start(out=ba[:], in_=G["lru_baT"].rearrange("p a b -> p (a b)")), writes=["ba"])
        P.dma("sp", lambda e: e.dma_start(out=bx[:], in_=G["lru_bxT"].rearrange("p a b -> p (a b)")), writes=["bx"])
        P.dma("sp", lambda e: e.dma_start(out=lam[:], in_=G["lru_lamT"].rearrange("p a b -> p (a b)")), writes=["lam"])
        P.op("dve", lambda e: e.memset(zero1[:], 0.0), writes=["zero1"])
        P.op("act", lambda e: e.activation(out=cf[:], in_=lam[:], func=AF.Exp, scale=-1.0), reads=["lam"], writes=["cf"])
        P.op("act", lambda e: e.activation(out=cf[:], in_=cf[:], func=AF.Ln, bias=1.0), reads=["cf"], writes=["cf"])
        P.op("dve", lambda e: e.tensor_scalar(out=cf[:], in0=cf[:], scalar1=-LRU_C, scalar2=None, op0=ALU.mult),
             reads=["cf"], writes=["cf"])
        P.op("dve", lambda e: e.tensor_scalar(out=cf2[:], in0=cf[:], scalar1=0.5, scalar2=None, op0=ALU.mult),
             reads=["cf"], writes=["cf2"])
        P.op("dve", lambda e: e.tensor_scalar(out=ba[:], in0=ba[:], scalar1=0.5, scalar2=None, op0=ALU.mult), reads=["ba"], writes=["ba"])
        P.op("dve", lambda e: e.tensor_scalar(out=bx[:], in0=bx[:], scalar1=0.5, scalar2=None, op0=ALU.mult), reads=["bx"], writes=["bx"])

        roi = 0
        jobs = [(n, seg, S, other) for n in range(8) for (seg, S, other) in (("A", SA, None), ("C", H, "B"), ("B", H, "C"))]

        def prep_chunk(n):
            wab_, wxb_ = wab[n % 2], wxb[n % 2]
            P.dma("pool", lambda e: e.dma_start(out=wab_[:], in_=lru_wa[:, n, :, :].rearrange("d i j -> i d j")), writes=[("wab", n % 2)])
            P.dma("pool", lambda e: e.dma_start(out=wxb_[:], in_=lru_wx[:, n, :, :].rearrange("d i j -> i d j")), writes=[("wxb", n % 2)])
            for k in range(4):
                P.op("dve", lambda e, k=k: e.tensor_scalar(out=diag[:, k, :], in0=ident[:], scalar1=cw[:, n, k:k + 1], scalar2=None, op0=ALU.mult),
                     reads=["ident", "cw"], writes=[("diag", k)])

        def load_job(ji):
            n, seg, S, other = jobs[ji]
            own = seg != "C"
            xp = xpad[ji % 2]
            xk = ("xpad", ji % 2)
            P.dma("sp", lambda e: e.dma_start(out=xp[:, 2:S + 2], in_=XR[seg][n, :, :]), reads=[("XR", seg)], writes=[(xk, "d")])
            if other is None:
                P.op("pool", lambda e: e.memset(xp[:, 0:2], 0.0), writes=[(xk, "l")])
                P.op("pool", lambda e: e.memset(xp[:, S + 2:S + 3], 0.0), writes=[(xk, "r")])
            else:
                fl_l = 0 if seg == "B" else 1
                fl_r = 1 if seg == "B" else 0
                P.dma("sp", lambda e: e.dma_start(out=hal[:, 0:2], in_=XR[other][n, :, S - 2:S], allow_slow_non_contiguous=True),
                      reads=[("XR", other)], writes=["hal_l"])
                P.dma("sp", lambda e: e.dma_start(out=hal[:, 2:3], in_=XR[other][n, :, 0:1], allow_slow_non_contiguous=True),
                      reads=[("XR", other)], writes=["hal_r"])
                P.op("dve", lambda e: e.tensor_scalar(out=xp[:, 0:2], in0=hal[:, 0:2], scalar1=flags_sb[:, fl_l:fl_l + 1], scalar2=None, op0=ALU.mult),
                     reads=["hal_l", "flags"], writes=[(xk, "l")])
                P.op("dve", lambda e: e.tensor_scalar(out=xp[:, S + 2:S + 3], in0=hal[:, 2:3], scalar1=flags_sb[:, fl_r:fl_r + 1], scalar2=None, op0=ALU.mult),
                     reads=["hal_r", "flags"], writes=[(xk, "r")])

        def load_gg(ji):
            n, seg, S, other = jobs[ji]
            gg_ = gg2[ji % 2]
            P.dma("sp", lambda e: e.dma_start(out=gg_[:, 0:S], in_=GG[seg][n, :, :]), reads=[("GG", seg)], writes=[("gg", 0)])

        def conv_slab(ji, j):
            n, seg, S, other = jobs[ji]
            xp = xpad[ji % 2]
            xk = ("xpad", ji % 2)
            xc_, xcb_ = xc2[ji % 2], xcb2[ji % 2]
            c0 = j * SL
            for jj in range(SL // 512):
                t0 = c0 + jj * 512
                for k in range(4):
                    P.op("pe", lambda e, jj=jj, k=k, t0=t0: e.matmul(
                        ps_r[:, jj * 512:(jj + 1) * 512], lhsT=diag[:, k, :], rhs=xp[:, t0 + k:t0 + k + 512], start=(k == 0), stop=(k == 3)),
                        reads=[("diag", k), (xk, "d"), (xk, "l"), (xk, "r")], writes=[("ps_r", 0, jj)], accum=True)

        def conv_evac(ji, j):
            n, seg, S, other = jobs[ji]
            xc_, xcb_ = xc2[ji % 2], xcb2[ji % 2]
            c0 = j * SL
            P.op("act", lambda e: e.activation(out=xc_[:, c0:c0 + SL], in_=ps_r[:], func=AF.Identity, bias=cb[:, n:n + 1]),
                 reads=[("ps_r", 0, jj) for jj in range(SL // 512)] + ["cb"], writes=[("xc", ji % 2, c0)])
            P.op("dve", lambda e: e.tensor_copy(out=xcb_[:, c0:c0 + SL], in_=xc_[:, c0:c0 + SL]),
                 reads=[("xc", ji % 2, c0)], writes=[("xcb", ji % 2, c0)])

        def slab_dir(ji, d, si_):
            n, seg, S, other = jobs[ji]
            own = seg != "C"
            nsl = S // SL
            wab_, wxb_ = wab[n % 2], wxb[n % 2]
            xc_, xcb_ = xc2[ji % 2], xcb2[ji % 2]
            pb = d
            c0 = (si_ if d == 0 else nsl - 1 - si_) * SL
            pq = d * 2 + (si_ % 2)
            r_, i_, a_, t_, hs = r2[pb], i2[pq], a2[pq], t2[pq], hs2[pb]
            rk_, ik_, ak_, tk_, hk_ = ("r_", pb), ("i_", pq), ("a_", pq), ("t_", pq), ("hs", pb)
            psr, psi = ps_r2[pb], ps_i2[pb]
            nj = SL // 512
            for j in range(nj):
                t0 = c0 + j * 512
                P.op("pe", lambda e, j=j, t0=t0: e.matmul(
                    psr[:, j * 512:(j + 1) * 512], lhsT=wab_[:, d, :], rhs=xcb_[:, t0:t0 + 512], start=True, stop=True),
                    reads=[("wab", n % 2), ("xcb", ji % 2, c0)], writes=[("ps_r", pb, j)])
                P.op("pe", lambda e, j=j, t0=t0: e.matmul(
                    psi[:, j * 512:(j + 1) * 512], lhsT=wxb_[:, d, :], rhs=xcb_[:, t0:t0 + 512], start=True, stop=True),
                    reads=[("wxb", n % 2), ("xcb", ji % 2, c0)], writes=[("ps_i", pb, j)])
            col = d * 8 + n
            P.op("act", lambda e: e.activation(out=r_[:], in_=psr[:], func=AF.Tanh, scale=0.5, bias=ba[:, col:col + 1]),
                 reads=[("ps_r", pb, j) for j in range(nj)] + ["ba"], writes=[rk_])
            P.op("act", lambda e: e.activation(out=i_[:], in_=psi[:], func=AF.Tanh, scale=0.5, bias=bx[:, col:col + 1]),
                 reads=[("ps_i", pb, j) for j in range(nj)] + ["bx"], writes=[ik_])
            yield
            P.op("act", lambda e: e.activation(out=a_[:], in_=r_[:], func=AF.Exp, scale=cf2[:, col:col + 1], bias=cf2[:, col:col + 1]),
                 reads=[rk_, "cf2"], writes=[ak_])
            P.op("act", lambda e: e.activation(out=t_[:], in_=r_[:], func=AF.Exp, scale=cf[:, col:col + 1], bias=cf[:, col:col + 1]),
                 reads=[rk_, "cf"], writes=[tk_])
            P.op("dve", lambda e: e.scalar_tensor_tensor(out=i_[:], in0=i_[:], scalar=1.0, in1=xc_[:, c0:c0 + SL], op0=ALU.add, op1=ALU.mult),
                 reads=[ik_, ("xc", ji % 2, c0)], writes=[ik_])
            yield
            P.op("act", lambda e: e.activation(out=t_[:], in_=t_[:], func=AF.Sqrt, scale=-0.25, bias=0.25), reads=[tk_], writes=[tk_])
            P.op("pool", lambda e: e.tensor_tensor(out=i_[:], in0=i_[:], in1=t_[:], op=ALU.mult), reads=[ik_, tk_], writes=[ik_])
            yield
            if si_ == 0:
                if seg == "B":
                    init_ap, init_k = init2[:, d:d + 1], "init2"
                else:
                    init_ap, init_k = zero1[:, 0:1], "zero1"
            else:
                init_ap, init_k = carry2[:, d:d + 1], ("carry", d)
            if d == 0:
                dst = hf[:, c0:c0 + SL] if own else hs[:]
                dk = ("hf", c0) if own else hk_
                P.op("dve", lambda e: e.tensor_tensor_scan(out=dst, data0=a_[:], data1=i_[:], initial=init_ap, op0=ALU.mult, op1=ALU.add),
                     reads=[ak_, ik_, init_k], writes=[dk])
                last = hf[:, c0 + SL - 1:c0 + SL] if own else hs[:, SL - 1:SL]
            else:
                dstb = hbt[:, c0:c0 + SL] if own else hs[:]
                dk = ("hb", c0) if own else hk_
                P.op("dve", lambda e: e.tensor_tensor_scan(out=dstb[:, ::-1], data0=a_[:, ::-1], data1=i_[:, ::-1], initial=init_ap,
                                                           op0=ALU.mult, op1=ALU.add),
                     reads=[ak_, ik_, init_k], writes=[dk])
                last = hbt[:, c0:c0 + 1] if own else hs[:, 0:1]
            if si_ + 1 < nsl:
                P.op("dve", lambda e: e.tensor_copy(out=carry2[:, d:d + 1], in_=last), reads=[dk], writes=[("carry", d)])
            elif seg == "C":
                P.op("dve", lambda e: e.tensor_copy(out=stC[:, d:d + 1], in_=last), reads=[dk], writes=["stC"])

        def combine(ji, c0):
            nonlocal roi
            n, seg, S, other = jobs[ji]
            gg_ = gg2[ji % 2]
            ro_ = ro[roi % 2]
            rok = ("ro", roi % 2)
            roi += 1
            P.op("dve", lambda e: e.tensor_tensor(out=hbt[:, c0:c0 + SL], in0=hbt[:, c0:c0 + SL], in1=hf[:, c0:c0 + SL], op=ALU.add),
                 reads=[("hb", c0), ("hf", c0)], writes=[("hb", c0)])
            P.op("pool", lambda e: e.tensor_tensor(out=ro_[:], in0=hbt[:, c0:c0 + SL], in1=gg_[:, c0:c0 + SL], op=ALU.mult),
                 reads=[("hb", c0), ("gg", 0)], writes=[rok])
            P.dma("sp", lambda e: e.dma_start(out=REC[seg][n, :, c0:c0 + SL], in_=ro_[:]), reads=[rok], writes=[("REC", seg, n, c0)])

        prep_chunk(0)
        load_job(0)
        for j in range(jobs[0][2] // SL):
            conv_slab(0, j)
            conv_evac(0, j)
        for ji in range(len(jobs)):
            n, seg, S, other = jobs[ji]
            nsl = S // SL
            nxt_conv = []
            if ji + 1 < len(jobs):
                if jobs[ji + 1][0] != n:
                    prep_chunk(jobs[ji + 1][0])
                load_job(ji + 1)
                nxt_conv = list(range(jobs[ji + 1][2] // SL))
            if seg != "C":
                load_gg(ji)
            if seg == "B":
                P.op("dve", lambda e: e.tensor_tensor(out=init2[:], in0=stC[:], in1=flags_sb[:], op=ALU.mult),
                     reads=["stC", "flags"], writes=["init2"])
            for j in range(nsl):
                alive = [slab_dir(ji, 0, j), slab_dir(ji, 1, j)]
                first = True
                pend_evac = None
                while alive:
                    nxt_alive = []
                    for g_ in alive:
                        try:
                            next(g_)
                            nxt_alive.append(g_)
                        except StopIteration:
                            pass
                    alive = nxt_alive
                    if first and nxt_conv:
                        pend_evac = nxt_conv.pop(0)
                        conv_slab(ji + 1, pend_evac)
                    first = False
                if pend_evac is not None:
                    conv_evac(ji + 1, pend_evac)
                    pend_evac = None
            while nxt_conv:
                jj_ = nxt_conv.pop(0)
                conv_slab(ji + 1, jj_)
                conv_evac(ji + 1, jj_)
            if seg != "C":
                for c0 in range(0, S, SL):
                    combine(ji, c0)
        P.end_phase()


def _bcast_row(nc, P, G, psb, ps, name, src_ap, width, row=None):
    ones_f = G["ones_f"]
    rkey = "rowtmp" if row is not None else name + "_row"
    if row is None:
        row = psb(name + "_row", [1, width])
    out = psb(name, [128, width])
    P.dma("sp", lambda e: e.dma_start(out=row[0:1, 0:width], in_=src_ap), writes=[rkey])
    for n0 in range(0, width, 512):
        w = min(512, width - n0)
        P.op("pe", lambda e, n0=n0, w=w: e.matmul(ps[:, 0:w], lhsT=ones_f[0:1, :], rhs=row[0:1, n0:n0 + w], start=True, stop=True),
             reads=["ones_f", rkey], writes=["ps_t"])
        P.op("dve", lambda e, n0=n0, w=w: e.tensor_copy(out=out[:, n0:n0 + w], in_=ps[:, 0:w]), reads=["ps_t"], writes=[name])
    return out


def _ln_stats(P, tag, src, st6, mv, sd, rstd, srckey, eps=LN_EPS):
    for c in range(2):
        P.op("dve", lambda e, c=c: e.bn_stats(out=st6[:, c, :], in_=src[:, c * 512:(c + 1) * 512]), reads=[srckey], writes=[(tag, "st6", c)])
    P.op("dve", lambda e: e.bn_aggr(out=mv[:], in_=st6[:].rearrange("p a b -> p (a b)")),
         reads=[(tag, "st6", 0), (tag, "st6", 1)], writes=[(tag, "mv")])
    P.op("act", lambda e: e.activation(out=sd[:], in_=mv[:, 1:2], func=AF.Sqrt, bias=eps), reads=[(tag, "mv")], writes=[(tag, "sd")])
    P.op("dve", lambda e: e.reciprocal(out=rstd[:], in_=sd[:]), reads=[(tag, "sd")], writes=[(tag, "rstd")])


def phase4(nc, P, cfg, G):
    SA, H, NE = cfg.SA, cfg.H, cfg.NE
    ATT, REC, GL, BC, X1, H2 = G["ATT"], G["REC"], G["GL"], G["BC"], G["X1"], G["H2"]
    x_seg, ident, ones_f = G["x_seg"], G["ident"], G["ones_f"]
    maskS, wS = G["maskS"], G["wS"]
    alpha = float(2.0 ** 0.25)
    with ExitStack() as st:
        psb = lambda name, shape, dt=F32: st.enter_context(nc.sbuf_tensor("p4_" + name, list(shape), dt))
        pps = lambda name, shape, dt=F32: st.enter_context(nc.psum_tensor("p4_" + name, list(shape), dt))
        wpa = psb("wpa", [128, 8, D], BF16)
        wpr = psb("wpr", [128, 8, D], BF16)
        wo = psb("wo", [128, 8, D], BF16)
        wr = psb("wr", [128, 8, NE])
        attT = [psb(f"attT{i}", [128, 8, 512], BF16) for i in range(2)]
        recT = [psb(f"recT{i}", [128, 8, 512], BF16) for i in range(2)]
        glq = [psb(f"glq{i}", [128, 2, 512]) for i in range(3)]
        mT2 = [psb(f"mT{i}", [128, 8, 512], BF16) for i in range(2)]
        ta = [psb(f"ta{i}", [128, 512]) for i in range(2)]
        tb = [psb(f"tb{i}", [128, 512]) for i in range(2)]
        bcs = psb("bcs", [128, 4, D])
        xt = [psb(f"xt{i}", [128, D]) for i in range(2)]
        yt = [psb(f"yt{i}", [128, D]) for i in range(2)]
        ht = [psb(f"ht{i}", [128, D]) for i in range(2)]
        hb = [psb(f"hb{i}", [128, D], BF16) for i in range(2)]
        h2T2 = [psb(f"h2T{i}", [128, 8, 128]) for i in range(2)]
        st62 = [psb(f"st6{i}", [128, 2, 6]) for i in range(2)]
        mv2 = [psb(f"mv{i}", [128, 2]) for i in range(2)]
        sd2 = [psb(f"sd{i}", [128, 1]) for i in range(2)]
        rstd2 = [psb(f"rstd{i}", [128, 1]) for i in range(2)]
        lg2 = [psb(f"lg{i}", [128, NE]) for i in range(2)]
        m82 = [psb(f"m8{i}", [128, 8]) for i in range(2)]
        nmx2 = [psb(f"nmx{i}", [128, 1]) for i in range(2)]
        ex2 = [psb(f"ex{i}", [128, NE]) for i in range(2)]
        ssum2 = [psb(f"ssum{i}", [128, 1]) for i in range(2)]
        ps_p = [pps(f"ps_p{i}", [128, 512]) for i in range(4)]
        ps_m = [pps(f"ps_m{i}", [128, 512]) for i in range(2)]
        ps_t = pps("ps_t", [128, 512])
        ps_l = pps("ps_l", [128, 512])

        for (wt, src, nm) in ((wpa, G["w_pa"], "wpa"), (wpr, G["w_pr"], "wpr"), (wo, G["w_out"], "wo")):
            for k in range(8):
                P.dma("pool", lambda e, wt=wt, src=src, k=k: e.dma_start(out=wt[:, k, :], in_=src[k * 128:(k + 1) * 128, :]),
                      writes=[(nm, k)])
        P.dma("sp", lambda e: e.dma_start(out=wr[:], in_=G["w_router"].rearrange("(k p) n -> p k n", p=128)), writes=["wr"])
        rowtmp = psb("rowtmp", [1, D])
        g1b = _bcast_row(nc, P, G, psb, ps_t, "ln1g", G["ln1_g"].ap(), D, row=rowtmp)
        b1b = _bcast_row(nc, P, G, psb, ps_t, "ln1b", G["ln1_b"].ap(), D, row=rowtmp)
        brb = _bcast_row(nc, P, G, psb, ps_t, "brb", G["b_router"].ap(), NE, row=rowtmp)

        pp = 0
        gq_i = 0
        blocks = []
        tok0 = 0
        for (seg, ntok, sidx) in (("A", SA, 0), ("B", H, 1)):
            for b in range(ntok // 512):
                blocks.append((seg, sidx, b, b * 512, tok0))
            tok0 += ntok

        def mloop_gen(bidx):
            nonlocal pp, gq_i
            seg, sidx, b, t0, tok0 = blocks[bidx]
            at_, rc_ = attT[bidx % 2], recT[bidx % 2]
            ak, rk = ("attT", bidx % 2), ("recT", bidx % 2)
            mT = mT2[bidx % 2]
            P.dma("sp", lambda e: e.dma_start(out=at_[:], in_=ATT[seg][:, :, t0:t0 + 512].rearrange("h p t -> p h t")),
                  reads=[("ATT", seg)], writes=[ak])
            P.dma("sp", lambda e: e.dma_start(out=rc_[:], in_=REC[seg][:, :, t0:t0 + 512].rearrange("h p t -> p h t")),
                  reads=[("REC", seg)], writes=[rk])
            for m in range(8):
                gl_ = glq[gq_i % 3]
                gk = ("glq", gq_i % 3)
                gq_i += 1
                for hh in range(2):
                    P.dma("sp", lambda e, gl_=gl_, m=m, hh=hh: e.dma_start(out=gl_[:, hh, :], in_=GL[seg][hh * 8 + m, :, t0:t0 + 512]),
                          reads=[("GL", seg)], writes=[gk])
                pa, pr = ps_p[pp % 4], ps_p[(pp + 1) % 4]
                pak, prk = ("ps_p", pp % 4), ("ps_p", (pp + 1) % 4)
                ta_, tb_ = ta[(pp // 2) % 2], tb[(pp // 2) % 2]
                tak, tbk = ("ta", (pp // 2) % 2), ("tb", (pp // 2) % 2)
                pp += 2
                for k in range(8):
                    P.op("pe", lambda e, pa=pa, k=k, m=m: e.matmul(pa[:], lhsT=wpa[:, k, m * 128:(m + 1) * 128], rhs=at_[:, k, :],
                                                                  start=(k == 0), stop=(k == 7)),
                         reads=[("wpa", k), ak], writes=[pak], accum=True)
                for k in range(8):
                    P.op("pe", lambda e, pr=pr, k=k, m=m: e.matmul(pr[:], lhsT=wpr[:, k, m * 128:(m + 1) * 128], rhs=rc_[:, k, :],
                                                                  start=(k == 0), stop=(k == 7)),
                         reads=[("wpr", k), rk], writes=[prk], accum=True)
                P.op("dve", lambda e, ta_=ta_, pa=pa, gl_=gl_: e.tensor_tensor(out=ta_[:], in0=pa[:], in1=gl_[:, 0, :], op=ALU.mult),
                     reads=[pak, gk], writes=[tak])
                P.op("dve", lambda e, tb_=tb_, pr=pr, gl_=gl_: e.tensor_tensor(out=tb_[:], in0=pr[:], in1=gl_[:, 1, :], op=ALU.mult),
                     reads=[prk, gk], writes=[tbk])
                P.op("pool", lambda e, ta_=ta_, tb_=tb_, m=m: e.tensor_tensor(out=mT[:, m, :], in0=ta_[:], in1=tb_[:], op=ALU.add),
                     reads=[tak, tbk], writes=[("mT", bidx % 2, m)])
                yield

        def tile_gen(t, par, bidx):
            seg, sidx, b, t0, tok0 = blocks[bidx]
            mT = mT2[bidx % 2]
            tile_idx = (tok0 + t0) // 128 + t
            r0 = tok0 + t0 + t * 128
            xt_, yt_, ht_, hb_ = xt[par], yt[par], ht[par], hb[par]
            xk, yk, hk, hbk = ("xt", par), ("yt", par), ("ht", par), ("hb", par)
            st6, mv, sd, rstd = st62[par], mv2[par], sd2[par], rstd2[par]
            lg, m8, nmx, ex, ssum, h2T = lg2[par], m82[par], nmx2[par], ex2[par], ssum2[par], h2T2[par]
            tag = ("ln1", par)
            P.dma("sp", lambda e: e.dma_start(out=xt_[:], in_=x_seg[seg][t0 + t * 128:t0 + (t + 1) * 128, :]), writes=[xk])
            for n in range(2):
                pm = ps_m[n]
                for k in range(8):
                    P.op("pe", lambda e, pm=pm, k=k, n=n: e.matmul(pm[:], lhsT=mT[:, k, t * 128:(t + 1) * 128],
                                                                  rhs=wo[:, k, n * 512:(n + 1) * 512], start=(k == 0), stop=(k == 7)),
                         reads=[("mT", bidx % 2, k), ("wo", k)], writes=[("ps_m", n)], accum=True)
                P.op("dve", lambda e, pm=pm, n=n: e.tensor_tensor(out=yt_[:, n * 512:(n + 1) * 512], in0=pm[:],
                                                                 in1=bcs[:, 0, n * 512:(n + 1) * 512], op=ALU.mult),
                     reads=[("ps_m", n), "bcs"], writes=[yk])
            yield
            P.op("dve", lambda e: e.scalar_tensor_tensor(out=yt_[:], in0=xt_[:], scalar=alpha, in1=yt_[:], op0=ALU.mult, op1=ALU.add),
                 reads=[xk, yk], writes=[yk])
            yield
            for c in range(2):
                P.op("dve", lambda e, c=c: e.bn_stats(out=st6[:, c, :], in_=yt_[:, c * 512:(c + 1) * 512]), reads=[yk], writes=[(tag, "st6", c)])
            yield
            P.op("dve", lambda e: e.bn_aggr(out=mv[:], in_=st6[:].rearrange("p a b -> p (a b)")),
                 reads=[(tag, "st6", 0), (tag, "st6", 1)], writes=[(tag, "mv")])
            yield
            P.op("act", lambda e: e.activation(out=sd[:], in_=mv[:, 1:2], func=AF.Sqrt, bias=LN_EPS), reads=[(tag, "mv")], writes=[(tag, "sd")])
            yield
            P.op("dve", lambda e: e.reciprocal(out=rstd[:], in_=sd[:]), reads=[(tag, "sd")], writes=[(tag, "rstd")])
            P.op("dve", lambda e: e.scalar_tensor_tensor(out=yt_[:], in0=yt_[:], scalar=mv[:, 0:1], in1=g1b[:], op0=ALU.subtract, op1=ALU.mult),
                 reads=[yk, (tag, "mv"), "ln1g"], writes=[yk])
            yield
            P.op("dve", lambda e: e.scalar_tensor_tensor(out=yt_[:], in0=yt_[:], scalar=rstd[:, 0:1], in1=b1b[:], op0=ALU.mult, op1=ALU.add),
                 reads=[yk, (tag, "rstd"), "ln1b"], writes=[yk])
            yield
            P.dma("sp", lambda e: e.dma_start(out=X1[r0:r0 + 128, :], in_=yt_[:]), reads=[yk], writes=[("X1", r0)])
            for c in range(2):
                P.op("dve", lambda e, c=c: e.bn_stats(out=st6[:, c, :], in_=yt_[:, c * 512:(c + 1) * 512]), reads=[yk], writes=[(tag, "st6", c)])
            yield
            P.op("dve", lambda e: e.bn_aggr(out=mv[:], in_=st6[:].rearrange("p a b -> p (a b)")),
                 reads=[(tag, "st6", 0), (tag, "st6", 1)], writes=[(tag, "mv")])
            yield
            P.op("act", lambda e: e.activation(out=sd[:], in_=mv[:, 1:2], func=AF.Sqrt, bias=LN_EPS), reads=[(tag, "mv")], writes=[(tag, "sd")])
            yield
            P.op("dve", lambda e: e.reciprocal(out=rstd[:], in_=sd[:]), reads=[(tag, "sd")], writes=[(tag, "rstd")])
            P.op("dve", lambda e: e.scalar_tensor_tensor(out=ht_[:], in0=yt_[:], scalar=mv[:, 0:1], in1=bcs[:, 2, :], op0=ALU.subtract, op1=ALU.mult),
                 reads=[yk, (tag, "mv"), "bcs"], writes=[hk])
            yield
            P.op("dve", lambda e: e.scalar_tensor_tensor(out=ht_[:], in0=ht_[:], scalar=rstd[:, 0:1], in1=bcs[:, 1, :], op0=ALU.mult, op1=ALU.add),
                 reads=[hk, (tag, "rstd"), "bcs"], writes=[hk])
            yield
            P.op("act", lambda e: e.activation(out=hb_[:], in_=ht_[:], func=AF.Identity), reads=[hk], writes=[hbk])
            P.dma("sp", lambda e: e.dma_start(out=H2[r0:r0 + 128, :], in_=hb_[:]), reads=[hbk], writes=[("H2", r0)])
            for half in range(2):
                for kk in range(4):
                    k = half * 4 + kk
                    P.op("pe", lambda e, kk=kk, k=k: e.transpose(out=ps_t[:, kk * 128:(kk + 1) * 128], in_=ht_[:, k * 128:(k + 1) * 128],
                                                                identity=ident[:]),
                         reads=[hk, "ident"], writes=["ps_t"], accum=True)
                P.op("act" if half == 0 else "dve",
                     (lambda e, half=half: e.activation(out=h2T[:, half * 4:(half + 1) * 4, :].rearrange("p a b -> p (a b)"), in_=ps_t[:], func=AF.Identity))
                     if half == 0 else
                     (lambda e, half=half: e.tensor_copy(out=h2T[:, half * 4:(half + 1) * 4, :].rearrange("p a b -> p (a b)"), in_=ps_t[:])),
                     reads=["ps_t"], writes=[("h2T", par, half)])
            yield
            for k in range(8):
                P.op("pe", lambda e, k=k: e.matmul(ps_l[:, 0:NE], lhsT=h2T[:, k, :], rhs=wr[:, k, :], start=(k == 0), stop=(k == 7)),
                     reads=[("h2T", par, k // 4), "wr"], writes=["ps_l"], accum=True)
            P.op("dve", lambda e: e.tensor_tensor(out=lg[:], in0=ps_l[:, 0:NE], in1=brb[:], op=ALU.add), reads=["ps_l", "brb"], writes=[("lg", par)])
            yield
            P.op("dve", lambda e: e.max(out=m8[:], in_=lg[:]), reads=[("lg", par)], writes=[("m8", par)])
            yield
            P.op("dve", lambda e: e.tensor_scalar(out=maskS[:, tile_idx, :], in0=lg[:], scalar1=m8[:, 3:4], scalar2=None, op0=ALU.is_ge),
                 reads=[("lg", par), ("m8", par)], writes=[("maskS", tile_idx)])
            P.op("dve", lambda e: e.tensor_scalar(out=nmx[:], in0=m8[:, 0:1], scalar1=-1.0, scalar2=None, op0=ALU.mult),
                 reads=[("m8", par)], writes=[("nmx", par)])
            yield
            P.op("act", lambda e: e.activation(out=ex[:], in_=lg[:], func=AF.Exp, bias=nmx[:, 0:1]), reads=[("lg", par), ("nmx", par)], writes=[("ex", par)])
            yield
            P.op("dve", lambda e: e.tensor_tensor(out=ex[:], in0=ex[:], in1=maskS[:, tile_idx, :], op=ALU.mult),
                 reads=[("ex", par), ("maskS", tile_idx)], writes=[("ex", par)])
            yield
            P.op("dve", lambda e: e.reduce_sum(out=ssum[:], in_=ex[:], axis=AX.X), reads=[("ex", par)], writes=[("ssum", par)])
            yield
            P.op("dve", lambda e: e.reciprocal(out=ssum[:], in_=ssum[:]), reads=[("ssum", par)], writes=[("ssum", par)])
            yield
            P.op("dve", lambda e: e.tensor_scalar(out=wS[:, tile_idx, :], in0=ex[:], scalar1=ssum[:, 0:1], scalar2=None, op0=ALU.mult),
                 reads=[("ex", par), ("ssum", par)], writes=[("wS", tile_idx)])


        def drain(g_):
            for _ in g_:
                pass

        drain(mloop_gen(0))
        cur_sidx = None
        for bidx in range(len(blocks)):
            seg, sidx, b, t0, tok0 = blocks[bidx]
            if sidx != cur_sidx:
                P.dma("sp", lambda e, sidx=sidx: e.dma_start(out=bcs[:], in_=BC[:, sidx, :, :]), reads=["BC"], writes=["bcs"])
                cur_sidx = sidx
            nxt = mloop_gen(bidx + 1) if bidx + 1 < len(blocks) else None
            rounds = 0
            for tp in range(2):
                gens = [tile_gen(tp * 2, 0, bidx), tile_gen(tp * 2 + 1, 1, bidx)]
                alive = [True, True]
                lag = 2
                step = 0
                while any(alive):
                    for gi_, g_ in enumerate(gens):
                        if not alive[gi_]:
                            continue
                        if gi_ == 1 and step < lag:
                            continue
                        try:
                            next(g_)
                        except StopIteration:
                            alive[gi_] = False
                    step += 1
                    rounds += 1
                    if nxt is not None and rounds % 5 == 0:
                        try:
                            next(nxt)
                        except StopIteration:
                            nxt = None
            if nxt is not None:
                drain(nxt)
        P.end_phase()


def phase5(nc, P, cfg, G):
    NE, NT, NBLK = cfg.NE, cfg.NT, cfg.NBLK
    NTL = NT // 128
    H2, XS = G["H2"], G["XS"]
    maskS, wS, d4i, w4, idxw, idxb1, idxb2 = G["maskS"], G["wS"], G["d4i"], G["w4"], G["idxw"], G["idxb1"], G["idxb2"]
    ones_bf = G["ones_bf"]
    W = NTL * NE
    with ExitStack() as st:
        psb = lambda name, shape, dt=F32: st.enter_context(nc.sbuf_tensor("p5_" + name, list(shape), dt))
        pps = lambda name, shape, dt=F32: st.enter_context(nc.psum_tensor("p5_" + name, list(shape), dt))
        mb = psb("mb", [128, W], BF16)
        tri = psb("tri", [128, 128], BF16)
        trif = psb("trif", [128, 128])
        pre = psb("pre", [128, NTL, NE])
        tot = psb("tot", [128, NTL, NE])
        off = psb("off", [128, NTL, NE])
        cntf = psb("cntf", [128, NE])
        cnti = psb("cnti", [128, NE], I32)
        padf = psb("padf", [128, NE])
        pend = psb("pend", [128, NE])
        pstart = psb("pstart", [128, NE])
        onesr = psb("onesr", [128, NE])
        dest = psb("dest", [128, NTL, NE])
        d8 = psb("d8", [128, 8])
        d4f = psb("d4f", [128, NTL, 4])
        oh = psb("oh", [128, NE])
        bst = psb("bst", [128, NBLK])
        eb = psb("eb", [128, NBLK])
        pk = psb("pk", [128, 8])
        pid = psb("pid", [128, 1])
        idxf = psb("idxf", [128, NBLK, 8])
        tmpf = psb("tmpf", [128, NBLK])
        hrow = [psb(f"hrow{i}", [128, D], BF16) for i in range(3)]
        ps_a = [pps(f"ps_a{i}", [128, 512]) for i in range(2)]

        P.op("pool", lambda e: e.memset(trif[:], 1.0), writes=["trif"])
        P.op("pool", lambda e: e.affine_select(out=trif[:], in_=trif[:], pattern=[[1, 128]], compare_op=ALU.is_gt, fill=0.0,
                                               base=0, channel_multiplier=-1), reads=["trif"], writes=["trif"])
        P.op("dve", lambda e: e.tensor_copy(out=tri[:], in_=trif[:]), reads=["trif"], writes=["tri"])
        P.op("dve", lambda e: e.tensor_copy(out=mb[:], in_=maskS[:].rearrange("p a b -> p (a b)")),
             reads=[("maskS", t) for t in range(NTL)], writes=["mb"])
        pre_f = pre[:].rearrange("p a b -> p (a b)")
        tot_f = tot[:].rearrange("p a b -> p (a b)")
        for c0 in range(0, W, 512):
            w = min(512, W - c0)
            P.op("pe", lambda e, c0=c0, w=w: e.matmul(ps_a[0][:, 0:w], lhsT=tri[:], rhs=mb[:, c0:c0 + w], start=True, stop=True),
                 reads=["tri", "mb"], writes=[("ps_a", 0)])
            P.op("pe", lambda e, c0=c0, w=w: e.matmul(ps_a[1][:, 0:w], lhsT=ones_bf[:], rhs=mb[:, c0:c0 + w], start=True, stop=True),
                 reads=["ones_bf", "mb"], writes=[("ps_a", 1)])
            P.op("dve", lambda e, c0=c0, w=w: e.tensor_copy(out=pre_f[:, c0:c0 + w], in_=ps_a[0][:, 0:w]), reads=[("ps_a", 0)], writes=["pre"])
            P.op("act", lambda e, c0=c0, w=w: e.activation(out=tot_f[:, c0:c0 + w], in_=ps_a[1][:, 0:w], func=AF.Identity),
                 reads=[("ps_a", 1)], writes=["tot"])
        P.op("dve", lambda e: e.tensor_reduce(out=cntf[:], in_=tot[:].rearrange("p a b -> p b a"), axis=AX.X, op=ALU.add),
             reads=["tot"], writes=["cntf"])
        P.op("dve", lambda e: e.tensor_scalar(out=cntf[:], in0=cntf[:], scalar1=float(MB - 1), scalar2=None, op0=ALU.add),
             reads=["cntf"], writes=["cntf"])
        P.op("dve", lambda e: e.tensor_copy(out=cnti[:], in_=cntf[:]), reads=["cntf"], writes=["cnti"])
        P.op("dve", lambda e: e.tensor_scalar(out=cnti[:], in0=cnti[:], scalar1=9, scalar2=9, op0=ALU.arith_shift_right,
                                              op1=ALU.logical_shift_left), reads=["cnti"], writes=["cnti"])
        P.op("dve", lambda e: e.tensor_copy(out=padf[:], in_=cnti[:]), reads=["cnti"], writes=["padf"])
        P.op("dve", lambda e: e.memset(onesr[:], 1.0), writes=["onesr"])
        P.op("dve", lambda e: e.tensor_tensor_scan(out=pend[:], data0=onesr[:], data1=padf[:], initial=0.0, op0=ALU.mult, op1=ALU.add),
             reads=["onesr", "padf"], writes=["pend"])
        P.op("dve", lambda e: e.tensor_tensor(out=pstart[:], in0=pend[:], in1=padf[:], op=ALU.subtract), reads=["pend", "padf"], writes=["pstart"])
        P.op("dve", lambda e: e.tensor_copy(out=off[:, 0, :], in_=pstart[:]), reads=["pstart"], writes=[("off", 0)])
        for t in range(1, NTL):
            P.op("dve", lambda e, t=t: e.tensor_tensor(out=off[:, t, :], in0=off[:, t - 1, :], in1=tot[:, t - 1, :], op=ALU.add),
                 reads=[("off", t - 1), "tot"], writes=[("off", t)])
        dest_f = dest[:].rearrange("p a b -> p (a b)")
        P.op("dve", lambda e: e.tensor_tensor(out=dest_f, in0=off[:].rearrange("p a b -> p (a b)"), in1=pre_f, op=ALU.add),
             reads=[("off", t) for t in range(NTL)] + ["pre"], writes=["dest"])
        P.op("dve", lambda e: e.scalar_tensor_tensor(out=dest_f, in0=dest_f, scalar=1.0, in1=maskS[:].rearrange("p a b -> p (a b)"),
                                                     op0=ALU.add, op1=ALU.mult), reads=["dest"], writes=["dest"])
        P.op("dve", lambda e: e.tensor_scalar(out=dest_f, in0=dest_f, scalar1=-1.0, scalar2=None, op0=ALU.add), reads=["dest"], writes=["dest"])
        for t in range(NTL):
            P.op("dve", lambda e, t=t: e.max(out=d8[:], in_=dest[:, t, :]), reads=["dest"], writes=["d8"])
            P.op("dve", lambda e, t=t: e.tensor_copy(out=d4f[:, t, :], in_=d8[:, 0:4]), reads=["d8"], writes=[("d4f", t)])
            for k in range(4):
                P.op("dve", lambda e, t=t, k=k: e.tensor_scalar(out=oh[:], in0=dest[:, t, :], scalar1=d8[:, k:k + 1], scalar2=None, op0=ALU.is_equal),
                     reads=["dest", "d8"], writes=["oh"])
                P.op("dve", lambda e, t=t: e.tensor_tensor(out=oh[:], in0=oh[:], in1=wS[:, t, :], op=ALU.mult), reads=["oh", ("wS", t)], writes=["oh"])
                P.op("dve", lambda e, t=t, k=k: e.reduce_sum(out=w4[:, t, k:k + 1], in_=oh[:], axis=AX.X), reads=["oh"], writes=[("w4", t, k)])
        P.op("dve", lambda e: e.tensor_copy(out=d4i[:].rearrange("p a b -> p (a b)"), in_=d4f[:].rearrange("p a b -> p (a b)")),
             reads=[("d4f", t) for t in range(NTL)], writes=["d4i"])
        P.op("pool", lambda e: e.iota(bst[:], pattern=[[MB, NBLK]], base=0, channel_multiplier=0, allow_small_or_imprecise_dtypes=True),
             writes=["bst"])
        P.op("pool", lambda e: e.iota(pk[:], pattern=[[128, 8]], base=0, channel_multiplier=1, allow_small_or_imprecise_dtypes=True),
             writes=["pk"])
        P.op("pool", lambda e: e.iota(pid[:], pattern=[[0, 1]], base=0, channel_multiplier=1, allow_small_or_imprecise_dtypes=True),
             writes=["pid"])
        P.op("dve", lambda e: e.memset(eb[:], 0.0), writes=["eb"])
        for ex_ in range(NE):
            P.op("dve", lambda e, ex_=ex_: e.scalar_tensor_tensor(out=eb[:], in0=bst[:], scalar=pend[:, ex_:ex_ + 1], in1=eb[:],
                                                                  op0=ALU.is_ge, op1=ALU.add), reads=["bst", "pend", "eb"], writes=["eb"])
        P.op("dve", lambda e: e.tensor_scalar(out=eb[:], in0=eb[:], scalar1=float(NE - 1), scalar2=None, op0=ALU.min), reads=["eb"], writes=["eb"])
        for k in range(8):
            P.op("dve", lambda e, k=k: e.tensor_scalar(out=idxf[:, :, k], in0=eb[:], scalar1=float(D), scalar2=pk[:, k:k + 1],
                                                      op0=ALU.mult, op1=ALU.add), reads=["eb", "pk"], writes=[("idxf", k)])
        P.op("dve", lambda e: e.tensor_copy(out=idxw[:].rearrange("p a b -> p (a b)"), in_=idxf[:].rearrange("p a b -> p (a b)")),
             reads=[("idxf", k) for k in range(8)], writes=["idxw"])
        P.op("dve", lambda e: e.tensor_scalar(out=tmpf[:], in0=eb[:], scalar1=128.0, scalar2=pid[:, 0:1], op0=ALU.mult, op1=ALU.add),
             reads=["eb", "pid"], writes=["tmpf"])
        P.op("dve", lambda e: e.tensor_copy(out=idxb1[:], in_=tmpf[:]), reads=["tmpf"], writes=["idxb1"])
        P.op("dve", lambda e: e.tensor_copy(out=idxb2[:], in_=eb[:]), reads=["eb"], writes=["idxb2"])
        for t in range(NTL):
            hr = hrow[t % 3]
            P.dma("sp", lambda e, hr=hr, t=t: e.dma_start(out=hr[:], in_=H2[t * 128:(t + 1) * 128, :]), reads=[("H2", t * 128)], writes=[("hrow", t % 3)])
            for k in range(4):
                P.dma("pool", lambda e, hr=hr, t=t, k=k: e.indirect_dma_start(
                    out=XS, out_offset=bass.IndirectOffsetOnAxis(ap=d4i[:, t, k:k + 1], axis=0), in_=hr[:], in_offset=None),
                    reads=[("hrow", t % 3), "d4i"], writes=[("XSs", t, k)])
        P.end_phase()


def phase6(nc, P, cfg, G):
    NE, NBLK = cfg.NE, cfg.NBLK
    XS, YS = G["XS"], G["YS"]
    w1, w2, b1T, b2 = G["w1"], G["w2"], G["b1T"], G["b2"]
    idxw, idxb1, idxb2, ident_bf = G["idxw"], G["idxb1"], G["idxb2"], G["ident_bf"]
    with ExitStack() as st:
        psb = lambda name, shape, dt=F32: st.enter_context(nc.sbuf_tensor("p6_" + name, list(shape), dt))
        pps = lambda name, shape, dt=F32: st.enter_context(nc.psum_tensor("p6_" + name, list(shape), dt))
        w1s = [psb(f"w1s{i}", [128, 8, 2 * DFF], BF16) for i in range(2)]
        w2s = [psb(f"w2s{i}", [128, 8, D], BF16) for i in range(2)]
        b1s = [psb(f"b1s{i}", [128, 16]) for i in range(2)]
        b2s = [psb(f"b2s{i}", [128, D]) for i in range(2)]
        xs = [psb(f"xs{i}", [128, 4, D], BF16) for i in range(2)]
        xsT = psb("xsT", [128, 8, 512], BF16)
        actT = psb("actT", [128, 8, 512], BF16)
        glu = [psb(f"glu{i}", [128, 512]) for i in range(2)]
        sg = [psb(f"sg{i}", [128, 512]) for i in range(2)]
        lin = [psb(f"lin{i}", [128, 512]) for i in range(2)]
        yo = [psb(f"yo{i}", [128, D]) for i in range(2)]
        ps_x = [pps(f"ps_x{i}", [128, 512], BF16) for i in range(2)]
        ps_g = [pps(f"ps_g{i}", [128, 512]) for i in range(2)]
        ps_l = [pps(f"ps_l{i}", [128, 512]) for i in range(2)]
        ps_y = [pps(f"ps_y{i}", [128, 512]) for i in range(2)]
        ji = 0
        yi = 0
        px = 0
        def load_blk(b):
            w1_, w2_, b1_, b2_, xs_ = w1s[b % 2], w2s[b % 2], b1s[b % 2], b2s[b % 2], xs[b % 2]
            wk1, wk2, bk1, bk2, xk = ("w1s", b % 2), ("w2s", b % 2), ("b1s", b % 2), ("b2s", b % 2), ("xs", b % 2)
            P.dma("sp", lambda e, xs_=xs_, b=b: e.dma_start(out=xs_[:], in_=XS[b * MB:(b + 1) * MB, :].rearrange("(t p) d -> p t d", p=128)),
                  reads=["XS"], writes=[xk])
            for k in range(8):
                P.dma("pool", lambda e, w1_=w1_, b=b, k=k: e.indirect_dma_start(
                    out=w1_[:, k, :], out_offset=None, in_=w1.ap(), in_offset=bass.IndirectOffsetOnAxis(ap=idxw[:, b, k:k + 1], axis=0)),
                    reads=["idxw"], writes=[(wk1, k)])
            for k in range(8):
                P.dma("pool", lambda e, w2_=w2_, b=b, k=k: e.indirect_dma_start(
                    out=w2_[:, k, :], out_offset=None, in_=w2.ap(), in_offset=bass.IndirectOffsetOnAxis(ap=idxw[:, b, k:k + 1], axis=0)),
                    reads=["idxw"], writes=[(wk2, k)])
            P.dma("pool", lambda e, b1_=b1_, b=b: e.indirect_dma_start(
                out=b1_[:], out_offset=None, in_=b1T.ap(), in_offset=bass.IndirectOffsetOnAxis(ap=idxb1[:, b:b + 1], axis=0)),
                reads=["idxb1"], writes=[bk1])
            P.dma("pool", lambda e, b2_=b2_, b=b: e.indirect_dma_start(
                out=b2_[:], out_offset=None, in_=b2.ap(), in_offset=bass.IndirectOffsetOnAxis(ap=idxb2[:, b:b + 1], axis=0)),
                reads=["idxb2"], writes=[bk2])

        load_blk(0)
        for b in range(NBLK):
            if b + 1 < NBLK:
                load_blk(b + 1)
            w1_, w2_, b1_, b2_, xs_ = w1s[b % 2], w2s[b % 2], b1s[b % 2], b2s[b % 2], xs[b % 2]
            wk1, wk2, bk1, bk2, xk = ("w1s", b % 2), ("w2s", b % 2), ("b1s", b % 2), ("b2s", b % 2), ("xs", b % 2)
            P.op("dve", lambda e, b1_=b1_: e.tensor_scalar(out=b1_[:, 8:16], in0=b1_[:, 8:16], scalar1=1.0, scalar2=None, op0=ALU.add),
                 reads=[bk1], writes=[bk1])
            for k in range(8):
                pxs = ps_x[px % 2]
                pxk = ("ps_x", px % 2)
                px += 1
                for t in range(4):
                    P.op("pe", lambda e, pxs=pxs, t=t, k=k, xs_=xs_: e.transpose(out=pxs[:, t * 128:(t + 1) * 128], in_=xs_[:, t, k * 128:(k + 1) * 128],
                                                                                identity=ident_bf[:]),
                         reads=[xk, "ident_bf"], writes=[pxk], accum=True)
                P.op("act" if k % 2 == 0 else "dve",
                     (lambda e, pxs=pxs, k=k: e.activation(out=xsT[:, k, :], in_=pxs[:], func=AF.Identity)) if k % 2 == 0 else
                     (lambda e, pxs=pxs, k=k: e.tensor_copy(out=xsT[:, k, :], in_=pxs[:])),
                     reads=[pxk], writes=[("xsT", k)])
            for j in range(8):
                pg, pl = ps_g[ji % 2], ps_l[ji % 2]
                pgk, plk = ("ps_g", ji % 2), ("ps_l", ji % 2)
                gl_, sg_, ln_ = glu[ji % 2], sg[ji % 2], lin[ji % 2]
                glk, sgk, lnk = ("glu", ji % 2), ("sg", ji % 2), ("lin", ji % 2)
                ji += 1
                for k in range(8):
                    P.op("pe", lambda e, pg=pg, k=k, j=j, w1_=w1_: e.matmul(pg[:], lhsT=w1_[:, k, j * 128:(j + 1) * 128], rhs=xsT[:, k, :],
                                                                          start=(k == 0), stop=(k == 7)),
                         reads=[(wk1, k), ("xsT", k)], writes=[pgk], accum=True)
                for k in range(8):
                    P.op("pe", lambda e, pl=pl, k=k, j=j, w1_=w1_: e.matmul(pl[:], lhsT=w1_[:, k, DFF + j * 128:DFF + (j + 1) * 128], rhs=xsT[:, k, :],
                                                                          start=(k == 0), stop=(k == 7)),
                         reads=[(wk1, k), ("xsT", k)], writes=[plk], accum=True)
                P.op("dve", lambda e, gl_=gl_, pg=pg, b1_=b1_, j=j: e.tensor_scalar(out=gl_[:], in0=pg[:], scalar1=b1_[:, j:j + 1], scalar2=LIMIT,
                                                                               op0=ALU.add, op1=ALU.min), reads=[pgk, bk1], writes=[glk])
                P.op("act", lambda e, sg_=sg_, gl_=gl_: e.activation(out=sg_[:], in_=gl_[:], func=AF.Sigmoid, scale=ALPHA), reads=[glk], writes=[sgk])
                P.op("dve", lambda e, ln_=ln_, pl=pl, b1_=b1_, j=j: e.tensor_scalar(out=ln_[:], in0=pl[:], scalar1=b1_[:, 8 + j:9 + j], scalar2=LIMIT + 1.0,
                                                                               op0=ALU.add, op1=ALU.min), reads=[plk, bk1], writes=[lnk])
                P.op("pool", lambda e, gl_=gl_, sg_=sg_: e.tensor_tensor(out=gl_[:], in0=gl_[:], in1=sg_[:], op=ALU.mult), reads=[glk, sgk], writes=[glk])
                P.op("dve", lambda e, gl_=gl_, ln_=ln_, j=j: e.scalar_tensor_tensor(out=actT[:, j, :], in0=ln_[:], scalar=1.0 - LIMIT, in1=gl_[:],
                                                                                  op0=ALU.max, op1=ALU.mult),
                     reads=[glk, lnk], writes=[("actT", j)])
            for t in range(4):
                yo_ = yo[yi % 2]
                yok = ("yo", yi % 2)
                yi += 1
                for n in range(2):
                    py = ps_y[n]
                    for j in range(8):
                        P.op("pe", lambda e, py=py, j=j, t=t, n=n, w2_=w2_: e.matmul(py[:], lhsT=actT[:, j, t * 128:(t + 1) * 128],
                                                                                   rhs=w2_[:, j, n * 512:(n + 1) * 512], start=(j == 0), stop=(j == 7)),
                             reads=[("actT", j), (wk2, j)], writes=[("ps_y", n)], accum=True)
                    P.op("dve", lambda e, py=py, yo_=yo_, n=n, b2_=b2_: e.tensor_tensor(out=yo_[:, n * 512:(n + 1) * 512], in0=py[:],
                                                                                       in1=b2_[:, n * 512:(n + 1) * 512], op=ALU.add),
                         reads=[("ps_y", n), bk2], writes=[yok])
                r0 = b * MB + t * 128
                P.dma("sp", lambda e, yo_=yo_, r0=r0: e.dma_start(out=YS[r0:r0 + 128, :], in_=yo_[:]), reads=[yok], writes=[("YS", r0)])
        P.end_phase()


def phase7(nc, P, cfg, G):
    SA, H, NT = cfg.SA, cfg.H, cfg.NT
    NTL = NT // 128
    X1, YS, BC, y_seg = G["X1"], G["YS"], G["BC"], G["y_seg"]
    d4i, w4 = G["d4i"], G["w4"]
    alpha = float(2.0 ** 0.25)
    with ExitStack() as st:
        psb = lambda name, shape, dt=F32: st.enter_context(nc.sbuf_tensor("p7_" + name, list(shape), dt))
        pps = lambda name, shape, dt=F32: st.enter_context(nc.psum_tensor("p7_" + name, list(shape), dt))
        yg = [psb(f"yg{i}", [128, 4, D]) for i in range(2)]
        x1t = [psb(f"x1t{i}", [128, D]) for i in range(2)]
        acc = [psb(f"acc{i}", [128, D]) for i in range(2)]
        bcs = psb("bcs", [128, D])
        st6 = psb("st6", [128, 2, 6])
        mv = psb("mv", [128, 2])
        sd = psb("sd", [128, 1])
        rstd = psb("rstd", [128, 1])
        ps_t = pps("ps_t", [128, 512])
        g2b = _bcast_row(nc, P, G, psb, ps_t, "ln2g", G["ln2_g"].ap(), D)
        b2b = _bcast_row(nc, P, G, psb, ps_t, "ln2b", G["ln2_b"].ap(), D)
        for t in range(NTL):
            r0 = t * 128
            seg, sidx, lr = ("A", 0, r0) if r0 < SA else ("B", 1, r0 - SA)
            if r0 == 0 or r0 == SA:
                P.dma("sp", lambda e, sidx=sidx: e.dma_start(out=bcs[:], in_=BC[:, sidx, 3, :]), reads=["BC"], writes=["bcs"])
            yg_, x1_, ac_ = yg[t % 2], x1t[t % 2], acc[t % 2]
            ygk, x1k, ack = ("yg", t % 2), ("x1t", t % 2), ("acc", t % 2)
            P.dma("sp", lambda e, x1_=x1_, r0=r0: e.dma_start(out=x1_[:], in_=X1[r0:r0 + 128, :]), reads=[("X1", r0)], writes=[x1k])
            for k in range(4):
                P.dma("pool", lambda e, yg_=yg_, t=t, k=k: e.indirect_dma_start(
                    out=yg_[:, k, :], out_offset=None, in_=YS, in_offset=bass.IndirectOffsetOnAxis(ap=d4i[:, t, k:k + 1], axis=0)),
                    reads=["d4i", "YS"], writes=[(ygk, k)])
            P.op("dve", lambda e, ac_=ac_, yg_=yg_, t=t: e.tensor_scalar(out=ac_[:], in0=yg_[:, 0, :], scalar1=w4[:, t, 0:1], scalar2=None, op0=ALU.mult),
                 reads=[(ygk, 0), ("w4", t, 0)], writes=[ack])
            for k in range(1, 4):
                P.op("dve", lambda e, ac_=ac_, yg_=yg_, t=t, k=k: e.scalar_tensor_tensor(
                    out=ac_[:], in0=yg_[:, k, :], scalar=w4[:, t, k:k + 1], in1=ac_[:], op0=ALU.mult, op1=ALU.add),
                    reads=[(ygk, k), ("w4", t, k), ack], writes=[ack])
            P.op("dve", lambda e, ac_=ac_: e.tensor_tensor(out=ac_[:], in0=ac_[:], in1=bcs[:], op=ALU.mult), reads=[ack, "bcs"], writes=[ack])
            P.op("dve", lambda e, ac_=ac_, x1_=x1_: e.scalar_tensor_tensor(out=ac_[:], in0=x1_[:], scalar=alpha, in1=ac_[:], op0=ALU.mult, op1=ALU.add),
                 reads=[x1k, ack], writes=[ack])
            _ln_stats(P, "ln2", ac_, st6, mv, sd, rstd, ack)
            P.op("dve", lambda e, ac_=ac_: e.scalar_tensor_tensor(out=ac_[:], in0=ac_[:], scalar=mv[:, 0:1], in1=g2b[:],
                                                                  op0=ALU.subtract, op1=ALU.mult), reads=[ack, ("ln2", "mv"), "ln2g"], writes=[ack])
            P.op("dve", lambda e, ac_=ac_: e.scalar_tensor_tensor(out=ac_[:], in0=ac_[:], scalar=rstd[:, 0:1], in1=b2b[:],
                                                                  op0=ALU.mult, op1=ALU.add), reads=[ack, ("ln2", "rstd"), "ln2b"], writes=[ack])
            P.dma("sp", lambda e, ac_=ac_, seg=seg, lr=lr: e.dma_start(out=y_seg[seg][lr:lr + 128, :], in_=ac_[:]), reads=[ack], writes=[("y", t)])
        P.end_phase()


def rope_tables_T(pos):
    pos = np.asarray(pos)
    rows = (pos // GRID_W).astype(np.float32)
    cols = (pos % GRID_W).astype(np.float32)
    axis_dim = HD // 2
    inv = (np.float32(ROPE_THETA) ** (-np.arange(0, axis_dim, 2, dtype=np.float32) / np.float32(axis_dim))).astype(np.float32)
    p = np.arange(128)
    j = p % 32
    comp = np.where((p // 64)[:, None] == 0, rows[None, :], cols[None, :]).astype(np.float32)
    ang = (comp * inv[j][:, None]).astype(np.float32)
    sgn = np.where((p % 64) < 32, -1.0, 1.0).astype(np.float32)[:, None]
    return np.cos(ang).astype(np.float32), (np.sin(ang) * sgn).astype(np.float32)


def make_in_maps(cfg, inputs):
    SA, H, NE = cfg.SA, cfg.H, cfg.NE
    f = lambda a: np.ascontiguousarray(np.asarray(a, dtype=np.float32))
    xp, xs = f(inputs["x_prompt"]), f(inputs["x_sample"])
    cp, cs = f(inputs["c_prompt"]), f(inputs["c_sample"])
    ident = np.eye(128, dtype=np.float32)
    perm = np.zeros((128, 128), np.float32)
    for m in range(128):
        partner = m + 32 if (m % 64) < 32 else m - 32
        perm[partner, m] = 1.0
    shared = {
        "ident": ident, "perm": perm,
        "w_ada": f(inputs["w_ada"][0]),
        "b_adaT": f(inputs["b_ada"][0].reshape(48, 128).T),
        "b_ada_row": f(inputs["b_ada"][0].reshape(1, -1)),
        "w_in": f(inputs["w_in"][0]),
        "qk_gain": f(np.stack([inputs["q_gain"][0], inputs["k_gain"][0]], axis=1)),
        "conv_wT": f(inputs["conv_w"][0].reshape(4, 8, 128).transpose(2, 1, 0)),
        "conv_bT": f(inputs["conv_b"][0].reshape(8, 128).T),
        "lru_wa": f(inputs["lru_wa"][0]), "lru_wx": f(inputs["lru_wx"][0]),
        "lru_baT": f(inputs["lru_ba"][0].reshape(2, 8, 128).transpose(2, 0, 1)),
        "lru_bxT": f(inputs["lru_bx"][0].reshape(2, 8, 128).transpose(2, 0, 1)),
        "lru_lamT": f(inputs["lru_lam"][0].reshape(2, 8, 128).transpose(2, 0, 1)),
        "w_pa": f(inputs["w_pa"][0]), "w_pr": f(inputs["w_pr"][0]), "w_out": f(inputs["w_out"][0]),
        "ln1_g": f(inputs["ln1_g"][0].reshape(1, -1)), "ln1_b": f(inputs["ln1_b"][0].reshape(1, -1)),
        "w_router": f(inputs["w_router"][0]), "b_router": f(inputs["b_router"][0].reshape(1, -1)),
        "w1": f(inputs["w1"][0].reshape(NE * D, 2 * DFF)),
        "b1T": f(inputs["b1"][0].reshape(NE, 16, 128).transpose(0, 2, 1).reshape(NE * 128, 16)),
        "w2": f(inputs["w2"][0].reshape(NE * DFF, D)),
        "b2": f(inputs["b2"][0]),
        "ln2_g": f(inputs["ln2_g"][0].reshape(1, -1)), "ln2_b": f(inputs["ln2_b"][0].reshape(1, -1)),
    }
    cosA, sinA = rope_tables_T(np.arange(SA))
    maps = []
    for c in range(8):
        sq, half = c // 2, c % 2
        own = np.arange(half * H, (half + 1) * H)
        oth = np.arange((1 - half) * H, (2 - half) * H)
        cosB, sinB = rope_tables_T(own)
        cosC, sinC = rope_tables_T(oth)
        cpair = np.stack([cp[c], cs[sq]], axis=0)
        cT = np.ascontiguousarray(cpair.reshape(2, 8, 128).transpose(2, 1, 0).reshape(128, 16))
        fl = np.zeros((128, 2), np.float32)
        fl[:, 0] = 1.0 if half == 1 else 0.0
        fl[:, 1] = 1.0 if half == 0 else 0.0
        m = dict(shared)
        m.update({
            "xa": f(xp[c]), "xb": f(xs[sq, own]), "xc": f(xs[sq, oth]),
            "cT": cT, "cosA": cosA, "sinA": sinA, "cosB": cosB, "sinB": sinB, "cosC": cosC, "sinC": sinC,
            "flags": fl,
        })
        maps.append(m)
    return maps


_NC_CACHE = {}


def run(cfg, inputs):
    key = (cfg.SA, cfg.H, cfg.NE, cfg.dbg)
    if key not in _NC_CACHE:
        _NC_CACHE[key] = build_program(cfg)
    nc = _NC_CACHE[key]
    maps = make_in_maps(cfg, inputs)
    used = set()
    for alloc in nc.allocations:
        try:
            if alloc.kind == "ExternalInput":
                used.add(alloc.memorylocations[0].name)
        except Exception:
            pass
    if used:
        maps = [{k: v for k, v in m.items() if k in used} for m in maps]
    import time as _t
    _t0 = _t.time()
    res = run_bass_kernel_spmd(nc, maps, core_ids=list(range(8)))
    print("[kernel] device run+transfer %.1fs, input MB/core %.1f" % (_t.time() - _t0, sum(v.nbytes for v in maps[0].values()) / 1e6))
    return res.results


def kernel(**inputs):
    cfg = Cfg()
    results = run(cfg, inputs)
    SA, H = cfg.SA, cfg.H
    yp = np.stack([np.asarray(results[c]["ya"], dtype=np.float32) for c in range(8)], axis=0)
    ys = np.zeros((4, 2 * H, D), np.float32)
    for c in range(8):
        ys[c // 2, (c % 2) * H:(c % 2 + 1) * H] = np.asarray(results[c]["yb"], dtype=np.float32)
    return (yp, ys)
```

```python
import numpy as np
import ml_dtypes
from contextlib import ExitStack
import concourse.bass as bass
import concourse.mybir as mybir
from concourse.bass_utils import run_bass_kernel_spmd

F32 = mybir.dt.float32
BF16 = mybir.dt.bfloat16
I32 = mybir.dt.int32
U32 = mybir.dt.uint32
AF = mybir.ActivationFunctionType
ALU = mybir.AluOpType
AX = mybir.AxisListType

D = 1024
NH = 8
NKV = 2
HD = 128
GRID_W = 64
ROPE_THETA = 10000.0
LRU_C = 8.0
TOPK = 4
DFF = 1024
LIMIT = 7.0
ALPHA = 1.702
LN_EPS = 1e-5
RMS_EPS = 1e-6
MB = 512


class Prog:
    COMPUTE = ("pe", "act", "dve", "pool")
    STREAMS = ("pe", "act", "dve", "pool", "sp")

    def __init__(self, nc, stack, ring_sizes=None):
        self.nc = nc
        ring_sizes = ring_sizes or {"sp": 20, "pool": 20, "act": 6}
        self.owners = list(self.COMPUTE)
        self.rings = {}
        for q, n in ring_sizes.items():
            self.rings[q] = []
            for i in range(n):
                self.rings[q].append(len(self.owners))
                self.owners.append(f"d_{q}{i}")
        self.nown = len(self.owners)
        self.sems = [stack.enter_context(nc.semaphore(f"s_{nm}")) for nm in self.owners]
        self.count = [0] * self.nown
        self.clock = {s: [0] * self.nown for s in self.STREAMS}
        self.vcs = [[None] for _ in range(self.nown)]
        self.rr = {q: 0 for q in self.rings}
        self.buf = {}
        self.sigbase = [0] * 4
        self.phase_start = [1] * 4
        self.reset_phase()

    def reset_phase(self):
        self.stream = {s: [] for s in self.STREAMS}
        self.signal = [set() for _ in range(4)]
        for o in range(4):
            self.phase_start[o] = self.count[o] + 1

    def _deps(self, reads, writes, accum_owner=None):
        deps = []
        for k in reads:
            st = self.buf.get(k)
            if st is not None and st[0] is not None:
                deps.append(st[0])
        for k in writes:
            st = self.buf.get(k)
            if st is not None:
                if st[0] is not None and not (accum_owner is not None and st[0][0] == accum_owner and not st[1]):
                    deps.append(st[0])
                for o, n in st[1].items():
                    deps.append((o, n))
        return deps

    def _sync(self, stream, deps):
        clk = self.clock[stream]
        for (o, n) in deps:
            if clk[o] < n:
                self.stream[stream].append(("wait", o, n))
                if o < 4:
                    assert n >= self.phase_start[o], "cross-phase dependency without barrier"
                    self.signal[o].add(n)
                vc = self.vcs[o][n]
                for i in range(self.nown):
                    if vc[i] > clk[i]:
                        clk[i] = vc[i]

    def _record(self, ev, reads, writes):
        for k in reads:
            st = self.buf.get(k)
            if st is None:
                st = self.buf[k] = [None, {}]
            st[1][ev[0]] = ev[1]
        for k in writes:
            self.buf[k] = [ev, {}]

    def op(self, stream, fn, reads=(), writes=(), accum=False):
        o = self.COMPUTE.index(stream)
        self._sync(stream, self._deps(reads, writes, o if accum else None))
        self.count[o] += 1
        n = self.count[o]
        vc = list(self.clock[stream])
        vc[o] = n
        self.vcs[o].append(vc)
        self.stream[stream].append(("op", fn, o, n))
        self._record((o, n), reads, writes)
        return (o, n)

    def dma(self, queue, fn, reads=(), writes=()):
        ring = self.rings[queue]
        o = ring[self.rr[queue] % len(ring)]
        self.rr[queue] += 1
        deps = self._deps(reads, writes)
        prev = self.count[o]
        if prev > 0:
            deps.append((o, prev))
        self._sync(queue, deps)
        self.count[o] = prev + 1
        n = prev + 1
        vc = list(self.clock[queue])
        vc[o] = n
        self.vcs[o].append(vc)
        self.stream[queue].append(("dma", fn, o, n))
        self._record((o, n), reads, writes)
        return (o, n)

    def barrier(self):
        for s in self.STREAMS:
            deps = [(o, self.count[o]) for o in range(self.nown) if self.count[o] > 0]
            self._sync(s, deps)

    def emit(self):
        nc = self.nc
        sig_sorted = [sorted(s) for s in self.signal]
        rank = [{n: self.sigbase[o] + i + 1 for i, n in enumerate(sig_sorted[o])} for o in range(4)]

        def val(o, n):
            if o < 4:
                return rank[o][n]
            return 16 * n

        def run(eng, ents):
            for ent in ents:
                if ent[0] == "wait":
                    eng.wait_ge(self.sems[ent[1]], val(ent[1], ent[2]))
                elif ent[0] == "op":
                    ins = ent[1](eng)
                    if ent[3] in self.signal[ent[2]]:
                        ins.then_inc(self.sems[ent[2]], 1)
                else:
                    ins = ent[1](eng)
                    ins.then_inc(self.sems[ent[2]], 16)

        with nc.Block() as block:
            @block.tensor
            def _(e):
                run(e, self.stream["pe"])

            @block.scalar
            def _(e):
                run(e, self.stream["act"])

            @block.vector
            def _(e):
                run(e, self.stream["dve"])

            @block.gpsimd
            def _(e):
                run(e, self.stream["pool"])

            @block.sync
            def _(e):
                run(e, self.stream["sp"])

        for o in range(4):
            self.sigbase[o] += len(sig_sorted[o])
        self.reset_phase()

    def end_phase(self):
        self.barrier()
        self.emit()


class Cfg:
    def __init__(self, SA=4096, H=4096, NE=32, dbg=()):
        self.SA = SA
        self.H = H
        self.NE = NE
        self.NT = SA + H
        self.NBLK = self.NT * TOPK // MB + NE
        self.CAP = self.NBLK * MB
        self.dbg = tuple(dbg)


W_IN_COLS = 5632


def build_program(cfg):
    nc = bass.Bass("TRN2", target_bir_lowering=False)
    SA, H, NE, NT = cfg.SA, cfg.H, cfg.NE, cfg.NT
    dbg = cfg.dbg

    class _LazyIn:
        def __init__(self, name, shape, dt):
            self.name, self.shape, self.dt, self._ap = name, list(shape), dt, None

        def ap(self):
            if self._ap is None:
                self._ap = nc.dram_tensor(self.name, self.shape, self.dt, kind="ExternalInput").ap()
            return self._ap

        def __getitem__(self, idx):
            return self.ap()[idx]

        def rearrange(self, *a, **k):
            return self.ap().rearrange(*a, **k)

    def din(name, shape, dt=F32):
        return _LazyIn(name, shape, dt)

    def dout(name, shape, dt=F32):
        return nc.dram_tensor(name, list(shape), dt, kind="ExternalOutput").ap()

    def dscr(name, shape, dt=F32):
        kind = "ExternalOutput" if name in dbg else "Internal"
        return nc.dram_tensor(name, list(shape), dt, kind=kind).ap()

    x_seg = {"A": din("xa", [SA, D]), "B": din("xb", [H, D]), "C": din("xc", [H, D])}
    cT = din("cT", [128, 16])
    rope_cos = {"A": din("cosA", [128, SA]), "B": din("cosB", [128, H]), "C": din("cosC", [128, H])}
    rope_sin = {"A": din("sinA", [128, SA]), "B": din("sinB", [128, H]), "C": din("sinC", [128, H])}
    flags = din("flags", [128, 2])
    ident_in = din("ident", [128, 128])
    perm_in = din("perm", [128, 128])
    w_ada = din("w_ada", [D, 6 * D])
    b_adaT = din("b_adaT", [128, 48])
    b_ada_row = din("b_ada_row", [1, 6 * D])
    w_in = din("w_in", [D, W_IN_COLS])
    qk_gain = din("qk_gain", [128, 2])
    conv_wT = din("conv_wT", [128, 8, 4])
    conv_bT = din("conv_bT", [128, 8])
    lru_wa = din("lru_wa", [2, 8, 128, 128])
    lru_wx = din("lru_wx", [2, 8, 128, 128])
    lru_baT = din("lru_baT", [128, 2, 8])
    lru_bxT = din("lru_bxT", [128, 2, 8])
    lru_lamT = din("lru_lamT", [128, 2, 8])
    w_pa = din("w_pa", [D, D])
    w_pr = din("w_pr", [D, D])
    w_out = din("w_out", [D, D])
    ln1_g = din("ln1_g", [1, D])
    ln1_b = din("ln1_b", [1, D])
    w_router = din("w_router", [D, NE])
    b_router = din("b_router", [1, NE])
    w1 = din("w1", [NE * D, 2 * DFF])
    b1T = din("b1T", [NE * 128, 16])
    w2 = din("w2", [NE * DFF, D])
    b2 = din("b2", [NE, D])
    ln2_g = din("ln2_g", [1, D])
    ln2_b = din("ln2_b", [1, D])

    y_seg = {"A": dout("ya", [SA, D]), "B": dout("yb", [H, D])}

    seglen = {"A": SA, "B": H, "C": H}
    QT = {s: dscr(f"QT{s}", [NH, 128, seglen[s]], BF16) for s in "AB"}
    KT = {"A": dscr("KTA", [NKV, 128, SA], BF16), "S": dscr("KTS", [NKV, 128, 2 * H], BF16)}
    VV = {"A": dscr("VA", [SA, 256], BF16), "S": dscr("VS", [2 * H, 256], BF16)}
    XR = {s: dscr(f"XR{s}", [8, 128, seglen[s]], F32) for s in "ABC"}
    GG = {s: dscr(f"GG{s}", [8, 128, seglen[s]], F32) for s in "AB"}
    GL = {s: dscr(f"GL{s}", [16, 128, seglen[s]], F32) for s in "AB"}
    ATT = {s: dscr(f"ATT{s}", [NH, 128, seglen[s]], BF16) for s in "AB"}
    REC = {s: dscr(f"REC{s}", [8, 128, seglen[s]], BF16) for s in "AB"}

    with ExitStack() as gstack:
        P = Prog(nc, gstack)
        sb = lambda name, shape, dt=F32: gstack.enter_context(nc.sbuf_tensor("g_" + name, list(shape), dt))

        ident = sb("ident", [128, 128])
        ident_bf = sb("ident_bf", [128, 128], BF16)
        perm_bf = sb("perm_bf", [128, 128], BF16)
        ones_bf = sb("ones_bf", [128, 128], BF16)
        ones_f = sb("ones_f", [128, 128])
        flags_sb = sb("flags_sb", [128, 2])
        sh1T = sb("sh1T", [128, 2, 8])
        sc1T = sb("sc1T", [128, 2, 8])
        BC = dscr("BC", [128, 2, 4, D])
        gain_sb = sb("gain_sb", [128, 2])
        negM = sb("negM", [128, 1])

        with ExitStack() as st:
            psb = lambda name, shape, dt=F32: st.enter_context(nc.sbuf_tensor("p0_" + name, list(shape), dt))
            pps = lambda name, shape, dt=F32: st.enter_context(nc.psum_tensor("p0_" + name, list(shape), dt))
            bc = psb("bc", [128, 2, 4, D])
            c_sb = psb("c_sb", [128, 16])
            sg_sb = psb("sg_sb", [128, 16])
            sc_sb = psb("sc_sb", [128, 16])
            screp = psb("screp", [128, 2, 8, 128])
            perm_f = psb("perm_f", [128, 128])
            badaT_sb = psb("badaT_sb", [128, 48])
            bada_row_sb = psb("bada_row_sb", [1, 6 * D])
            wad = [psb(f"wad{i}", [128, 8, D]) for i in range(2)]
            gq = psb("gq", [1, 2, 128])
            gmax = psb("gmax", [1, 2])
            gprod = psb("gprod", [1, 1])
            ps_m = pps("ps_m", [128, 32])
            ps_b = [pps(f"ps_b{i}", [128, 512]) for i in range(2)]
            ps_g = pps("ps_g", [128, 1])

            P.dma("sp", lambda e: e.dma_start(out=ident[:], in_=ident_in.ap()), writes=["ident"])
            P.dma("sp", lambda e: e.dma_start(out=perm_f[:], in_=perm_in.ap()), writes=["perm_f"])
            P.dma("sp", lambda e: e.dma_start(out=c_sb[:], in_=cT.ap()), writes=["c_sb"])
            P.dma("sp", lambda e: e.dma_start(out=flags_sb[:], in_=flags.ap()), writes=["flags"])
            P.dma("sp", lambda e: e.dma_start(out=badaT_sb[:], in_=b_adaT.ap()), writes=["badaT"])
            P.dma("sp", lambda e: e.dma_start(out=bada_row_sb[:], in_=b_ada_row.ap()), writes=["bada_row"])
            P.dma("sp", lambda e: e.dma_start(out=gain_sb[:], in_=qk_gain.ap()), writes=["gain"])
            P.dma("sp", lambda e: e.dma_start(out=gq[:], in_=qk_gain.rearrange("p (o g) -> o g p", o=1),
                                              allow_slow_non_contiguous=True), writes=["gq"])
            P.op("dve", lambda e: e.memset(ones_f[:], 1.0), writes=["ones_f"])
            P.op("dve", lambda e: e.memset(ones_bf[:], 1.0), writes=["ones_bf"])
            P.op("dve", lambda e: e.tensor_copy(out=ident_bf[:], in_=ident[:]), reads=["ident"], writes=["ident_bf"])
            P.op("dve", lambda e: e.tensor_copy(out=perm_bf[:], in_=perm_f[:]), reads=["perm_f"], writes=["perm_bf"])
            P.op("dve", lambda e: e.tensor_reduce(out=gmax[:], in_=gq[:], axis=AX.X, op=ALU.max,
                                                  apply_absolute_value=True), reads=["gq"], writes=["gmax"])
            P.op("dve", lambda e: e.scalar_tensor_tensor(out=gprod[:], in0=gmax[:, 0:1], scalar=-float(np.sqrt(128.0)),
                                                         in1=gmax[:, 1:2], op0=ALU.mult, op1=ALU.mult),
                 reads=["gmax"], writes=["gprod"])
            P.op("pe", lambda e: e.matmul(ps_g[:], lhsT=ones_f[0:1, :], rhs=gprod[:], start=True, stop=True),
                 reads=["ones_f", "gprod"], writes=["ps_g"])
            P.op("dve", lambda e: e.tensor_copy(out=negM[:], in_=ps_g[:]), reads=["ps_g"], writes=["negM"])
            P.op("act", lambda e: e.activation(out=sg_sb[:], in_=c_sb[:], func=AF.Sigmoid), reads=["c_sb"], writes=["sg"])
            P.op("dve", lambda e: e.tensor_tensor(out=sc_sb[:], in0=c_sb[:], in1=sg_sb[:], op=ALU.mult),
                 reads=["c_sb", "sg"], writes=["sc"])
            for s in range(2):
                for i in range(8):
                    P.op("dve", lambda e, s=s, i=i: e.tensor_scalar(out=screp[:, s, i, :], in0=ones_f[:],
                                                                   scalar1=sc_sb[:, i * 2 + s:i * 2 + s + 1], scalar2=None,
                                                                   op0=ALU.mult),
                         reads=["ones_f", "sc"], writes=[("screp", s, i)])
            for j in range(6):
                wt = wad[j % 2]
                wk = ("wad", j % 2)
                for half in range(2):
                    P.dma("sp", lambda e, wt=wt, j=j, half=half: e.dma_start(
                        out=wt[:, half * 4:(half + 1) * 4, :],
                        in_=w_ada[half * 512:(half + 1) * 512, j * D:(j + 1) * D].rearrange("(i p) n -> p i n", p=128)),
                        writes=[(wk, half)])
                if j < 2:
                    for k in range(8):
                        for i in range(8):
                            P.op("pe", lambda e, wt=wt, j=j, k=k, i=i: e.matmul(
                                ps_m[:, (j * 8 + k) * 2:(j * 8 + k) * 2 + 2], lhsT=wt[:, i, k * 128:(k + 1) * 128],
                                rhs=sc_sb[:, i * 2:i * 2 + 2], start=(i == 0), stop=(i == 7)),
                                reads=[(wk, i // 4), "sc"], writes=["ps_m"], accum=True)
                    if j == 1:
                        for s in range(2):
                            P.op("dve", lambda e, s=s: e.tensor_tensor(
                                out=sh1T[:, s, :], in0=ps_m[:, s:16:2], in1=badaT_sb[:, 0:8], op=ALU.add),
                                reads=["ps_m", "badaT"], writes=[("sh1T", s)])
                            P.op("dve", lambda e, s=s: e.scalar_tensor_tensor(
                                out=sc1T[:, s, :], in0=ps_m[:, 16 + s:32:2], scalar=1.0, in1=badaT_sb[:, 8:16],
                                op0=ALU.add, op1=ALU.add),
                                reads=["ps_m", "badaT"], writes=[("sc1T", s)])
                else:
                    for s in range(2):
                        for n in range(2):
                            pb = ps_b[(s * 2 + n) % 2]
                            pk = ("ps_b", (s * 2 + n) % 2)
                            for i in range(8):
                                P.op("pe", lambda e, pb=pb, wt=wt, s=s, n=n, i=i: e.matmul(
                                    pb[:], lhsT=screp[:, s, i, :], rhs=wt[:, i, n * 512:(n + 1) * 512],
                                    start=(i == 0), stop=False),
                                    reads=[(wk, i // 4), ("screp", s, i)], writes=[pk], accum=True)
                            P.op("pe", lambda e, pb=pb, j=j, n=n: e.matmul(
                                pb[:], lhsT=ones_f[0:1, :], rhs=bada_row_sb[0:1, j * D + n * 512:j * D + (n + 1) * 512],
                                start=False, stop=True),
                                reads=["ones_f", "bada_row"], writes=[pk], accum=True)
                            addc = 0.0 if j == 3 else 1.0
                            P.op("act" if n == 0 else "dve",
                                 (lambda e, pb=pb, s=s, j=j, n=n, addc=addc: e.activation(
                                     out=bc[:, s, j - 2, n * 512:(n + 1) * 512], in_=pb[:], func=AF.Identity, bias=addc))
                                 if n == 0 else
                                 (lambda e, pb=pb, s=s, j=j, n=n, addc=addc: e.tensor_scalar(
                                     out=bc[:, s, j - 2, n * 512:(n + 1) * 512], in0=pb[:], scalar1=addc, scalar2=None,
                                     op0=ALU.add)),
                                 reads=[pk], writes=[("bc", s, j - 2, n)])
            P.dma("sp", lambda e: e.dma_start(out=BC, in_=bc[:]),
                  reads=[("bc", s_, j_, n_) for s_ in range(2) for j_ in range(4) for n_ in range(2)], writes=["BC"])
            P.end_phase()

        if "p0" in dbg:
            o_sh1 = dout("o_sh1T", [128, 16])
            o_sc1 = dout("o_sc1T", [128, 16])
            o_negM = dout("o_negM", [128, 1])
            P.dma("sp", lambda e: e.dma_start(out=o_sh1, in_=sh1T[:].rearrange("p a b -> p (a b)")))
            P.dma("sp", lambda e: e.dma_start(out=o_sc1, in_=sc1T[:].rearrange("p a b -> p (a b)")))
            P.dma("sp", lambda e: e.dma_start(out=o_negM, in_=negM[:]))
            P.end_phase()

        if "stop0" not in dbg:
            G = locals()
            phase1(nc, P, cfg, G)
            if "stop1" not in dbg:
                if "skip2" not in dbg:
                    phase2(nc, P, cfg, G)
                if "stop2" not in dbg:
                    if "skip3" not in dbg:
                        phase3(nc, P, cfg, G)
                    if "stop3" not in dbg:
                        NTL = NT // 128
                        X1 = dscr("X1", [NT, D])
                        H2 = dscr("H2", [NT, D], BF16)
                        XS = dscr("XS", [cfg.CAP, D], BF16)
                        YS = dscr("YS", [cfg.CAP, D])
                        maskS = sb("maskS", [128, NTL, NE])
                        wS = sb("wS", [128, NTL, NE])
                        d4i = sb("d4i", [128, NTL, 4], I32)
                        w4 = sb("w4", [128, NTL, 4])
                        idxw = sb("idxw", [128, cfg.NBLK, 8], I32)
                        idxb1 = sb("idxb1", [128, cfg.NBLK], I32)
                        idxb2 = sb("idxb2", [128, cfg.NBLK], I32)
                        G = locals()
                        phase4(nc, P, cfg, G)
                        if "stop4" not in dbg:
                            phase5(nc, P, cfg, G)
                            phase6(nc, P, cfg, G)
                            phase7(nc, P, cfg, G)

    return nc


def phase1(nc, P, cfg, G):
    SA, H = cfg.SA, cfg.H
    x_seg, rope_cos, rope_sin = G["x_seg"], G["rope_cos"], G["rope_sin"]
    QT, KT, VV, XR, GG, GL = G["QT"], G["KT"], G["VV"], G["XR"], G["GG"], G["GL"]
    ident, perm_bf, ones_bf = G["ident"], G["perm_bf"], G["ones_bf"]
    sh1T, sc1T, gain_sb, w_in = G["sh1T"], G["sc1T"], G["gain_sb"], G["w_in"]
    with ExitStack() as st:
        psb = lambda name, shape, dt=F32: st.enter_context(nc.sbuf_tensor("p1_" + name, list(shape), dt))
        pps = lambda name, shape, dt=F32: st.enter_context(nc.psum_tensor("p1_" + name, list(shape), dt))
        w_sb = psb("w_in_sb", [128, 8, W_IN_COLS], BF16)
        xbuf = [psb(f"xbuf{i}", [128, 4, D]) for i in range(2)]
        st6 = psb("st6", [128, 4, 2, 6])
        mv = psb("mv", [128, 4, 2])
        sd = psb("sd", [128, 4])
        rstd = psb("rstd", [128, 4])
        nmr = psb("nmr", [128, 4])
        hT = [psb(f"hT{i}", [128, 8, 512], BF16) for i in range(2)]
        cosb = [psb(f"cosb{i}", [128, 512]) for i in range(2)]
        sinb = [psb(f"sinb{i}", [128, 512]) for i in range(2)]
        NST = 4
        zc = [psb(f"zc{i}", [128, 512], BF16) for i in range(NST)]
        zsq = [psb(f"zsq{i}", [128, 512], BF16) for i in range(NST)]
        rt = [psb(f"rt{i}", [128, 512]) for i in range(NST)]
        t1 = [psb(f"t1{i}", [128, 512]) for i in range(NST)]
        t2 = [psb(f"t2{i}", [128, 512]) for i in range(NST)]
        qo = [psb(f"qo{i}", [128, 512], BF16) for i in range(NST)]
        fo = [psb(f"fo{i}", [128, 512]) for i in range(3)]
        g1 = [psb(f"g1{i}", [128, 512]) for i in range(2)]
        g2 = [psb(f"g2{i}", [128, 512]) for i in range(2)]
        vo = [psb(f"vo{i}", [128, 4, 256], BF16) for i in range(2)]
        ps_h = [pps(f"ps_h{i}", [128, 512]) for i in range(2)]
        ps_z = [pps(f"ps_z{i}", [128, 512]) for i in range(3)]
        ps_a = [pps(f"ps_a{i}", [128, 512]) for i in range(2)]

        for k in range(8):
            for c0 in range(0, W_IN_COLS, 1408):
                P.dma("pool", lambda e, k=k, c0=c0: e.dma_start(out=w_sb[:, k, c0:c0 + 1408],
                                                               in_=w_in[k * 128:(k + 1) * 128, c0:c0 + 1408]),
                      writes=[("w_sb", k, c0)])
        wkeys = lambda k, c: [("w_sb", k, (c // 1408) * 1408)] + (
            [("w_sb", k, ((c + 127) // 1408) * 1408)] if (c + 127) // 1408 != c // 1408 else [])

        cnt = {"z": 0, "st": 0, "f": 0, "g": 0, "blk": 0}
        segs = [("A", SA, 0, "A", 0), ("B", H, 1, "S", 0), ("C", H, 1, "S", H)]
        blocks = []
        for (seg, ntok, sidx, kvname, kvoff) in segs:
            for b in range(ntok // 512):
                blocks.append((seg, ntok, sidx, kvname, kvoff, b))

        def load_block(bi):
            seg, ntok, sidx, kvname, kvoff, b = blocks[bi]
            t0 = b * 512
            xb_, cb_, sb_ = xbuf[bi % 2], cosb[bi % 2], sinb[bi % 2]
            kx, kn = ("xbuf", bi % 2), ("xn", bi % 2)
            P.dma("sp", lambda e: e.dma_start(
                out=xb_[:], in_=x_seg[seg][t0:t0 + 512, :].rearrange("(t p) d -> p t d", p=128)), writes=[kx] + [(kn, t) for t in range(4)])
            P.dma("sp", lambda e: e.dma_start(out=cb_[:], in_=rope_cos[seg][:, t0:t0 + 512]), writes=[("cos", bi % 2)])
            P.dma("sp", lambda e: e.dma_start(out=sb_[:], in_=rope_sin[seg][:, t0:t0 + 512]), writes=[("sin", bi % 2)])

        def front_gen(bi):
            seg, ntok, sidx, kvname, kvoff, b = blocks[bi]
            xb_, hT_ = xbuf[bi % 2], hT[bi % 2]
            xn_ = xb_
            kx, kn, kh = ("xbuf", bi % 2), ("xn", bi % 2), ("hT", bi % 2)
            for t in range(4):
                for c in range(2):
                    P.op("dve", lambda e, xb_=xb_, t=t, c=c: e.bn_stats(out=st6[:, t, c, :], in_=xb_[:, t, c * 512:(c + 1) * 512]),
                         reads=[kx], writes=[("st6", t, c)])
                P.op("dve", lambda e, t=t: e.bn_aggr(out=mv[:, t, :], in_=st6[:, t, :, :].rearrange("p a b -> p (a b)")),
                     reads=[("st6", t, 0), ("st6", t, 1)], writes=[("mv", t)])
            yield
            P.op("act", lambda e: e.activation(out=sd[:], in_=mv[:, :, 1], func=AF.Sqrt, bias=LN_EPS),
                 reads=[("mv", t) for t in range(4)], writes=["sd"])
            P.op("dve", lambda e: e.reciprocal(out=rstd[:], in_=sd[:]), reads=["sd"], writes=["rstd"])
            P.op("dve", lambda e: e.scalar_tensor_tensor(out=nmr[:], in0=mv[:, :, 0], scalar=-1.0, in1=rstd[:],
                                                         op0=ALU.mult, op1=ALU.mult),
                 reads=[("mv", t) for t in range(4)] + ["rstd"], writes=["nmr"])
            yield
            for t in range(4):
                P.op("act", lambda e, xb_=xb_, xn_=xn_, t=t: e.activation(
                    out=xn_[:, t, :], in_=xb_[:, t, :], func=AF.Identity, scale=rstd[:, t:t + 1], bias=nmr[:, t:t + 1]),
                    reads=[kx, "nmr", "rstd"], writes=[kx, (kn, t)])
            for k in range(8):
                yield
                ph = ps_h[k % 2]
                for t in range(4):
                    P.op("pe", lambda e, ph=ph, xn_=xn_, t=t, k=k: e.transpose(
                        out=ph[:, t * 128:(t + 1) * 128], in_=xn_[:, t, k * 128:(k + 1) * 128], identity=ident[:]),
                        reads=[(kn, t), "ident"], writes=[("ps_h", k % 2)], accum=True)
                if k % 2 == 0:
                    P.op("act", lambda e, ph=ph, hT_=hT_, k=k, sidx=sidx: e.activation(
                        out=hT_[:, k, :], in_=ph[:], func=AF.Identity, scale=sc1T[:, sidx, k:k + 1],
                        bias=sh1T[:, sidx, k:k + 1]), reads=[("ps_h", k % 2), ("sc1T", sidx), ("sh1T", sidx)],
                        writes=[(kh, k)])
                else:
                    P.op("dve", lambda e, ph=ph, hT_=hT_, k=k, sidx=sidx: e.tensor_scalar(
                        out=hT_[:, k, :], in0=ph[:], scalar1=sc1T[:, sidx, k:k + 1], scalar2=sh1T[:, sidx, k:k + 1],
                        op0=ALU.mult, op1=ALU.add), reads=[("ps_h", k % 2), ("sc1T", sidx), ("sh1T", sidx)],
                        writes=[(kh, k)])


        def advance(g_):
            try:
                next(g_)
                return g_
            except StopIteration:
                return None

        load_block(0)
        for _ in front_gen(0):
            pass
        for bi_ in range(len(blocks)):
            if True:
                seg, ntok, sidx, kvname, kvoff, b = blocks[bi_]
                full = seg != "C"
                bi = bi_
                nf = None
                if bi + 1 < len(blocks):
                    load_block(bi + 1)
                    nf = front_gen(bi + 1)
                t0 = b * 512
                xb_, hT_ = xbuf[bi % 2], hT[bi % 2]
                xn_ = xb_
                cb_, sb_ = cosb[bi % 2], sinb[bi % 2]
                kx, kn, kh = ("xbuf", bi % 2), ("xn", bi % 2), ("hT", bi % 2)
                if "hT0" in cfg.dbg and bi == 0:
                    o_hT = nc.dram_tensor("o_hT", [128, 8, 512], BF16, kind="ExternalOutput").ap()
                    o_xn = nc.dram_tensor("o_xn", [128, 4, D], F32, kind="ExternalOutput").ap()
                    P.dma("sp", lambda e, hT_=hT_: e.dma_start(out=o_hT, in_=hT_[:]), reads=[(kh, k) for k in range(8)])
                    P.dma("sp", lambda e, xn_=xn_: e.dma_start(out=o_xn, in_=xn_[:]), reads=[(kn, t) for t in range(4)])

                def zmm(c):
                    zi = cnt["z"] % 3
                    cnt["z"] += 1
                    pz = ps_z[zi]
                    for k in range(8):
                        P.op("pe", lambda e, pz=pz, k=k, c=c, hT_=hT_: e.matmul(
                            pz[:], lhsT=w_sb[:, k, c * 128:(c + 1) * 128], rhs=hT_[:, k, :], start=(k == 0), stop=(k == 7)),
                            reads=wkeys(k, c * 128) + [(kh, k)], writes=[("ps_z", zi)], accum=True)
                    return pz, ("ps_z", zi)

                chunks = list(range(0, 10)) + list(range(12, 44)) if full else [8, 9] + list(range(12, 20))
                for c in chunks:
                    pz, pzk = zmm(c)
                    if c >= 12 and nf is not None:
                        nf = advance(nf)
                    if c < 10:
                        si = cnt["st"] % NST
                        cnt["st"] += 1
                        ai = si % 2
                        gi = 0 if c < 8 else 1
                        zc_, zsq_, rt_, t1_, t2_, qo_ = zc[si], zsq[si], rt[si], t1[si], t2[si], qo[si]
                        P.op("act", lambda e, zc_=zc_, pz=pz, gi=gi: e.activation(out=zc_[:], in_=pz[:], func=AF.Identity,
                                                                            scale=gain_sb[:, gi:gi + 1]),
                             reads=[pzk, "gain"], writes=[("zc", si)])
                        P.op("act", lambda e, zsq_=zsq_, pz=pz: e.activation(out=zsq_[:], in_=pz[:], func=AF.Square),
                             reads=[pzk], writes=[("zsq", si)])
                        pa_ss, pa_rot = ps_a[0], ps_a[1]
                        P.op("pe", lambda e, zsq_=zsq_, pa_ss=pa_ss: e.matmul(pa_ss[:], lhsT=ones_bf[:], rhs=zsq_[:], start=True, stop=True),
                             reads=["ones_bf", ("zsq", si)], writes=[("ps_a", 0)])
                        P.op("pe", lambda e, zc_=zc_, pa_rot=pa_rot: e.matmul(pa_rot[:], lhsT=perm_bf[:], rhs=zc_[:], start=True, stop=True),
                             reads=["perm_bf", ("zc", si)], writes=[("ps_a", 1)])
                        P.op("act", lambda e, rt_=rt_, pa_ss=pa_ss: e.activation(out=rt_[:], in_=pa_ss[:], func=AF.Sqrt,
                                                                           scale=1.0 / 128.0, bias=RMS_EPS),
                             reads=[("ps_a", 0)], writes=[("rt", si)])
                        P.op("pool", lambda e, t1_=t1_, zc_=zc_, cb_=cb_: e.tensor_tensor(out=t1_[:], in0=zc_[:], in1=cb_[:], op=ALU.mult),
                             reads=[("zc", si), ("cos", bi % 2)], writes=[("t1", si)])
                        P.op("dve", lambda e, t2_=t2_, pa_rot=pa_rot, sb_=sb_: e.tensor_tensor(out=t2_[:], in0=pa_rot[:], in1=sb_[:], op=ALU.mult),
                             reads=[("ps_a", 1), ("sin", bi % 2)], writes=[("t2", si)])
                        P.op("dve", lambda e, rt_=rt_: e.reciprocal(out=rt_[:], in_=rt_[:]), reads=[("rt", si)], writes=[("rt", si)])
                        P.op("pool", lambda e, t1_=t1_, t2_=t2_: e.tensor_tensor(out=t1_[:], in0=t1_[:], in1=t2_[:], op=ALU.add),
                             reads=[("t1", si), ("t2", si)], writes=[("t1", si)])
                        P.op("pool", lambda e, qo_=qo_, t1_=t1_, rt_=rt_: e.tensor_tensor(out=qo_[:], in0=t1_[:], in1=rt_[:], op=ALU.mult),
                             reads=[("t1", si), ("rt", si)], writes=[("qo", si)])
                        if c < 8:
                            dst = QT[seg][c, :, t0:t0 + 512]
                        else:
                            dst = KT[kvname][c - 8, :, kvoff + t0:kvoff + t0 + 512]
                        P.dma("sp", lambda e, dst=dst, qo_=qo_: e.dma_start(out=dst, in_=qo_[:]),
                              reads=[("qo", si)], writes=[("QK", seg, c, b)])
                    elif c < 20:
                        fi = cnt["f"] % 3
                        cnt["f"] += 1
                        fo_ = fo[fi]
                        P.op("act" if c % 2 == 0 else "dve",
                             (lambda e, fo_=fo_, pz=pz: e.activation(out=fo_[:], in_=pz[:], func=AF.Identity)) if c % 2 == 0 else
                             (lambda e, fo_=fo_, pz=pz: e.tensor_copy(out=fo_[:], in_=pz[:])),
                             reads=[pzk], writes=[("fo", fi)])
                        P.dma("sp", lambda e, fo_=fo_, seg=seg, c=c, t0=t0: e.dma_start(out=XR[seg][c - 12, :, t0:t0 + 512], in_=fo_[:]),
                              reads=[("fo", fi)], writes=[("XR", seg, c, b)])
                    elif c < 28:
                        gi_ = cnt["g"] % 2
                        cnt["g"] += 1
                        fi = cnt["f"] % 3
                        cnt["f"] += 1
                        g1_, g2_, fo_ = g1[gi_], g2[gi_], fo[fi]
                        P.op("act", lambda e, g1_=g1_, pz=pz: e.activation(out=g1_[:], in_=pz[:], func=AF.Square),
                             reads=[pzk], writes=[("g1", gi_)])
                        P.op("dve", lambda e, g1_=g1_: e.tensor_scalar(out=g1_[:], in0=g1_[:], scalar1=0.044715, scalar2=1.0,
                                                                        op0=ALU.mult, op1=ALU.add),
                             reads=[("g1", gi_)], writes=[("g1", gi_)])
                        P.op("dve", lambda e, g1_=g1_, g2_=g2_, pz=pz: e.tensor_tensor(out=g2_[:], in0=pz[:], in1=g1_[:], op=ALU.mult),
                             reads=[pzk, ("g1", gi_)], writes=[("g2", gi_)])
                        P.op("act", lambda e, g2_=g2_: e.activation(out=g2_[:], in_=g2_[:], func=AF.Sigmoid, scale=1.5957691216057308),
                             reads=[("g2", gi_)], writes=[("g2", gi_)])
                        P.op("dve", lambda e, fo_=fo_, g2_=g2_, pz=pz: e.tensor_tensor(out=fo_[:], in0=pz[:], in1=g2_[:], op=ALU.mult),
                             reads=[pzk, ("g2", gi_)], writes=[("fo", fi)])
                        P.dma("sp", lambda e, fo_=fo_, seg=seg, c=c, t0=t0: e.dma_start(out=GG[seg][c - 20, :, t0:t0 + 512], in_=fo_[:]),
                              reads=[("fo", fi)], writes=[("GG", seg, c, b)])
                    else:
                        fi = cnt["f"] % 3
                        cnt["f"] += 1
                        fo_ = fo[fi]
                        P.op("act", lambda e, fo_=fo_, pz=pz: e.activation(out=fo_[:], in_=pz[:], func=AF.Sigmoid),
                             reads=[pzk], writes=[("fo", fi)])
                        P.dma("sp", lambda e, fo_=fo_, seg=seg, c=c, t0=t0: e.dma_start(out=GL[seg][c - 28, :, t0:t0 + 512], in_=fo_[:]),
                              reads=[("fo", fi)], writes=[("GL", seg, c, b)])
                while nf is not None:
                    nf = advance(nf)
                vo_ = vo[bi % 2]
                for half in range(2):
                    zi = cnt["z"] % 3
                    cnt["z"] += 1
                    pz = ps_z[zi]
                    for tt in range(2):
                        t = half * 2 + tt
                        for k in range(8):
                            P.op("pe", lambda e, pz=pz, tt=tt, t=t, k=k, hT_=hT_: e.matmul(
                                pz[:, tt * 256:(tt + 1) * 256], lhsT=hT_[:, k, t * 128:(t + 1) * 128], rhs=w_sb[:, k, 1280:1536],
                                start=(k == 0), stop=(k == 7)),
                                reads=[(kh, k), ("w_sb", k, 0), ("w_sb", k, 1408)], writes=[("ps_z", zi)], accum=True)
                    P.op("dve", lambda e, vo_=vo_, pz=pz, half=half: e.tensor_copy(
                        out=vo_[:, half * 2:half * 2 + 2, :].rearrange("p a b -> p (a b)"), in_=pz[:]),
                        reads=[("ps_z", zi)], writes=[("vo", bi % 2, half)])
                P.dma("sp", lambda e, vo_=vo_, kvname=kvname, r0=kvoff + t0: e.dma_start(
                    out=VV[kvname][r0:r0 + 512, :].rearrange("(t p) d -> p t d", p=128), in_=vo_[:]),
                    reads=[("vo", bi % 2, 0), ("vo", bi % 2, 1)], writes=[("VV", seg, b)])
        P.end_phase()


def phase2(nc, P, cfg, G):
    SA, H = cfg.SA, cfg.H
    QT, KT, VV, ATT = G["QT"], G["KT"], G["VV"], G["ATT"]
    ones_bf, negM = G["ones_bf"], G["negM"]
    NKMAX = max(SA, 2 * H)
    scale = float(HD) ** -0.5
    with ExitStack() as st:
        psb = lambda name, shape, dt=F32: st.enter_context(nc.sbuf_tensor("p2_" + name, list(shape), dt))
        pps = lambda name, shape, dt=F32: st.enter_context(nc.psum_tensor("p2_" + name, list(shape), dt))
        kT = [psb(f"kT{i}", [128, NKMAX], BF16) for i in range(2)]
        vS = [psb(f"vS{i}", [128, NKMAX // 128, 128], BF16) for i in range(2)]
        qS = [psb(f"qS{i}", [128, 512], BF16) for i in range(3)]
        pS = [psb(f"pS{i}", [128, 1024], BF16) for i in range(3)]
        pA = [psb(f"pA{i}", [128, 512], BF16) for i in range(3)]
        rd = [psb(f"rd{i}", [128, 512]) for i in range(2)]
        ao = [psb(f"ao{i}", [128, 512], BF16) for i in range(2)]
        ps_s = [pps(f"ps_s{i}", [128, 1024]) for i in range(2)]
        ps_o = [pps(f"ps_o{i}", [128, 512]) for i in range(2)]
        ps_d = [pps(f"ps_d{i}", [128, 512]) for i in range(2)]

        groups = []
        qblocks = []
        for (seg, nq, kvname, nk) in (("A", SA, "A", SA), ("B", H, "S", 2 * H)):
            for g in range(NKV):
                gidx = len(groups)
                groups.append((kvname, g, nk))
                for hq in range(4):
                    for qb in range(nq // 512):
                        qblocks.append((seg, g * 4 + hq, qb, gidx))

        def load_group(gidx):
            kvname, g, nk = groups[gidx]
            nkc = nk // 128
            kT_, vS_ = kT[gidx % 2], vS[gidx % 2]
            kk, kvk = ("kT", gidx % 2), ("vS", gidx % 2)
            P.dma("sp", lambda e: e.dma_start(out=kT_[:, 0:nk], in_=KT[kvname][g, :, :]), reads=[("KT", kvname)], writes=[kk])
            for v0 in range(0, nkc, 16):
                v1 = min(nkc, v0 + 16)
                P.dma("sp", lambda e, v0=v0, v1=v1: e.dma_start(
                    out=vS_[:, v0:v1, :], in_=VV[kvname][v0 * 128:v1 * 128, g * 128:(g + 1) * 128].rearrange("(t p) d -> p t d", p=128)),
                    reads=[("VV", kvname)], writes=[kvk])

        def load_q(qi):
            seg, h, qb, gidx = qblocks[qi]
            q_ = qS[qi % 3]
            P.dma("sp", lambda e: e.dma_start(out=q_[:], in_=QT[seg][h, :, qb * 512:(qb + 1) * 512]),
                  reads=[("QT", seg)], writes=[("qS", qi % 3)])

        loaded_groups = set()
        load_group(0)
        loaded_groups.add(0)
        load_q(0)
        si = 0
        pi = 0
        def do_block(qi):
            nonlocal si, pi
            seg, h, qb, gidx = qblocks[qi]
            if qi + 1 < len(qblocks):
                ng = qblocks[qi + 1][3]
                if ng not in loaded_groups:
                    load_group(ng)
                    loaded_groups.add(ng)
                load_q(qi + 1)
            kvname, g, nk = groups[gidx]
            nkp = nk // 256
            kT_, vS_ = kT[gidx % 2], vS[gidx % 2]
            kk, kvk = ("kT", gidx % 2), ("vS", gidx % 2)
            q_ = qS[qi % 3]
            qk = ("qS", qi % 3)
            po, pd = ps_o[qi % 2], ps_d[qi % 2]
            pok, pdk = ("ps_o", qi % 2), ("ps_d", qi % 2)
            rd_, ao_ = rd[qi % 2], ao[qi % 2]
            rdk, aok = ("rd", qi % 2), ("ao", qi % 2)

            def smm(kp):
                nonlocal si
                ps = ps_s[si % 2]
                key = ("ps_s", si % 2)
                si += 1
                for hh in range(2):
                    kc = kp * 2 + hh
                    P.op("pe", lambda e, ps=ps, kc=kc, hh=hh: e.matmul(
                        ps[:, hh * 512:(hh + 1) * 512], lhsT=kT_[:, kc * 128:(kc + 1) * 128], rhs=q_[:], start=True, stop=True),
                        reads=[kk, qk], writes=[(key, hh)])
                return ps, key

            nxt = smm(0)
            for kp in range(nkp):
                cur = nxt
                if kp + 1 < nkp:
                    nxt = smm(kp + 1)
                p_, pa_ = pS[pi % 3], pA[pi % 3]
                pk, pak = ("pS", pi % 3), ("pA", pi % 3)
                pi += 1
                P.op("act", lambda e, p_=p_, cur=cur: e.activation(out=p_[:], in_=cur[0][:], func=AF.Exp, scale=scale, bias=negM[:, 0:1]),
                     reads=[(cur[1], 0), (cur[1], 1), "negM"], writes=[pk])
                for hh in range(2):
                    kc = kp * 2 + hh
                    P.op("pe", lambda e, kc=kc, hh=hh, p_=p_: e.matmul(
                        po[:], lhsT=vS_[:, kc, :], rhs=p_[:, hh * 512:(hh + 1) * 512], start=(kc == 0), stop=(kc == 2 * nkp - 1)),
                        reads=[kvk, pk], writes=[pok], accum=True)
                P.op("dve", lambda e, p_=p_, pa_=pa_: e.tensor_tensor(out=pa_[:], in0=p_[:, 0:512], in1=p_[:, 512:1024], op=ALU.add),
                     reads=[pk], writes=[pak])
                P.op("pe", lambda e, kp=kp, pa_=pa_: e.matmul(pd[:], lhsT=ones_bf[:], rhs=pa_[:], start=(kp == 0), stop=(kp == nkp - 1)),
                     reads=["ones_bf", pak], writes=[pdk], accum=True)
            P.op("dve", lambda e: e.reciprocal(out=rd_[:], in_=pd[:]), reads=[pdk], writes=[rdk])
            P.op("dve", lambda e: e.tensor_tensor(out=ao_[:], in0=po[:], in1=rd_[:], op=ALU.mult), reads=[pok, rdk], writes=[aok])
            P.dma("sp", lambda e: e.dma_start(out=ATT[seg][h, :, qb * 512:(qb + 1) * 512], in_=ao_[:]),
                  reads=[aok], writes=[("ATT", seg, h, qb)])

        for qi in range(len(qblocks)):
            do_block(qi)
        P.end_phase()


def phase3(nc, P, cfg, G):
    SA, H = cfg.SA, cfg.H
    XR, GG, REC = G["XR"], G["GG"], G["REC"]
    ident, flags_sb = G["ident"], G["flags_sb"]
    conv_wT, conv_bT = G["conv_wT"], G["conv_bT"]
    lru_wa, lru_wx = G["lru_wa"], G["lru_wx"]
    SMAX = max(SA, H)
    with ExitStack() as st:
        psb = lambda name, shape, dt=F32: st.enter_context(nc.sbuf_tensor("p3_" + name, list(shape), dt))
        pps = lambda name, shape, dt=F32: st.enter_context(nc.psum_tensor("p3_" + name, list(shape), dt))
        SL = min(1024, SA, H)
        cw = psb("cw", [128, 8, 4])
        cb = psb("cb", [128, 8])
        ba = psb("ba", [128, 16])
        bx = psb("bx", [128, 16])
        lam = psb("lam", [128, 16])
        cf = psb("cf", [128, 16])
        diag = psb("diag", [128, 4, 128])
        wab = [psb(f"wab{i}", [128, 2, 128], BF16) for i in range(2)]
        wxb = [psb(f"wxb{i}", [128, 2, 128], BF16) for i in range(2)]
        xpad = [psb(f"xpad{i}", [128, SMAX + 3]) for i in range(2)]
        hal = psb("hal", [128, 3])
        xc2 = [psb(f"xc{i}", [128, SMAX]) for i in range(2)]
        xcb2 = [psb(f"xcb{i}", [128, SMAX], BF16) for i in range(2)]
        hf = psb("hf", [128, SMAX])
        hbt = psb("hbt", [128, SMAX])
        gg_one = psb("gg0", [128, SMAX])
        gg2 = [gg_one, gg_one]
        r2 = [psb(f"r_{i}", [128, SL]) for i in range(2)]
        i2 = [psb(f"i_{i}", [128, SL]) for i in range(4)]
        a2 = [psb(f"a_{i}", [128, SL]) for i in range(4)]
        t2 = [psb(f"t_{i}", [128, SL]) for i in range(4)]
        hs2 = [psb(f"hs{i}", [128, SL]) for i in range(2)]
        cf2 = psb("cf2", [128, 16])
        carry2 = psb("carry2", [128, 2])
        ro = [psb(f"ro{i}", [128, SL], BF16) for i in range(2)]
        stC = psb("stC", [128, 2])
        init2 = psb("init2", [128, 2])
        zero1 = psb("zero1", [128, 1])
        ps_r2 = [pps(f"ps_r{i}", [128, SL]) for i in range(2)]
        ps_i2 = [pps(f"ps_i{i}", [128, SL]) for i in range(2)]
        ps_r = ps_r2[0]

        P.dma("sp", lambda e: e.dma_start(out=cw[:], in_=conv_wT.ap()), writes=["cw"])
        P.dma("sp", lambda e: e.dma_start(out=cb[:], in_=conv_bT.ap()), writes=["cb"])
        P.dma("sp", lambda e: e.dma_start(out=ba[:], in_=G["lru_baT"].rearrange("p a b -> p (a b)")), writes=["ba"])
        P.dma("sp", lambda e: e.dma_start(out=bx[:], in_=G["lru_bxT"].rearrange("p a b -> p (a b)")), writes=["bx"])
        P.dma("sp", lambda e: e.dma_start(out=lam[:], in_=G["lru_lamT"].rearrange("p a b -> p (a b)")), writes=["lam"])
        P.op("dve", lambda e: e.memset(zero1[:], 0.0), writes=["zero1"])
        P.op("act", lambda e: e.activation(out=cf[:], in_=lam[:], func=AF.Exp, scale=-1.0), reads=["lam"], writes=["cf"])
        P.op("act", lambda e: e.activation(out=cf[:], in_=cf[:], func=AF.Ln, bias=1.0), reads=["cf"], writes=["cf"])
        P.op("dve", lambda e: e.tensor_scalar(out=cf[:], in0=cf[:], scalar1=-LRU_C, scalar2=None, op0=ALU.mult),
             reads=["cf"], writes=["cf"])
        P.op("dve", lambda e: e.tensor_scalar(out=cf2[:], in0=cf[:], scalar1=0.5, scalar2=None, op0=ALU.mult),
             reads=["cf"], writes=["cf2"])
        P.op("dve", lambda e: e.tensor_scalar(out=ba[:], in0=ba[:], scalar1=0.5, scalar2=None, op0=ALU.mult), reads=["ba"], writes=["ba"])
        P.op("dve", lambda e: e.tensor_scalar(out=bx[:], in0=bx[:], scalar1=0.5, scalar2=None, op0=ALU.mult), reads=["bx"], writes=["bx"])

        roi = 0
        jobs = [(n, seg, S, other) for n in range(8) for (seg, S, other) in (("A", SA, None), ("C", H, "B"), ("B", H, "C"))]

        def prep_chunk(n):
            wab_, wxb_ = wab[n % 2], wxb[n % 2]
            P.dma("pool", lambda e: e.dma_start(out=wab_[:], in_=lru_wa[:, n, :, :].rearrange("d i j -> i d j")), writes=[("wab", n % 2)])
            P.dma("pool", lambda e: e.dma_start(out=wxb_[:], in_=lru_wx[:, n, :, :].rearrange("d i j -> i d j")), writes=[("wxb", n % 2)])
            for k in range(4):
                P.op("dve", lambda e, k=k: e.tensor_scalar(out=diag[:, k, :], in0=ident[:], scalar1=cw[:, n, k:k + 1], scalar2=None, op0=ALU.mult),
                     reads=["ident", "cw"], writes=[("diag", k)])

        def load_job(ji):
            n, seg, S, other = jobs[ji]
            own = seg != "C"
            xp = xpad[ji % 2]
            xk = ("xpad", ji % 2)
            P.dma("sp", lambda e: e.dma_start(out=xp[:, 2:S + 2], in_=XR[seg][n, :, :]), reads=[("XR", seg)], writes=[(xk, "d")])
            if other is None:
                P.op("pool", lambda e: e.memset(xp[:, 0:2], 0.0), writes=[(xk, "l")])
                P.op("pool", lambda e: e.memset(xp[:, S + 2:S + 3], 0.0), writes=[(xk, "r")])
            else:
                fl_l = 0 if seg == "B" else 1
                fl_r = 1 if seg == "B" else 0
                P.dma("sp", lambda e: e.dma_start(out=hal[:, 0:2], in_=XR[other][n, :, S - 2:S], allow_slow_non_contiguous=True),
                      reads=[("XR", other)], writes=["hal_l"])
                P.dma("sp", lambda e: e.dma_start(out=hal[:, 2:3], in_=XR[other][n, :, 0:1], allow_slow_non_contiguous=True),
                      reads=[("XR", other)], writes=["hal_r"])
                P.op("dve", lambda e: e.tensor_scalar(out=xp[:, 0:2], in0=hal[:, 0:2], scalar1=flags_sb[:, fl_l:fl_l + 1], scalar2=None, op0=ALU.mult),
                     reads=["hal_l", "flags"], writes=[(xk, "l")])
                P.op("dve", lambda e: e.tensor_scalar(out=xp[:, S + 2:S + 3], in0=hal[:, 2:3], scalar1=flags_sb[:, fl_r:fl_r + 1], scalar2=None, op0=ALU.mult),
                     reads=["hal_r", "flags"], writes=[(xk, "r")])

        def load_gg(ji):
            n, seg, S, other = jobs[ji]
            gg_ = gg2[ji % 2]
            P.dma("sp", lambda e: e.dma_start(out=gg_[:, 0:S], in_=GG[seg][n, :, :]), reads=[("GG", seg)], writes=[("gg", 0)])

        def conv_slab(ji, j):
            n, seg, S, other = jobs[ji]
            xp = xpad[ji % 2]
            xk = ("xpad", ji % 2)
            xc_, xcb_ = xc2[ji % 2], xcb2[ji % 2]
            c0 = j * SL
            for jj in range(SL // 512):
                t0 = c0 + jj * 512
                for k in range(4):
                    P.op("pe", lambda e, jj=jj, k=k, t0=t0: e.matmul(
                        ps_r[:, jj * 512:(jj + 1) * 512], lhsT=diag[:, k, :], rhs=xp[:, t0 + k:t0 + k + 512], start=(k == 0), stop=(k == 3)),
                        reads=[("diag", k), (xk, "d"), (xk, "l"), (xk, "r")], writes=[("ps_r", 0, jj)], accum=True)

        def conv_evac(ji, j):
            n, seg, S, other = jobs[ji]
            xc_, xcb_ = xc2[ji % 2], xcb2[ji % 2]
            c0 = j * SL
            P.op("act", lambda e: e.activation(out=xc_[:, c0:c0 + SL], in_=ps_r[:], func=AF.Identity, bias=cb[:, n:n + 1]),
                 reads=[("ps_r", 0, jj) for jj in range(SL // 512)] + ["cb"], writes=[("xc", ji % 2, c0)])
            P.op("dve", lambda e: e.tensor_copy(out=xcb_[:, c0:c0 + SL], in_=xc_[:, c0:c0 + SL]),
                 reads=[("xc", ji % 2, c0)], writes=[("xcb", ji % 2, c0)])

        def slab_dir(ji, d, si_):
            n, seg, S, other = jobs[ji]
            own = seg != "C"
            nsl = S // SL
            wab_, wxb_ = wab[n % 2], wxb[n % 2]
            xc_, xcb_ = xc2[ji % 2], xcb2[ji % 2]
            pb = d
            c0 = (si_ if d == 0 else nsl - 1 - si_) * SL
            pq = d * 2 + (si_ % 2)
            r_, i_, a_, t_, hs = r2[pb], i2[pq], a2[pq], t2[pq], hs2[pb]
            rk_, ik_, ak_, tk_, hk_ = ("r_", pb), ("i_", pq), ("a_", pq), ("t_", pq), ("hs", pb)
            psr, psi = ps_r2[pb], ps_i2[pb]
            nj = SL // 512
            for j in range(nj):
                t0 = c0 + j * 512
                P.op("pe", lambda e, j=j, t0=t0: e.matmul(
                    psr[:, j * 512:(j + 1) * 512], lhsT=wab_[:, d, :], rhs=xcb_[:, t0:t0 + 512], start=True, stop=True),
                    reads=[("wab", n % 2), ("xcb", ji % 2, c0)], writes=[("ps_r", pb, j)])
                P.op("pe", lambda e, j=j, t0=t0: e.matmul(
                    psi[:, j * 512:(j + 1) * 512], lhsT=wxb_[:, d, :], rhs=xcb_[:, t0:t0 + 512], start=True, stop=True),
                    reads=[("wxb", n % 2), ("xcb", ji % 2, c0)], writes=[("ps_i", pb, j)])
            col = d * 8 + n
            P.op("act", lambda e: e.activation(out=r_[:], in_=psr[:], func=AF.Tanh, scale=0.5, bias=ba[:, col:col + 1]),
                 reads=[("ps_r", pb, j) for j in range(nj)] + ["ba"], writes=[rk_])
            P.op("act", lambda e: e.activation(out=i_[:], in_=psi[:], func=AF.Tanh, scale=0.5, bias=bx[:, col:col + 1]),
                 reads=[("ps_i", pb, j) for j in range(nj)] + ["bx"], writes=[ik_])
            yield
            P.op("act", lambda e: e.activation(out=a_[:], in_=r_[:], func=AF.Exp, scale=cf2[:, col:col + 1], bias=cf2[:, col:col + 1]),
                 reads=[rk_, "cf2"], writes=[ak_])
            P.op("act", lambda e: e.activation(out=t_[:], in_=r_[:], func=AF.Exp, scale=cf[:, col:col + 1], bias=cf[:, col:col + 1]),
                 reads=[rk_, "cf"], writes=[tk_])
            P.op("dve", lambda e: e.scalar_tensor_tensor(out=i_[:], in0=i_[:], scalar=1.0, in1=xc_[:, c0:c0 + SL], op0=ALU.add, op1=ALU.mult),
                 reads=[ik_, ("xc", ji % 2, c0)], writes=[ik_])
            yield
            P.op("act", lambda e: e.activation(out=t_[:], in_=t_[:], func=AF.Sqrt, scale=-0.25, bias=0.25), reads=[tk_], writes=[tk_])
            P.op("pool", lambda e: e.tensor_tensor(out=i_[:], in0=i_[:], in1=t_[:], op=ALU.mult), reads=[ik_, tk_], writes=[ik_])
            yield
            if si_ == 0:
                if seg == "B":
                    init_ap, init_k = init2[:, d:d + 1], "init2"
                else:
                    init_ap, init_k = zero1[:, 0:1], "zero1"
            else:
                init_ap, init_k = carry2[:, d:d + 1], ("carry", d)
            if d == 0:
                dst = hf[:, c0:c0 + SL] if own else hs[:]
                dk = ("hf", c0) if own else hk_
                P.op("dve", lambda e: e.tensor_tensor_scan(out=dst, data0=a_[:], data1=i_[:], initial=init_ap, op0=ALU.mult, op1=ALU.add),
                     reads=[ak_, ik_, init_k], writes=[dk])
                last = hf[:, c0 + SL - 1:c0 + SL] if own else hs[:, SL - 1:SL]
            else:
                dstb = hbt[:, c0:c0 + SL] if own else hs[:]
                dk = ("hb", c0) if own else hk_
                P.op("dve", lambda e: e.tensor_tensor_scan(out=dstb[:, ::-1], data0=a_[:, ::-1], data1=i_[:, ::-1], initial=init_ap,
                                                           op0=ALU.mult, op1=ALU.add),
                     reads=[ak_, ik_, init_k], writes=[dk])
                last = hbt[:, c0:c0 + 1] if own else hs[:, 0:1]
            if si_ + 1 < nsl:
                P.op("dve", lambda e: e.tensor_copy(out=carry2[:, d:d + 1], in_=last), reads=[dk], writes=[("carry", d)])
            elif seg == "C":
                P.op("dve", lambda e: e.tensor_copy(out=stC[:, d:d + 1], in_=last), reads=[dk], writes=["stC"])

        def combine(ji, c0):
            nonlocal roi
            n, seg, S, other = jobs[ji]
            gg_ = gg2[ji % 2]
            ro_ = ro[roi % 2]
            rok = ("ro", roi % 2)
            roi += 1
            P.op("dve", lambda e: e.tensor_tensor(out=hbt[:, c0:c0 + SL], in0=hbt[:, c0:c0 + SL], in1=hf[:, c0:c0 + SL], op=ALU.add),
                 reads=[("hb", c0), ("hf", c0)], writes=[("hb", c0)])
            P.op("pool", lambda e: e.tensor_tensor(out=ro_[:], in0=hbt[:, c0:c0 + SL], in1=gg_[:, c0:c0 + SL], op=ALU.mult),
                 reads=[("hb", c0), ("gg", 0)], writes=[rok])
            P.dma("sp", lambda e: e.dma_start(out=REC[seg][n, :, c0:c0 + SL], in_=ro_[:]), reads=[rok], writes=[("REC", seg, n, c0)])

        prep_chunk(0)
        load_job(0)
        for j in range(jobs[0][2] // SL):
            conv_slab(0, j)
            conv_evac(0, j)
        for ji in range(len(jobs)):
            n, seg, S, other = jobs[ji]
            nsl = S // SL
            nxt_conv = []
            if ji + 1 < len(jobs):
                if jobs[ji + 1][0] != n:
                    prep_chunk(jobs[ji + 1][0])
                load_job(ji + 1)
                nxt_conv = list(range(jobs[ji + 1][2] // SL))
            if seg != "C":
                load_gg(ji)
            if seg == "B":
                P.op("dve", lambda e: e.tensor_tensor(out=init2[:], in0=stC[:], in1=flags_sb[:], op=ALU.mult),
                     reads=["stC", "flags"], writes=["init2"])
            for j in range(nsl):
                alive = [slab_dir(ji, 0, j), slab_dir(ji, 1, j)]
                first = True
                pend_evac = None
                while alive:
                    nxt_alive = []
                    for g_ in alive:
                        try:
                            next(g_)
                            nxt_alive.append(g_)
                        except StopIteration:
                            pass
                    alive = nxt_alive
                    if first and nxt_conv:
                        pend_evac = nxt_conv.pop(0)
                        conv_slab(ji + 1, pend_evac)
                    first = False
                if pend_evac is not None:
                    conv_evac(ji + 1, pend_evac)
                    pend_evac = None
            while nxt_conv:
                jj_ = nxt_conv.pop(0)
                conv_slab(ji + 1, jj_)
                conv_evac(ji + 1, jj_)
            if seg != "C":
                for c0 in range(0, S, SL):
                    combine(ji, c0)
        P.end_phase()


def _bcast_row(nc, P, G, psb, ps, name, src_ap, width, row=None):
    ones_f = G["ones_f"]
    rkey = "rowtmp" if row is not None else name + "_row"
    if row is None:
        row = psb(name + "_row", [1, width])
    out = psb(name, [128, width])
    P.dma("sp", lambda e: e.dma_start(out=row[0:1, 0:width], in_=src_ap), writes=[rkey])
    for n0 in range(0, width, 512):
        w = min(512, width - n0)
        P.op("pe", lambda e, n0=n0, w=w: e.matmul(ps[:, 0:w], lhsT=ones_f[0:1, :], rhs=row[0:1, n0:n0 + w], start=True, stop=True),
             reads=["ones_f", rkey], writes=["ps_t"])
        P.op("dve", lambda e, n0=n0, w=w: e.tensor_copy(out=out[:, n0:n0 + w], in_=ps[:, 0:w]), reads=["ps_t"], writes=[name])
    return out


def _ln_stats(P, tag, src, st6, mv, sd, rstd, srckey, eps=LN_EPS):
    for c in range(2):
        P.op("dve", lambda e, c=c: e.bn_stats(out=st6[:, c, :], in_=src[:, c * 512:(c + 1) * 512]), reads=[srckey], writes=[(tag, "st6", c)])
    P.op("dve", lambda e: e.bn_aggr(out=mv[:], in_=st6[:].rearrange("p a b -> p (a b)")),
         reads=[(tag, "st6", 0), (tag, "st6", 1)], writes=[(tag, "mv")])
    P.op("act", lambda e: e.activation(out=sd[:], in_=mv[:, 1:2], func=AF.Sqrt, bias=eps), reads=[(tag, "mv")], writes=[(tag, "sd")])
    P.op("dve", lambda e: e.reciprocal(out=rstd[:], in_=sd[:]), reads=[(tag, "sd")], writes=[(tag, "rstd")])


def phase4(nc, P, cfg, G):
    SA, H, NE = cfg.SA, cfg.H, cfg.NE
    ATT, REC, GL, BC, X1, H2 = G["ATT"], G["REC"], G["GL"], G["BC"], G["X1"], G["H2"]
    x_seg, ident, ones_f = G["x_seg"], G["ident"], G["ones_f"]
    maskS, wS = G["maskS"], G["wS"]
    alpha = float(2.0 ** 0.25)
    with ExitStack() as st:
        psb = lambda name, shape, dt=F32: st.enter_context(nc.sbuf_tensor("p4_" + name, list(shape), dt))
        pps = lambda name, shape, dt=F32: st.enter_context(nc.psum_tensor("p4_" + name, list(shape), dt))
        wpa = psb("wpa", [128, 8, D], BF16)
        wpr = psb("wpr", [128, 8, D], BF16)
        wo = psb("wo", [128, 8, D], BF16)
        wr = psb("wr", [128, 8, NE])
        attT = [psb(f"attT{i}", [128, 8, 512], BF16) for i in range(2)]
        recT = [psb(f"recT{i}", [128, 8, 512], BF16) for i in range(2)]
        glq = [psb(f"glq{i}", [128, 2, 512]) for i in range(3)]
        mT2 = [psb(f"mT{i}", [128, 8, 512], BF16) for i in range(2)]
        ta = [psb(f"ta{i}", [128, 512]) for i in range(2)]
        tb = [psb(f"tb{i}", [128, 512]) for i in range(2)]
        bcs = psb("bcs", [128, 4, D])
        xt = [psb(f"xt{i}", [128, D]) for i in range(2)]
        yt = [psb(f"yt{i}", [128, D]) for i in range(2)]
        ht = [psb(f"ht{i}", [128, D]) for i in range(2)]
        hb = [psb(f"hb{i}", [128, D], BF16) for i in range(2)]
        h2T2 = [psb(f"h2T{i}", [128, 8, 128]) for i in range(2)]
        st62 = [psb(f"st6{i}", [128, 2, 6]) for i in range(2)]
        mv2 = [psb(f"mv{i}", [128, 2]) for i in range(2)]
        sd2 = [psb(f"sd{i}", [128, 1]) for i in range(2)]
        rstd2 = [psb(f"rstd{i}", [128, 1]) for i in range(2)]
        lg2 = [psb(f"lg{i}", [128, NE]) for i in range(2)]
        m82 = [psb(f"m8{i}", [128, 8]) for i in range(2)]
        nmx2 = [psb(f"nmx{i}", [128, 1]) for i in range(2)]
        ex2 = [psb(f"ex{i}", [128, NE]) for i in range(2)]
        ssum2 = [psb(f"ssum{i}", [128, 1]) for i in range(2)]
        ps_p = [pps(f"ps_p{i}", [128, 512]) for i in range(4)]
        ps_m = [pps(f"ps_m{i}", [128, 512]) for i in range(2)]
        ps_t = pps("ps_t", [128, 512])
        ps_l = pps("ps_l", [128, 512])

        for (wt, src, nm) in ((wpa, G["w_pa"], "wpa"), (wpr, G["w_pr"], "wpr"), (wo, G["w_out"], "wo")):
            for k in range(8):
                P.dma("pool", lambda e, wt=wt, src=src, k=k: e.dma_start(out=wt[:, k, :], in_=src[k * 128:(k + 1) * 128, :]),
                      writes=[(nm, k)])
        P.dma("sp", lambda e: e.dma_start(out=wr[:], in_=G["w_router"].rearrange("(k p) n -> p k n", p=128)), writes=["wr"])
        rowtmp = psb("rowtmp", [1, D])
        g1b = _bcast_row(nc, P, G, psb, ps_t, "ln1g", G["ln1_g"].ap(), D, row=rowtmp)
        b1b = _bcast_row(nc, P, G, psb, ps_t, "ln1b", G["ln1_b"].ap(), D, row=rowtmp)
        brb = _bcast_row(nc, P, G, psb, ps_t, "brb", G["b_router"].ap(), NE, row=rowtmp)

        pp = 0
        gq_i = 0
        blocks = []
        tok0 = 0
        for (seg, ntok, sidx) in (("A", SA, 0), ("B", H, 1)):
            for b in range(ntok // 512):
                blocks.append((seg, sidx, b, b * 512, tok0))
            tok0 += ntok

        def mloop_gen(bidx):
            nonlocal pp, gq_i
            seg, sidx, b, t0, tok0 = blocks[bidx]
            at_, rc_ = attT[bidx % 2], recT[bidx % 2]
            ak, rk = ("attT", bidx % 2), ("recT", bidx % 2)
            mT = mT2[bidx % 2]
            P.dma("sp", lambda e: e.dma_start(out=at_[:], in_=ATT[seg][:, :, t0:t0 + 512].rearrange("h p t -> p h t")),
                  reads=[("ATT", seg)], writes=[ak])
            P.dma("sp", lambda e: e.dma_start(out=rc_[:], in_=REC[seg][:, :, t0:t0 + 512].rearrange("h p t -> p h t")),
                  reads=[("REC", seg)], writes=[rk])
            for m in range(8):
                gl_ = glq[gq_i % 3]
                gk = ("glq", gq_i % 3)
                gq_i += 1
                for hh in range(2):
                    P.dma("sp", lambda e, gl_=gl_, m=m, hh=hh: e.dma_start(out=gl_[:, hh, :], in_=GL[seg][hh * 8 + m, :, t0:t0 + 512]),
                          reads=[("GL", seg)], writes=[gk])
                pa, pr = ps_p[pp % 4], ps_p[(pp + 1) % 4]
                pak, prk = ("ps_p", pp % 4), ("ps_p", (pp + 1) % 4)
                ta_, tb_ = ta[(pp // 2) % 2], tb[(pp // 2) % 2]
                tak, tbk = ("ta", (pp // 2) % 2), ("tb", (pp // 2) % 2)
                pp += 2
                for k in range(8):
                    P.op("pe", lambda e, pa=pa, k=k, m=m: e.matmul(pa[:], lhsT=wpa[:, k, m * 128:(m + 1) * 128], rhs=at_[:, k, :],
                                                                  start=(k == 0), stop=(k == 7)),
                         reads=[("wpa", k), ak], writes=[pak], accum=True)
                for k in range(8):
                    P.op("pe", lambda e, pr=pr, k=k, m=m: e.matmul(pr[:], lhsT=wpr[:, k, m * 128:(m + 1) * 128], rhs=rc_[:, k, :],
                                                                  start=(k == 0), stop=(k == 7)),
                         reads=[("wpr", k), rk], writes=[prk], accum=True)
                P.op("dve", lambda e, ta_=ta_, pa=pa, gl_=gl_: e.tensor_tensor(out=ta_[:], in0=pa[:], in1=gl_[:, 0, :], op=ALU.mult),
                     reads=[pak, gk], writes=[tak])
                P.op("dve", lambda e, tb_=tb_, pr=pr, gl_=gl_: e.tensor_tensor(out=tb_[:], in0=pr[:], in1=gl_[:, 1, :], op=ALU.mult),
                     reads=[prk, gk], writes=[tbk])
                P.op("pool", lambda e, ta_=ta_, tb_=tb_, m=m: e.tensor_tensor(out=mT[:, m, :], in0=ta_[:], in1=tb_[:], op=ALU.add),
                     reads=[tak, tbk], writes=[("mT", bidx % 2, m)])
                yield

        def tile_gen(t, par, bidx):
            seg, sidx, b, t0, tok0 = blocks[bidx]
            mT = mT2[bidx % 2]
            tile_idx = (tok0 + t0) // 128 + t
            r0 = tok0 + t0 + t * 128
            xt_, yt_, ht_, hb_ = xt[par], yt[par], ht[par], hb[par]
            xk, yk, hk, hbk = ("xt", par), ("yt", par), ("ht", par), ("hb", par)
            st6, mv, sd, rstd = st62[par], mv2[par], sd2[par], rstd2[par]
            lg, m8, nmx, ex, ssum, h2T = lg2[par], m82[par], nmx2[par], ex2[par], ssum2[par], h2T2[par]
            tag = ("ln1", par)
            P.dma("sp", lambda e: e.dma_start(out=xt_[:], in_=x_seg[seg][t0 + t * 128:t0 + (t + 1) * 128, :]), writes=[xk])
            for n in range(2):
                pm = ps_m[n]
                for k in range(8):
                    P.op("pe", lambda e, pm=pm, k=k, n=n: e.matmul(pm[:], lhsT=mT[:, k, t * 128:(t + 1) * 128],
                                                                  rhs=wo[:, k, n * 512:(n + 1) * 512], start=(k == 0), stop=(k == 7)),
                         reads=[("mT", bidx % 2, k), ("wo", k)], writes=[("ps_m", n)], accum=True)
                P.op("dve", lambda e, pm=pm, n=n: e.tensor_tensor(out=yt_[:, n * 512:(n + 1) * 512], in0=pm[:],
                                                                 in1=bcs[:, 0, n * 512:(n + 1) * 512], op=ALU.mult),
                     reads=[("ps_m", n), "bcs"], writes=[yk])
            yield
            P.op("dve", lambda e: e.scalar_tensor_tensor(out=yt_[:], in0=xt_[:], scalar=alpha, in1=yt_[:], op0=ALU.mult, op1=ALU.add),
                 reads=[xk, yk], writes=[yk])
            yield
            for c in range(2):
                P.op("dve", lambda e, c=c: e.bn_stats(out=st6[:, c, :], in_=yt_[:, c * 512:(c + 1) * 512]), reads=[yk], writes=[(tag, "st6", c)])
            yield
            P.op("dve", lambda e: e.bn_aggr(out=mv[:], in_=st6[:].rearrange("p a b -> p (a b)")),
                 reads=[(tag, "st6", 0), (tag, "st6", 1)], writes=[(tag, "mv")])
            yield
            P.op("act", lambda e: e.activation(out=sd[:], in_=mv[:, 1:2], func=AF.Sqrt, bias=LN_EPS), reads=[(tag, "mv")], writes=[(tag, "sd")])
            yield
            P.op("dve", lambda e: e.reciprocal(out=rstd[:], in_=sd[:]), reads=[(tag, "sd")], writes=[(tag, "rstd")])
            P.op("dve", lambda e: e.scalar_tensor_tensor(out=yt_[:], in0=yt_[:], scalar=mv[:, 0:1], in1=g1b[:], op0=ALU.subtract, op1=ALU.mult),
                 reads=[yk, (tag, "mv"), "ln1g"], writes=[yk])
            yield
            P.op("dve", lambda e: e.scalar_tensor_tensor(out=yt_[:], in0=yt_[:], scalar=rstd[:, 0:1], in1=b1b[:], op0=ALU.mult, op1=ALU.add),
                 reads=[yk, (tag, "rstd"), "ln1b"], writes=[yk])
            yield
            P.dma("sp", lambda e: e.dma_start(out=X1[r0:r0 + 128, :], in_=yt_[:]), reads=[yk], writes=[("X1", r0)])
            for c in range(2):
                P.op("dve", lambda e, c=c: e.bn_stats(out=st6[:, c, :], in_=yt_[:, c * 512:(c + 1) * 512]), reads=[yk], writes=[(tag, "st6", c)])
            yield
            P.op("dve", lambda e: e.bn_aggr(out=mv[:], in_=st6[:].rearrange("p a b -> p (a b)")),
                 reads=[(tag, "st6", 0), (tag, "st6", 1)], writes=[(tag, "mv")])
            yield
            P.op("act", lambda e: e.activation(out=sd[:], in_=mv[:, 1:2], func=AF.Sqrt, bias=LN_EPS), reads=[(tag, "mv")], writes=[(tag, "sd")])
            yield
            P.op("dve", lambda e: e.reciprocal(out=rstd[:], in_=sd[:]), reads=[(tag, "sd")], writes=[(tag, "rstd")])
            P.op("dve", lambda e: e.scalar_tensor_tensor(out=ht_[:], in0=yt_[:], scalar=mv[:, 0:1], in1=bcs[:, 2, :], op0=ALU.subtract, op1=ALU.mult),
                 reads=[yk, (tag, "mv"), "bcs"], writes=[hk])
            yield
            P.op("dve", lambda e: e.scalar_tensor_tensor(out=ht_[:], in0=ht_[:], scalar=rstd[:, 0:1], in1=bcs[:, 1, :], op0=ALU.mult, op1=ALU.add),
                 reads=[hk, (tag, "rstd"), "bcs"], writes=[hk])
            yield
            P.op("act", lambda e: e.activation(out=hb_[:], in_=ht_[:], func=AF.Identity), reads=[hk], writes=[hbk])
            P.dma("sp", lambda e: e.dma_start(out=H2[r0:r0 + 128, :], in_=hb_[:]), reads=[hbk], writes=[("H2", r0)])
            for half in range(2):
                for kk in range(4):
                    k = half * 4 + kk
                    P.op("pe", lambda e, kk=kk, k=k: e.transpose(out=ps_t[:, kk * 128:(kk + 1) * 128], in_=ht_[:, k * 128:(k + 1) * 128],
                                                                identity=ident[:]),
                         reads=[hk, "ident"], writes=["ps_t"], accum=True)
                P.op("act" if half == 0 else "dve",
                     (lambda e, half=half: e.activation(out=h2T[:, half * 4:(half + 1) * 4, :].rearrange("p a b -> p (a b)"), in_=ps_t[:], func=AF.Identity))
                     if half == 0 else
                     (lambda e, half=half: e.tensor_copy(out=h2T[:, half * 4:(half + 1) * 4, :].rearrange("p a b -> p (a b)"), in_=ps_t[:])),
                     reads=["ps_t"], writes=[("h2T", par, half)])
            yield
            for k in range(8):
                P.op("pe", lambda e, k=k: e.matmul(ps_l[:, 0:NE], lhsT=h2T[:, k, :], rhs=wr[:, k, :], start=(k == 0), stop=(k == 7)),
                     reads=[("h2T", par, k // 4), "wr"], writes=["ps_l"], accum=True)
            P.op("dve", lambda e: e.tensor_tensor(out=lg[:], in0=ps_l[:, 0:NE], in1=brb[:], op=ALU.add), reads=["ps_l", "brb"], writes=[("lg", par)])
            yield
            P.op("dve", lambda e: e.max(out=m8[:], in_=lg[:]), reads=[("lg", par)], writes=[("m8", par)])
            yield
            P.op("dve", lambda e: e.tensor_scalar(out=maskS[:, tile_idx, :], in0=lg[:], scalar1=m8[:, 3:4], scalar2=None, op0=ALU.is_ge),
                 reads=[("lg", par), ("m8", par)], writes=[("maskS", tile_idx)])
            P.op("dve", lambda e: e.tensor_scalar(out=nmx[:], in0=m8[:, 0:1], scalar1=-1.0, scalar2=None, op0=ALU.mult),
                 reads=[("m8", par)], writes=[("nmx", par)])
            yield
            P.op("act", lambda e: e.activation(out=ex[:], in_=lg[:], func=AF.Exp, bias=nmx[:, 0:1]), reads=[("lg", par), ("nmx", par)], writes=[("ex", par)])
            yield
            P.op("dve", lambda e: e.tensor_tensor(out=ex[:], in0=ex[:], in1=maskS[:, tile_idx, :], op=ALU.mult),
                 reads=[("ex", par), ("maskS", tile_idx)], writes=[("ex", par)])
            yield
            P.op("dve", lambda e: e.reduce_sum(out=ssum[:], in_=ex[:], axis=AX.X), reads=[("ex", par)], writes=[("ssum", par)])
            yield
            P.op("dve", lambda e: e.reciprocal(out=ssum[:], in_=ssum[:]), reads=[("ssum", par)], writes=[("ssum", par)])
            yield
            P.op("dve", lambda e: e.tensor_scalar(out=wS[:, tile_idx, :], in0=ex[:], scalar1=ssum[:, 0:1], scalar2=None, op0=ALU.mult),
                 reads=[("ex", par), ("ssum", par)], writes=[("wS", tile_idx)])


        def drain(g_):
            for _ in g_:
                pass

        drain(mloop_gen(0))
        cur_sidx = None
        for bidx in range(len(blocks)):
            seg, sidx, b, t0, tok0 = blocks[bidx]
            if sidx != cur_sidx:
                P.dma("sp", lambda e, sidx=sidx: e.dma_start(out=bcs[:], in_=BC[:, sidx, :, :]), reads=["BC"], writes=["bcs"])
                cur_sidx = sidx
            nxt = mloop_gen(bidx + 1) if bidx + 1 < len(blocks) else None
            rounds = 0
            for tp in range(2):
                gens = [tile_gen(tp * 2, 0, bidx), tile_gen(tp * 2 + 1, 1, bidx)]
                alive = [True, True]
                lag = 2
                step = 0
                while any(alive):
                    for gi_, g_ in enumerate(gens):
                        if not alive[gi_]:
                            continue
                        if gi_ == 1 and step < lag:
                            continue
                        try:
                            next(g_)
                        except StopIteration:
                            alive[gi_] = False
                    step += 1
                    rounds += 1
                    if nxt is not None and rounds % 5 == 0:
                        try:
                            next(nxt)
                        except StopIteration:
                            nxt = None
            if nxt is not None:
                drain(nxt)
        P.end_phase()


def phase5(nc, P, cfg, G):
    NE, NT, NBLK = cfg.NE, cfg.NT, cfg.NBLK
    NTL = NT // 128
    H2, XS = G["H2"], G["XS"]
    maskS, wS, d4i, w4, idxw, idxb1, idxb2 = G["maskS"], G["wS"], G["d4i"], G["w4"], G["idxw"], G["idxb1"], G["idxb2"]
    ones_bf = G["ones_bf"]
    W = NTL * NE
    with ExitStack() as st:
        psb = lambda name, shape, dt=F32: st.enter_context(nc.sbuf_tensor("p5_" + name, list(shape), dt))
        pps = lambda name, shape, dt=F32: st.enter_context(nc.psum_tensor("p5_" + name, list(shape), dt))
        mb = psb("mb", [128, W], BF16)
        tri = psb("tri", [128, 128], BF16)
        trif = psb("trif", [128, 128])
        pre = psb("pre", [128, NTL, NE])
        tot = psb("tot", [128, NTL, NE])
        off = psb("off", [128, NTL, NE])
        cntf = psb("cntf", [128, NE])
        cnti = psb("cnti", [128, NE], I32)
        padf = psb("padf", [128, NE])
        pend = psb("pend", [128, NE])
        pstart = psb("pstart", [128, NE])
        onesr = psb("onesr", [128, NE])
        dest = psb("dest", [128, NTL, NE])
        d8 = psb("d8", [128, 8])
        d4f = psb("d4f", [128, NTL, 4])
        oh = psb("oh", [128, NE])
        bst = psb("bst", [128, NBLK])
        eb = psb("eb", [128, NBLK])
        pk = psb("pk", [128, 8])
        pid = psb("pid", [128, 1])
        idxf = psb("idxf", [128, NBLK, 8])
        tmpf = psb("tmpf", [128, NBLK])
        hrow = [psb(f"hrow{i}", [128, D], BF16) for i in range(3)]
        ps_a = [pps(f"ps_a{i}", [128, 512]) for i in range(2)]

        P.op("pool", lambda e: e.memset(trif[:], 1.0), writes=["trif"])
        P.op("pool", lambda e: e.affine_select(out=trif[:], in_=trif[:], pattern=[[1, 128]], compare_op=ALU.is_gt, fill=0.0,
                                               base=0, channel_multiplier=-1), reads=["trif"], writes=["trif"])
        P.op("dve", lambda e: e.tensor_copy(out=tri[:], in_=trif[:]), reads=["trif"], writes=["tri"])
        P.op("dve", lambda e: e.tensor_copy(out=mb[:], in_=maskS[:].rearrange("p a b -> p (a b)")),
             reads=[("maskS", t) for t in range(NTL)], writes=["mb"])
        pre_f = pre[:].rearrange("p a b -> p (a b)")
        tot_f = tot[:].rearrange("p a b -> p (a b)")
        for c0 in range(0, W, 512):
            w = min(512, W - c0)
            P.op("pe", lambda e, c0=c0, w=w: e.matmul(ps_a[0][:, 0:w], lhsT=tri[:], rhs=mb[:, c0:c0 + w], start=True, stop=True),
                 reads=["tri", "mb"], writes=[("ps_a", 0)])
            P.op("pe", lambda e, c0=c0, w=w: e.matmul(ps_a[1][:, 0:w], lhsT=ones_bf[:], rhs=mb[:, c0:c0 + w], start=True, stop=True),
                 reads=["ones_bf", "mb"], writes=[("ps_a", 1)])
            P.op("dve", lambda e, c0=c0, w=w: e.tensor_copy(out=pre_f[:, c0:c0 + w], in_=ps_a[0][:, 0:w]), reads=[("ps_a", 0)], writes=["pre"])
            P.op("act", lambda e, c0=c0, w=w: e.activation(out=tot_f[:, c0:c0 + w], in_=ps_a[1][:, 0:w], func=AF.Identity),
                 reads=[("ps_a", 1)], writes=["tot"])
        P.op("dve", lambda e: e.tensor_reduce(out=cntf[:], in_=tot[:].rearrange("p a b -> p b a"), axis=AX.X, op=ALU.add),
             reads=["tot"], writes=["cntf"])
        P.op("dve", lambda e: e.tensor_scalar(out=cntf[:], in0=cntf[:], scalar1=float(MB - 1), scalar2=None, op0=ALU.add),
             reads=["cntf"], writes=["cntf"])
        P.op("dve", lambda e: e.tensor_copy(out=cnti[:], in_=cntf[:]), reads=["cntf"], writes=["cnti"])
        P.op("dve", lambda e: e.tensor_scalar(out=cnti[:], in0=cnti[:], scalar1=9, scalar2=9, op0=ALU.arith_shift_right,
                                              op1=ALU.logical_shift_left), reads=["cnti"], writes=["cnti"])
        P.op("dve", lambda e: e.tensor_copy(out=padf[:], in_=cnti[:]), reads=["cnti"], writes=["padf"])
        P.op("dve", lambda e: e.memset(onesr[:], 1.0), writes=["onesr"])
        P.op("dve", lambda e: e.tensor_tensor_scan(out=pend[:], data0=onesr[:], data1=padf[:], initial=0.0, op0=ALU.mult, op1=ALU.add),
             reads=["onesr", "padf"], writes=["pend"])
        P.op("dve", lambda e: e.tensor_tensor(out=pstart[:], in0=pend[:], in1=padf[:], op=ALU.subtract), reads=["pend", "padf"], writes=["pstart"])
        P.op("dve", lambda e: e.tensor_copy(out=off[:, 0, :], in_=pstart[:]), reads=["pstart"], writes=[("off", 0)])
        for t in range(1, NTL):
            P.op("dve", lambda e, t=t: e.tensor_tensor(out=off[:, t, :], in0=off[:, t - 1, :], in1=tot[:, t - 1, :], op=ALU.add),
                 reads=[("off", t - 1), "tot"], writes=[("off", t)])
        dest_f = dest[:].rearrange("p a b -> p (a b)")
        P.op("dve", lambda e: e.tensor_tensor(out=dest_f, in0=off[:].rearrange("p a b -> p (a b)"), in1=pre_f, op=ALU.add),
             reads=[("off", t) for t in range(NTL)] + ["pre"], writes=["dest"])
        P.op("dve", lambda e: e.scalar_tensor_tensor(out=dest_f, in0=dest_f, scalar=1.0, in1=maskS[:].rearrange("p a b -> p (a b)"),
                                                     op0=ALU.add, op1=ALU.mult), reads=["dest"], writes=["dest"])
        P.op("dve", lambda e: e.tensor_scalar(out=dest_f, in0=dest_f, scalar1=-1.0, scalar2=None, op0=ALU.add), reads=["dest"], writes=["dest"])
        for t in range(NTL):
            P.op("dve", lambda e, t=t: e.max(out=d8[:], in_=dest[:, t, :]), reads=["dest"], writes=["d8"])
            P.op("dve", lambda e, t=t: e.tensor_copy(out=d4f[:, t, :], in_=d8[:, 0:4]), reads=["d8"], writes=[("d4f", t)])
            for k in range(4):
                P.op("dve", lambda e, t=t, k=k: e.tensor_scalar(out=oh[:], in0=dest[:, t, :], scalar1=d8[:, k:k + 1], scalar2=None, op0=ALU.is_equal),
                     reads=["dest", "d8"], writes=["oh"])
                P.op("dve", lambda e, t=t: e.tensor_tensor(out=oh[:], in0=oh[:], in1=wS[:, t, :], op=ALU.mult), reads=["oh", ("wS", t)], writes=["oh"])
                P.op("dve", lambda e, t=t, k=k: e.reduce_sum(out=w4[:, t, k:k + 1], in_=oh[:], axis=AX.X), reads=["oh"], writes=[("w4", t, k)])
        P.op("dve", lambda e: e.tensor_copy(out=d4i[:].rearrange("p a b -> p (a b)"), in_=d4f[:].rearrange("p a b -> p (a b)")),
             reads=[("d4f", t) for t in range(NTL)], writes=["d4i"])
        P.op("pool", lambda e: e.iota(bst[:], pattern=[[MB, NBLK]], base=0, channel_multiplier=0, allow_small_or_imprecise_dtypes=True),
             writes=["bst"])
        P.op("pool", lambda e: e.iota(pk[:], pattern=[[128, 8]], base=0, channel_multiplier=1, allow_small_or_imprecise_dtypes=True),
             writes=["pk"])
        P.op("pool", lambda e: e.iota(pid[:], pattern=[[0, 1]], base=0, channel_multiplier=1, allow_small_or_imprecise_dtypes=True),
             writes=["pid"])
        P.op("dve", lambda e: e.memset(eb[:], 0.0), writes=["eb"])
        for ex_ in range(NE):
            P.op("dve", lambda e, ex_=ex_: e.scalar_tensor_tensor(out=eb[:], in0=bst[:], scalar=pend[:, ex_:ex_ + 1], in1=eb[:],
                                                                  op0=ALU.is_ge, op1=ALU.add), reads=["bst", "pend", "eb"], writes=["eb"])
        P.op("dve", lambda e: e.tensor_scalar(out=eb[:], in0=eb[:], scalar1=float(NE - 1), scalar2=None, op0=ALU.min), reads=["eb"], writes=["eb"])
        for k in range(8):
            P.op("dve", lambda e, k=k: e.tensor_scalar(out=idxf[:, :, k], in0=eb[:], scalar1=float(D), scalar2=pk[:, k:k + 1],
                                                      op0=ALU.mult, op1=ALU.add), reads=["eb", "pk"], writes=[("idxf", k)])
        P.op("dve", lambda e: e.tensor_copy(out=idxw[:].rearrange("p a b -> p (a b)"), in_=idxf[:].rearrange("p a b -> p (a b)")),
             reads=[("idxf", k) for k in range(8)], writes=["idxw"])
        P.op("dve", lambda e: e.tensor_scalar(out=tmpf[:], in0=eb[:], scalar1=128.0, scalar2=pid[:, 0:1], op0=ALU.mult, op1=ALU.add),
             reads=["eb", "pid"], writes=["tmpf"])
        P.op("dve", lambda e: e.tensor_copy(out=idxb1[:], in_=tmpf[:]), reads=["tmpf"], writes=["idxb1"])
        P.op("dve", lambda e: e.tensor_copy(out=idxb2[:], in_=eb[:]), reads=["eb"], writes=["idxb2"])
        for t in range(NTL):
            hr = hrow[t % 3]
            P.dma("sp", lambda e, hr=hr, t=t: e.dma_start(out=hr[:], in_=H2[t * 128:(t + 1) * 128, :]), reads=[("H2", t * 128)], writes=[("hrow", t % 3)])
            for k in range(4):
                P.dma("pool", lambda e, hr=hr, t=t, k=k: e.indirect_dma_start(
                    out=XS, out_offset=bass.IndirectOffsetOnAxis(ap=d4i[:, t, k:k + 1], axis=0), in_=hr[:], in_offset=None),
                    reads=[("hrow", t % 3), "d4i"], writes=[("XSs", t, k)])
        P.end_phase()


def phase6(nc, P, cfg, G):
    NE, NBLK = cfg.NE, cfg.NBLK
    XS, YS = G["XS"], G["YS"]
    w1, w2, b1T, b2 = G["w1"], G["w2"], G["b1T"], G["b2"]
    idxw, idxb1, idxb2, ident_bf = G["idxw"], G["idxb1"], G["idxb2"], G["ident_bf"]
    with ExitStack() as st:
        psb = lambda name, shape, dt=F32: st.enter_context(nc.sbuf_tensor("p6_" + name, list(shape), dt))
        pps = lambda name, shape, dt=F32: st.enter_context(nc.psum_tensor("p6_" + name, list(shape), dt))
        w1s = [psb(f"w1s{i}", [128, 8, 2 * DFF], BF16) for i in range(2)]
        w2s = [psb(f"w2s{i}", [128, 8, D], BF16) for i in range(2)]
        b1s = [psb(f"b1s{i}", [128, 16]) for i in range(2)]
        b2s = [psb(f"b2s{i}", [128, D]) for i in range(2)]
        xs = [psb(f"xs{i}", [128, 4, D], BF16) for i in range(2)]
        xsT = psb("xsT", [128, 8, 512], BF16)
        actT = psb("actT", [128, 8, 512], BF16)
        glu = [psb(f"glu{i}", [128, 512]) for i in range(2)]
        sg = [psb(f"sg{i}", [128, 512]) for i in range(2)]
        lin = [psb(f"lin{i}", [128, 512]) for i in range(2)]
        yo = [psb(f"yo{i}", [128, D]) for i in range(2)]
        ps_x = [pps(f"ps_x{i}", [128, 512], BF16) for i in range(2)]
        ps_g = [pps(f"ps_g{i}", [128, 512]) for i in range(2)]
        ps_l = [pps(f"ps_l{i}", [128, 512]) for i in range(2)]
        ps_y = [pps(f"ps_y{i}", [128, 512]) for i in range(2)]
        ji = 0
        yi = 0
        px = 0
        def load_blk(b):
            w1_, w2_, b1_, b2_, xs_ = w1s[b % 2], w2s[b % 2], b1s[b % 2], b2s[b % 2], xs[b % 2]
            wk1, wk2, bk1, bk2, xk = ("w1s", b % 2), ("w2s", b % 2), ("b1s", b % 2), ("b2s", b % 2), ("xs", b % 2)
            P.dma("sp", lambda e, xs_=xs_, b=b: e.dma_start(out=xs_[:], in_=XS[b * MB:(b + 1) * MB, :].rearrange("(t p) d -> p t d", p=128)),
                  reads=["XS"], writes=[xk])
            for k in range(8):
                P.dma("pool", lambda e, w1_=w1_, b=b, k=k: e.indirect_dma_start(
                    out=w1_[:, k, :], out_offset=None, in_=w1.ap(), in_offset=bass.IndirectOffsetOnAxis(ap=idxw[:, b, k:k + 1], axis=0)),
                    reads=["idxw"], writes=[(wk1, k)])
            for k in range(8):
                P.dma("pool", lambda e, w2_=w2_, b=b, k=k: e.indirect_dma_start(
                    out=w2_[:, k, :], out_offset=None, in_=w2.ap(), in_offset=bass.IndirectOffsetOnAxis(ap=idxw[:, b, k:k + 1], axis=0)),
                    reads=["idxw"], writes=[(wk2, k)])
            P.dma("pool", lambda e, b1_=b1_, b=b: e.indirect_dma_start(
                out=b1_[:], out_offset=None, in_=b1T.ap(), in_offset=bass.IndirectOffsetOnAxis(ap=idxb1[:, b:b + 1], axis=0)),
                reads=["idxb1"], writes=[bk1])
            P.dma("pool", lambda e, b2_=b2_, b=b: e.indirect_dma_start(
                out=b2_[:], out_offset=None, in_=b2.ap(), in_offset=bass.IndirectOffsetOnAxis(ap=idxb2[:, b:b + 1], axis=0)),
                reads=["idxb2"], writes=[bk2])

        load_blk(0)
        for b in range(NBLK):
            if b + 1 < NBLK:
                load_blk(b + 1)
            w1_, w2_, b1_, b2_, xs_ = w1s[b % 2], w2s[b % 2], b1s[b % 2], b2s[b % 2], xs[b % 2]
            wk1, wk2, bk1, bk2, xk = ("w1s", b % 2), ("w2s", b % 2), ("b1s", b % 2), ("b2s", b % 2), ("xs", b % 2)
            P.op("dve", lambda e, b1_=b1_: e.tensor_scalar(out=b1_[:, 8:16], in0=b1_[:, 8:16], scalar1=1.0, scalar2=None, op0=ALU.add),
                 reads=[bk1], writes=[bk1])
            for k in range(8):
                pxs = ps_x[px % 2]
                pxk = ("ps_x", px % 2)
                px += 1
                for t in range(4):
                    P.op("pe", lambda e, pxs=pxs, t=t, k=k, xs_=xs_: e.transpose(out=pxs[:, t * 128:(t + 1) * 128], in_=xs_[:, t, k * 128:(k + 1) * 128],
                                                                                identity=ident_bf[:]),
                         reads=[xk, "ident_bf"], writes=[pxk], accum=True)
                P.op("act" if k % 2 == 0 else "dve",
                     (lambda e, pxs=pxs, k=k: e.activation(out=xsT[:, k, :], in_=pxs[:], func=AF.Identity)) if k % 2 == 0 else
                     (lambda e, pxs=pxs, k=k: e.tensor_copy(out=xsT[:, k, :], in_=pxs[:])),
                     reads=[pxk], writes=[("xsT", k)])
            for j in range(8):
                pg, pl = ps_g[ji % 2], ps_l[ji % 2]
                pgk, plk = ("ps_g", ji % 2), ("ps_l", ji % 2)
                gl_, sg_, ln_ = glu[ji % 2], sg[ji % 2], lin[ji % 2]
                glk, sgk, lnk = ("glu", ji % 2), ("sg", ji % 2), ("lin", ji % 2)
                ji += 1
                for k in range(8):
                    P.op("pe", lambda e, pg=pg, k=k, j=j, w1_=w1_: e.matmul(pg[:], lhsT=w1_[:, k, j * 128:(j + 1) * 128], rhs=xsT[:, k, :],
                                                                          start=(k == 0), stop=(k == 7)),
                         reads=[(wk1, k), ("xsT", k)], writes=[pgk], accum=True)
                for k in range(8):
                    P.op("pe", lambda e, pl=pl, k=k, j=j, w1_=w1_: e.matmul(pl[:], lhsT=w1_[:, k, DFF + j * 128:DFF + (j + 1) * 128], rhs=xsT[:, k, :],
                                                                          start=(k == 0), stop=(k == 7)),
                         reads=[(wk1, k), ("xsT", k)], writes=[plk], accum=True)
                P.op("dve", lambda e, gl_=gl_, pg=pg, b1_=b1_, j=j: e.tensor_scalar(out=gl_[:], in0=pg[:], scalar1=b1_[:, j:j + 1], scalar2=LIMIT,
                                                                               op0=ALU.add, op1=ALU.min), reads=[pgk, bk1], writes=[glk])
                P.op("act", lambda e, sg_=sg_, gl_=gl_: e.activation(out=sg_[:], in_=gl_[:], func=AF.Sigmoid, scale=ALPHA), reads=[glk], writes=[sgk])
                P.op("dve", lambda e, ln_=ln_, pl=pl, b1_=b1_, j=j: e.tensor_scalar(out=ln_[:], in0=pl[:], scalar1=b1_[:, 8 + j:9 + j], scalar2=LIMIT + 1.0,
                                                                               op0=ALU.add, op1=ALU.min), reads=[plk, bk1], writes=[lnk])
                P.op("pool", lambda e, gl_=gl_, sg_=sg_: e.tensor_tensor(out=gl_[:], in0=gl_[:], in1=sg_[:], op=ALU.mult), reads=[glk, sgk], writes=[glk])
                P.op("dve", lambda e, gl_=gl_, ln_=ln_, j=j: e.scalar_tensor_tensor(out=actT[:, j, :], in0=ln_[:], scalar=1.0 - LIMIT, in1=gl_[:],
                                                                                  op0=ALU.max, op1=ALU.mult),
                     reads=[glk, lnk], writes=[("actT", j)])
            for t in range(4):
                yo_ = yo[yi % 2]
                yok = ("yo", yi % 2)
                yi += 1
                for n in range(2):
                    py = ps_y[n]
                    for j in range(8):
                        P.op("pe", lambda e, py=py, j=j, t=t, n=n, w2_=w2_: e.matmul(py[:], lhsT=actT[:, j, t * 128:(t + 1) * 128],
                                                                                   rhs=w2_[:, j, n * 512:(n + 1) * 512], start=(j == 0), stop=(j == 7)),
                             reads=[("actT", j), (wk2, j)], writes=[("ps_y", n)], accum=True)
                    P.op("dve", lambda e, py=py, yo_=yo_, n=n, b2_=b2_: e.tensor_tensor(out=yo_[:, n * 512:(n + 1) * 512], in0=py[:],
                                                                                       in1=b2_[:, n * 512:(n + 1) * 512], op=ALU.add),
                         reads=[("ps_y", n), bk2], writes=[yok])
                r0 = b * MB + t * 128
                P.dma("sp", lambda e, yo_=yo_, r0=r0: e.dma_start(out=YS[r0:r0 + 128, :], in_=yo_[:]), reads=[yok], writes=[("YS", r0)])
        P.end_phase()


def phase7(nc, P, cfg, G):
    SA, H, NT = cfg.SA, cfg.H, cfg.NT
    NTL = NT // 128
    X1, YS, BC, y_seg = G["X1"], G["YS"], G["BC"], G["y_seg"]
    d4i, w4 = G["d4i"], G["w4"]
    alpha = float(2.0 ** 0.25)
    with ExitStack() as st:
        psb = lambda name, shape, dt=F32: st.enter_context(nc.sbuf_tensor("p7_" + name, list(shape), dt))
        pps = lambda name, shape, dt=F32: st.enter_context(nc.psum_tensor("p7_" + name, list(shape), dt))
        yg = [psb(f"yg{i}", [128, 4, D]) for i in range(2)]
        x1t = [psb(f"x1t{i}", [128, D]) for i in range(2)]
        acc = [psb(f"acc{i}", [128, D]) for i in range(2)]
        bcs = psb("bcs", [128, D])
        st6 = psb("st6", [128, 2, 6])
        mv = psb("mv", [128, 2])
        sd = psb("sd", [128, 1])
        rstd = psb("rstd", [128, 1])
        ps_t = pps("ps_t", [128, 512])
        g2b = _bcast_row(nc, P, G, psb, ps_t, "ln2g", G["ln2_g"].ap(), D)
        b2b = _bcast_row(nc, P, G, psb, ps_t, "ln2b", G["ln2_b"].ap(), D)
        for t in range(NTL):
            r0 = t * 128
            seg, sidx, lr = ("A", 0, r0) if r0 < SA else ("B", 1, r0 - SA)
            if r0 == 0 or r0 == SA:
                P.dma("sp", lambda e, sidx=sidx: e.dma_start(out=bcs[:], in_=BC[:, sidx, 3, :]), reads=["BC"], writes=["bcs"])
            yg_, x1_, ac_ = yg[t % 2], x1t[t % 2], acc[t % 2]
            ygk, x1k, ack = ("yg", t % 2), ("x1t", t % 2), ("acc", t % 2)
            P.dma("sp", lambda e, x1_=x1_, r0=r0: e.dma_start(out=x1_[:], in_=X1[r0:r0 + 128, :]), reads=[("X1", r0)], writes=[x1k])
            for k in range(4):
                P.dma("pool", lambda e, yg_=yg_, t=t, k=k: e.indirect_dma_start(
                    out=yg_[:, k, :], out_offset=None, in_=YS, in_offset=bass.IndirectOffsetOnAxis(ap=d4i[:, t, k:k + 1], axis=0)),
                    reads=["d4i", "YS"], writes=[(ygk, k)])
            P.op("dve", lambda e, ac_=ac_, yg_=yg_, t=t: e.tensor_scalar(out=ac_[:], in0=yg_[:, 0, :], scalar1=w4[:, t, 0:1], scalar2=None, op0=ALU.mult),
                 reads=[(ygk, 0), ("w4", t, 0)], writes=[ack])
            for k in range(1, 4):
                P.op("dve", lambda e, ac_=ac_, yg_=yg_, t=t, k=k: e.scalar_tensor_tensor(
                    out=ac_[:], in0=yg_[:, k, :], scalar=w4[:, t, k:k + 1], in1=ac_[:], op0=ALU.mult, op1=ALU.add),
                    reads=[(ygk, k), ("w4", t, k), ack], writes=[ack])
            P.op("dve", lambda e, ac_=ac_: e.tensor_tensor(out=ac_[:], in0=ac_[:], in1=bcs[:], op=ALU.mult), reads=[ack, "bcs"], writes=[ack])
            P.op("dve", lambda e, ac_=ac_, x1_=x1_: e.scalar_tensor_tensor(out=ac_[:], in0=x1_[:], scalar=alpha, in1=ac_[:], op0=ALU.mult, op1=ALU.add),
                 reads=[x1k, ack], writes=[ack])
            _ln_stats(P, "ln2", ac_, st6, mv, sd, rstd, ack)
            P.op("dve", lambda e, ac_=ac_: e.scalar_tensor_tensor(out=ac_[:], in0=ac_[:], scalar=mv[:, 0:1], in1=g2b[:],
                                                                  op0=ALU.subtract, op1=ALU.mult), reads=[ack, ("ln2", "mv"), "ln2g"], writes=[ack])
            P.op("dve", lambda e, ac_=ac_: e.scalar_tensor_tensor(out=ac_[:], in0=ac_[:], scalar=rstd[:, 0:1], in1=b2b[:],
                                                                  op0=ALU.mult, op1=ALU.add), reads=[ack, ("ln2", "rstd"), "ln2b"], writes=[ack])
            P.dma("sp", lambda e, ac_=ac_, seg=seg, lr=lr: e.dma_start(out=y_seg[seg][lr:lr + 128, :], in_=ac_[:]), reads=[ack], writes=[("y", t)])
        P.end_phase()


def rope_tables_T(pos):
    pos = np.asarray(pos)
    rows = (pos // GRID_W).astype(np.float32)
    cols = (pos % GRID_W).astype(np.float32)
    axis_dim = HD // 2
    inv = (np.float32(ROPE_THETA) ** (-np.arange(0, axis_dim, 2, dtype=np.float32) / np.float32(axis_dim))).astype(np.float32)
    p = np.arange(128)
    j = p % 32
    comp = np.where((p // 64)[:, None] == 0, rows[None, :], cols[None, :]).astype(np.float32)
    ang = (comp * inv[j][:, None]).astype(np.float32)
    sgn = np.where((p % 64) < 32, -1.0, 1.0).astype(np.float32)[:, None]
    return np.cos(ang).astype(np.float32), (np.sin(ang) * sgn).astype(np.float32)


def make_in_maps(cfg, inputs):
    SA, H, NE = cfg.SA, cfg.H, cfg.NE
    f = lambda a: np.ascontiguousarray(np.asarray(a, dtype=np.float32))
    xp, xs = f(inputs["x_prompt"]), f(inputs["x_sample"])
    cp, cs = f(inputs["c_prompt"]), f(inputs["c_sample"])
    ident = np.eye(128, dtype=np.float32)
    perm = np.zeros((128, 128), np.float32)
    for m in range(128):
        partner = m + 32 if (m % 64) < 32 else m - 32
        perm[partner, m] = 1.0
    shared = {
        "ident": ident, "perm": perm,
        "w_ada": f(inputs["w_ada"][0]),
        "b_adaT": f(inputs["b_ada"][0].reshape(48, 128).T),
        "b_ada_row": f(inputs["b_ada"][0].reshape(1, -1)),
        "w_in": f(inputs["w_in"][0]),
        "qk_gain": f(np.stack([inputs["q_gain"][0], inputs["k_gain"][0]], axis=1)),
        "conv_wT": f(inputs["conv_w"][0].reshape(4, 8, 128).transpose(2, 1, 0)),
        "conv_bT": f(inputs["conv_b"][0].reshape(8, 128).T),
        "lru_wa": f(inputs["lru_wa"][0]), "lru_wx": f(inputs["lru_wx"][0]),
        "lru_baT": f(inputs["lru_ba"][0].reshape(2, 8, 128).transpose(2, 0, 1)),
        "lru_bxT": f(inputs["lru_bx"][0].reshape(2, 8, 128).transpose(2, 0, 1)),
        "lru_lamT": f(inputs["lru_lam"][0].reshape(2, 8, 128).transpose(2, 0, 1)),
        "w_pa": f(inputs["w_pa"][0]), "w_pr": f(inputs["w_pr"][0]), "w_out": f(inputs["w_out"][0]),
        "ln1_g": f(inputs["ln1_g"][0].reshape(1, -1)), "ln1_b": f(inputs["ln1_b"][0].reshape(1, -1)),
        "w_router": f(inputs["w_router"][0]), "b_router": f(inputs["b_router"][0].reshape(1, -1)),
        "w1": f(inputs["w1"][0].reshape(NE * D, 2 * DFF)),
        "b1T": f(inputs["b1"][0].reshape(NE, 16, 128).transpose(0, 2, 1).reshape(NE * 128, 16)),
        "w2": f(inputs["w2"][0].reshape(NE * DFF, D)),
        "b2": f(inputs["b2"][0]),
        "ln2_g": f(inputs["ln2_g"][0].reshape(1, -1)), "ln2_b": f(inputs["ln2_b"][0].reshape(1, -1)),
    }
    cosA, sinA = rope_tables_T(np.arange(SA))
    maps = []
    for c in range(8):
        sq, half = c // 2, c % 2
        own = np.arange(half * H, (half + 1) * H)
        oth = np.arange((1 - half) * H, (2 - half) * H)
        cosB, sinB = rope_tables_T(own)
        cosC, sinC = rope_tables_T(oth)
        cpair = np.stack([cp[c], cs[sq]], axis=0)
        cT = np.ascontiguousarray(cpair.reshape(2, 8, 128).transpose(2, 1, 0).reshape(128, 16))
        fl = np.zeros((128, 2), np.float32)
        fl[:, 0] = 1.0 if half == 1 else 0.0
        fl[:, 1] = 1.0 if half == 0 else 0.0
        m = dict(shared)
        m.update({
            "xa": f(xp[c]), "xb": f(xs[sq, own]), "xc": f(xs[sq, oth]),
            "cT": cT, "cosA": cosA, "sinA": sinA, "cosB": cosB, "sinB": sinB, "cosC": cosC, "sinC": sinC,
            "flags": fl,
        })
        maps.append(m)
    return maps


_NC_CACHE = {}


def run(cfg, inputs):
    key = (cfg.SA, cfg.H, cfg.NE, cfg.dbg)
    if key not in _NC_CACHE:
        _NC_CACHE[key] = build_program(cfg)
    nc = _NC_CACHE[key]
    maps = make_in_maps(cfg, inputs)
    used = set()
    for alloc in nc.allocations:
        try:
            if alloc.kind == "ExternalInput":
                used.add(alloc.memorylocations[0].name)
        except Exception:
            pass
    if used:
        maps = [{k: v for k, v in m.items() if k in used} for m in maps]
    import time as _t
    _t0 = _t.time()
    res = run_bass_kernel_spmd(nc, maps, core_ids=list(range(8)))
    print("[kernel] device run+transfer %.1fs, input MB/core %.1f" % (_t.time() - _t0, sum(v.nbytes for v in maps[0].values()) / 1e6))
    return res.results


def kernel(**inputs):
    cfg = Cfg()
    results = run(cfg, inputs)
    SA, H = cfg.SA, cfg.H
    yp = np.stack([np.asarray(results[c]["ya"], dtype=np.float32) for c in range(8)], axis=0)
    ys = np.zeros((4, 2 * H, D), np.float32)
    for c in range(8):
        ys[c // 2, (c % 2) * H:(c % 2 + 1) * H] = np.asarray(results[c]["yb"], dtype=np.float32)
    return (yp, ys)
```
